# Optimizing a Trainium2 kernel written in Bass

```python
import jax, jax.numpy as jnp
from jax import lax
import numpy as np

D_MODEL = 1024
BATCH = 4
SEQ = 8192
DEPTH = 1

MIX_WIDTH = D_MODEL
HEAD_DIM = 64
ATTN_WIDTH = MIX_WIDTH // 2
GMLP_WIDTH = MIX_WIDTH - ATTN_WIDTH
N_ATTN_HEADS = ATTN_WIDTH // HEAD_DIM
N_GMLP_HEADS = GMLP_WIDTH // HEAD_DIM
GMLP_HEAD_DIM = GMLP_WIDTH // N_GMLP_HEADS
IN_PROJ_WIDTH = 3 * ATTN_WIDTH + 2 * GMLP_WIDTH
MOBA_BLOCK = 256
MOBA_TOPK = 3
Q_CHUNK = 32
GMLP_CHUNK = 128
D_FF = 2816
ROPE_THETA = 10000.0
NORM_EPS = 1e-6
N_MOD = 9
NEG_INF = -1e30

kernel_name = "hymba_moba_gmlp_macaron_adaln"


def rmsnorm(x, g):
    xf = x.astype(jnp.float32)
    y = xf * lax.rsqrt(jnp.mean(xf * xf, axis=-1, keepdims=True) + NORM_EPS)
    return (y * g.astype(jnp.float32)).astype(x.dtype)


def layernorm(x, g, b):
    xf = x.astype(jnp.float32)
    mu = jnp.mean(xf, axis=-1, keepdims=True)
    var = jnp.mean(jnp.square(xf - mu), axis=-1, keepdims=True)
    y = (xf - mu) * lax.rsqrt(var + NORM_EPS)
    return (y * g.astype(jnp.float32) + b.astype(jnp.float32)).astype(x.dtype)


def modulate(x, shift, scale):
    return x * (1 + scale[:, None, :]) + shift[:, None, :]


def swiglu(x, w_gu, w_down):
    g, u = jnp.split(x @ w_gu, 2, axis=-1)
    return (jax.nn.silu(g) * u) @ w_down


def rope(x, pos):
    half = x.shape[-1] // 2
    inv_freq = ROPE_THETA ** (-jnp.arange(half, dtype=jnp.float32) / half)
    ang = pos[:, None] * inv_freq[None, :]
    cos = jnp.cos(ang)[None, :, None, :]
    sin = jnp.sin(ang)[None, :, None, :]
    xf = x.astype(jnp.float32)
    x1, x2 = xf[..., :half], xf[..., half:]
    return jnp.concatenate([x1 * cos - x2 * sin, x2 * cos + x1 * sin], axis=-1).astype(x.dtype)


def moba_attention(q, k, v):
    B, S, H, dh = q.shape
    n_blk = -(-S // MOBA_BLOCK)
    s_pad = n_blk * MOBA_BLOCK
    k_sel = min(MOBA_TOPK, n_blk)
    pad = ((0, 0), (0, s_pad - S), (0, 0), (0, 0))
    q, k, v = (jnp.pad(t, pad).transpose(0, 2, 1, 3) for t in (q, k, v))
    kb = k.reshape(B, H, n_blk, MOBA_BLOCK, dh)
    vb = v.reshape(B, H, n_blk, MOBA_BLOCK, dh)
    k_mean = jnp.mean(kb.astype(jnp.float32), axis=3)
    n_chunks = s_pad // Q_CHUNK
    q_chunks = q.reshape(B, H, n_chunks, Q_CHUNK, dh).transpose(2, 0, 1, 3, 4)
    scale = dh ** -0.5
    b_idx = jnp.arange(B)[:, None, None, None]
    h_idx = jnp.arange(H)[None, :, None, None]
    blk_ids = jnp.arange(n_blk)

    def one_chunk(args):
        qc, ci = args
        q_pos = ci * Q_CHUNK + jnp.arange(Q_CHUNK)
        own = q_pos[0] // MOBA_BLOCK
        gate = jnp.einsum('bhqd,bhnd->bhqn', qc.astype(jnp.float32), k_mean)
        gate = jnp.where(blk_ids < own, gate, NEG_INF)
        _, top_idx = lax.top_k(gate, k_sel)
        sel_valid = jnp.arange(k_sel) < own
        own_idx = jnp.broadcast_to(own, top_idx.shape[:-1] + (1,)).astype(top_idx.dtype)
        idx = jnp.concatenate([top_idx, own_idx], axis=-1)
        kg = kb[b_idx, h_idx, idx]
        vg = vb[b_idx, h_idx, idx]
        s = jnp.einsum('bhqd,bhqnkd->bhqnk', qc, kg).astype(jnp.float32) * scale
        key_pos = own * MOBA_BLOCK + jnp.arange(MOBA_BLOCK)
        own_mask = key_pos[None, :] <= q_pos[:, None]
        sel_mask = jnp.broadcast_to(sel_valid[None, :, None], (Q_CHUNK, k_sel, MOBA_BLOCK))
        mask = jnp.concatenate([sel_mask, own_mask[:, None, :]], axis=1)
        s = jnp.where(mask, s, NEG_INF)
        p = jax.nn.softmax(s.reshape(B, H, Q_CHUNK, -1), axis=-1).reshape(s.shape)
        return jnp.einsum('bhqnk,bhqnkd->bhqd', p.astype(vg.dtype), vg)

    out = lax.map(one_chunk, (q_chunks, jnp.arange(n_chunks)))
    out = out.transpose(1, 0, 3, 2, 4).reshape(B, s_pad, H, dh)[:, :S]
    return out.reshape(B, S, H * dh)


def gmlp_spatial_gating(u, v, ln_g, ln_b, w_s, b_s):
    B, S, _ = u.shape
    v = layernorm(v, ln_g, ln_b)
    n_c = S // GMLP_CHUNK
    causal = jnp.tril(jnp.ones((GMLP_CHUNK, GMLP_CHUNK), dtype=bool))
    w = jnp.where(causal[None], w_s, 0)
    vc = v.reshape(B, n_c, GMLP_CHUNK, N_GMLP_HEADS, GMLP_HEAD_DIM)
    mixed = jnp.einsum('hts,bnshd->bnthd', w, vc) + b_s.T[None, None, :, :, None]
    return u * mixed.reshape(B, S, GMLP_WIDTH)


def setup_inputs(seed: int = 0) -> dict:
    key = jax.random.key(seed)
    ks = jax.random.split(key, 20)
    f32 = jnp.float32
    nrm = lambda k, shape, s: jax.random.normal(k, shape, f32) * s
    L, D = DEPTH, D_MODEL
    return {
        "x": nrm(ks[0], (BATCH, SEQ, D), 1.0),
        "c": nrm(ks[1], (BATCH, D), 1.0),
        "w_ada": nrm(ks[2], (L, D, N_MOD * D), 0.5 * D ** -0.5),
        "b_ada": nrm(ks[3], (L, N_MOD * D), 0.01),
        "norm_ffn1": 1.0 + nrm(ks[4], (L, D), 0.01),
        "w_ffn1_gu": nrm(ks[5], (L, D, 2 * D_FF), D ** -0.5),
        "w_ffn1_down": nrm(ks[6], (L, D_FF, D), D_FF ** -0.5),
        "norm_mix": 1.0 + nrm(ks[7], (L, D), 0.01),
        "w_in": nrm(ks[8], (L, D, IN_PROJ_WIDTH), D ** -0.5),
        "gmlp_ln_g": 1.0 + nrm(ks[9], (L, GMLP_WIDTH), 0.01),
        "gmlp_ln_b": nrm(ks[10], (L, GMLP_WIDTH), 0.01),
        "gmlp_w_s": nrm(ks[11], (L, N_GMLP_HEADS, GMLP_CHUNK, GMLP_CHUNK), GMLP_CHUNK ** -0.5),
        "gmlp_b_s": 1.0 + nrm(ks[12], (L, N_GMLP_HEADS, GMLP_CHUNK), 0.01),
        "g_attn_out": 1.0 + nrm(ks[13], (L, ATTN_WIDTH), 0.01),
        "g_gmlp_out": 1.0 + nrm(ks[14], (L, GMLP_WIDTH), 0.01),
        "w_out": nrm(ks[15], (L, MIX_WIDTH, D), MIX_WIDTH ** -0.5),
        "norm_ffn2": 1.0 + nrm(ks[16], (L, D), 0.01),
        "w_ffn2_gu": nrm(ks[17], (L, D, 2 * D_FF), D ** -0.5),
        "w_ffn2_down": nrm(ks[18], (L, D_FF, D), D_FF ** -0.5),
        "norm_final": 1.0 + nrm(ks[19], (D,), 0.01),
    }


def reference(x, c, w_ada, b_ada, norm_ffn1, w_ffn1_gu, w_ffn1_down, norm_mix, w_in,
              gmlp_ln_g, gmlp_ln_b, gmlp_w_s, gmlp_b_s, g_attn_out, g_gmlp_out, w_out,
              norm_ffn2, w_ffn2_gu, w_ffn2_down, norm_final):
    B, S, D = x.shape
    pos = jnp.arange(S, dtype=jnp.float32)
    c_act = jax.nn.silu(c)
    h = x
    for l in range(DEPTH):
        mod = c_act @ w_ada[l] + b_ada[l]
        (sh1, sc1, gt1, sh2, sc2, gt2, sh3, sc3, gt3) = jnp.split(mod, N_MOD, axis=-1)

        y = modulate(rmsnorm(h, norm_ffn1[l]), sh1, sc1)
        h = h + 0.5 * gt1[:, None, :] * swiglu(y, w_ffn1_gu[l], w_ffn1_down[l])

        y = modulate(rmsnorm(h, norm_mix[l]), sh2, sc2)
        proj = y @ w_in[l]
        q, k, v, gu, gv = jnp.split(
            proj, np.cumsum([ATTN_WIDTH, ATTN_WIDTH, ATTN_WIDTH, GMLP_WIDTH]).tolist(), axis=-1)
        q = rope(q.reshape(B, S, N_ATTN_HEADS, HEAD_DIM), pos)
        k = rope(k.reshape(B, S, N_ATTN_HEADS, HEAD_DIM), pos)
        v = v.reshape(B, S, N_ATTN_HEADS, HEAD_DIM)
        attn_out = moba_attention(q, k, v)
        gmlp_out = gmlp_spatial_gating(jax.nn.gelu(gu), jax.nn.gelu(gv), gmlp_ln_g[l],
                                       gmlp_ln_b[l], gmlp_w_s[l], gmlp_b_s[l])
        merged = jnp.concatenate([rmsnorm(attn_out, g_attn_out[l]),
                                  rmsnorm(gmlp_out, g_gmlp_out[l])], axis=-1)
        h = h + gt2[:, None, :] * (merged @ w_out[l])

        y = modulate(rmsnorm(h, norm_ffn2[l]), sh3, sc3)
        h = h + 0.5 * gt3[:, None, :] * swiglu(y, w_ffn2_gu[l], w_ffn2_down[l])
    return rmsnorm(h, norm_final)
```

```python
import contextlib
import os
import numpy as np
import concourse.bass as bass
import concourse.mybir as mybir
from concourse.bass_utils import run_bass_kernel_spmd

F32 = mybir.dt.float32
BF16 = mybir.dt.bfloat16
AF = mybir.ActivationFunctionType
ALU = mybir.AluOpType
AX = mybir.AxisListType

ENGS = ("pe", "act", "dve", "pool", "sp")
SELF_SYNC = {"pe": False, "act": True, "dve": True, "pool": True, "sp": False}


class Res:
    __slots__ = ("name", "w", "r", "dsem")

    def __init__(self, name):
        self.name = name
        self.w = {}
        self.r = {}
        self.dsem = None


class KB:
    def __init__(self, nc):
        self.nc = nc
        self.q = {e: [] for e in ENGS}
        self.cnt = {e: 0 for e in ENGS}
        self.seen = {e: {} for e in ENGS}
        self.pend = {e: ([], []) for e in ENGS}
        self.dcnt = {}
        self.sems = {}
        self.nd = 0

    def _need(self, eng, waits, sem, c):
        if sem == eng and not SELF_SYNC[eng]:
            return
        if self.seen[eng].get(sem, 0) >= c:
            return
        if waits.get(sem, 0) < c:
            waits[sem] = c

    def _deps(self, eng, reads, writes):
        waits = {}
        for r in reads:
            for s, c in r.w.items():
                self._need(eng, waits, s, c)
        for w in writes:
            for s, c in w.w.items():
                self._need(eng, waits, s, c)
            for s, c in w.r.items():
                self._need(eng, waits, s, c)
        for s, c in waits.items():
            self.q[eng].append(("wait", s, c))
            self.seen[eng][s] = c

    def _mark(self, ev, reads, writes):
        s, c = ev
        for r in reads:
            if r.r.get(s, 0) < c:
                r.r[s] = c
        for w in writes:
            w.w = {s: c}
            w.r = {}

    def op(self, eng, fn, reads=(), writes=(), inc=True):
        for r in list(reads) + list(writes):
            for e2 in ENGS:
                if e2 != eng and (r in self.pend[e2][1]):
                    raise RuntimeError("resource %s pending on %s" % (r.name, e2))
        for w in writes:
            for e2 in ENGS:
                if e2 != eng and (w in self.pend[e2][0]):
                    raise RuntimeError("resource %s pending-read on %s" % (w.name, e2))
        self._deps(eng, reads, writes)
        self.q[eng].append(("op", fn, inc))
        if inc:
            self.cnt[eng] += 1
            ev = (eng, self.cnt[eng])
            pr, pw = self.pend[eng]
            self._mark(ev, pr, pw)
            self.pend[eng] = ([], [])
            self._mark(ev, reads, writes)
        else:
            self.pend[eng][0].extend(reads)
            self.pend[eng][1].extend(writes)

    def dma(self, eng, out, in_, reads=(), writes=(), sem=None, **kw):
        if sem is None:
            sem = (list(writes) + list(reads))[0]
        if isinstance(sem, Res):
            if sem.dsem is None:
                sem.dsem = "d%d_%s" % (self.nd, sem.name)
                self.nd += 1
            sem = sem.dsem
        self._deps(eng, reads, writes)
        self.dcnt[sem] = self.dcnt.get(sem, 0) + 16
        self.q[eng].append(("dma", out, in_, sem, kw))
        self._mark((sem, self.dcnt[sem]), reads, writes)

    def barrier(self, skip_prefix=None):
        for e in ENGS:
            assert not self.pend[e][0] and not self.pend[e][1], e
        allev = [(e, self.cnt[e]) for e in ENGS if self.cnt[e] > 0]
        allev += [(k_, v_) for k_, v_ in self.dcnt.items() if not (skip_prefix and k_.startswith(skip_prefix))]
        for e in ENGS:
            for s, c in allev:
                if s == e or c == 0:
                    continue
                if self.seen[e].get(s, 0) < c:
                    self.q[e].append(("wait", s, c))
                    self.seen[e][s] = c

    def emit(self):
        nc = self.nc
        names = list(ENGS) + list(self.dcnt.keys())
        import contextlib
        with contextlib.ExitStack() as es:
            for n in names:
                self.sems[n] = es.enter_context(nc.semaphore("s_" + n))
            block = es.enter_context(nc.Block())

            def run(e, eng):
                for it in self.q[e]:
                    if it[0] == "wait":
                        eng.wait_ge(self.sems[it[1]], it[2])
                    elif it[0] == "op":
                        ins = it[1](eng)
                        if it[2]:
                            ins.then_inc(self.sems[e], 1)
                    else:
                        _, out, in_, sem, kw = it
                        eng.dma_start(out=out, in_=in_, **kw).then_inc(self.sems[sem], 16)

            @block.tensor
            def _(eng):
                run("pe", eng)

            @block.scalar
            def _(eng):
                run("act", eng)

            @block.vector
            def _(eng):
                run("dve", eng)

            @block.gpsimd
            def _(eng):
                run("pool", eng)

            @block.sync
            def _(eng):
                run("sp", eng)


D = 1024
DFF = 2816
NJ = DFF // 128
H = 8
DH = 64
T = 512
BLK = 256
EPS = 1e-6
BIG = 30000.0
CW = 53000


class Arena:
    def __init__(self, ap):
        self.ap = ap
        self.off = 0

    def alloc(self, shape, dt):
        n = 1
        for s in shape[1:]:
            n *= s
        nf = n if dt == F32 else (n + 1) // 2
        a = self.ap[:, self.off:self.off + nf]
        self.off += nf
        assert self.off <= CW, ("arena overflow", self.off)
        v = a if dt == F32 else a.bitcast(BF16)[:, 0:n]
        if len(shape) == 3:
            v = v.rearrange("p (a b) -> p a b", a=shape[1])
        elif len(shape) == 4:
            v = v.rearrange("p (a b c) -> p a b c", a=shape[1], b=shape[2])
        return v


def build(S, debug=False, stop_after=9):
    NT = S // T
    NP = NT // 2
    SO = S // 2
    NKT = S // 128
    NB = S // BLK
    nc = bass.Bass("TRN2", target_bir_lowering=False)

    def din(name, shape, dt=F32):
        return nc.dram_tensor(name, list(shape), dt, kind="ExternalInput").ap()

    def dscr(name, shape, dt, dbg=False):
        kind = "ExternalOutput" if (debug and dbg) else "Internal"
        return nc.dram_tensor(name, list(shape), dt, kind=kind).ap()

    x_d = din("x", [2, SO, D])
    vecs_d = din("vecs", [120, 128])
    bgate_d = din("bgate", [3, D])
    nfin_d = din("nfin", [1, D])
    lng_d = din("lng", [1, 512])
    lnb_d = din("lnb", [1, 512])
    ws_d = din("w_s", [8, 128, 128])
    wada_d = din("w_ada", [D, 9 * D])
    wgu_d = [din("w_gu1", [D, 2 * DFF]), din("w_gu2", [D, 2 * DFF])]
    wdn_d = [din("w_dn1", [DFF, D]), din("w_dn2", [DFF, D])]
    win_d = din("w_in", [D, 2560])
    wout_d = din("w_out", [D, D])
    ident_d = din("ident", [128, 128])
    cos_d = din("cosT", [2, SO, 32])
    sin_d = din("sinT", [2, SO, 32])
    kind_d = din("kind", [32, S])
    cm_d = din("cmask", [128, 2, 256])
    patt_d = din("patt", [1, (SO // 128) * 32])
    tril_d = din("tril", [128, 128])
    out_d = nc.dram_tensor("out", [SO, D], F32, kind="ExternalOutput").ap()

    wgu_b = [dscr("wgu1_b", [D, 2 * DFF], BF16), dscr("wgu2_b", [D, 2 * DFF], BF16)]
    wdn_b = [dscr("wdn1_b", [DFF, D], BF16), dscr("wdn2_b", [DFF, D], BF16)]
    win_b = dscr("win_b", [D, 2560], BF16)
    h1_scr = dscr("h1_scr", [SO, D], F32, True)
    q_scr = dscr("q_scr", [4, 128, SO], F32, True)
    k_scr = dscr("k_scr", [4, 128, S], BF16)
    v_scr = dscr("v_scr", [8, 128, NKT, 65], BF16)
    gmT_scr = dscr("gmT_scr", [4, 128, SO], BF16)
    km_scr = dscr("km_scr", [4, 128, NB], F32, True)
    attn_scr = dscr("attn_scr", [SO, 512], F32, True)

    es = contextlib.ExitStack()
    with es:
        arena_t = es.enter_context(nc.sbuf_tensor("arena", [128, CW], F32))
        AR = Arena(arena_t[:, :])
        PGU = [es.enter_context(nc.psum_tensor("pgu%d" % i, [128, 512], F32))[:, :] for i in range(4)]
        PO = [es.enter_context(nc.psum_tensor("po%d" % i, [128, 512], F32))[:, :] for i in range(2)]
        PT = [es.enter_context(nc.psum_tensor("pt%d" % i, [128, 1024], BF16))[:, :] for i in range(2)]
        R_PGU = [Res("pgu%d" % i) for i in range(4)]
        R_PO = [Res("po%d" % i) for i in range(2)]
        R_PT = []
        for i in range(2):
            r_ = Res("pt%d" % i)
            R_PT.append([r_, r_])
        kb = KB(nc)

        def MM(out, lhsT, rhs, st, sp, R=(), W=(), inc=True):
            kb.op("pe", lambda e: e.matmul(out, lhsT=lhsT, rhs=rhs, start=st, stop=sp), R, W, inc)

        def TR(out, in_, ident, R=(), W=(), inc=True):
            kb.op("pe", lambda e: e.transpose(out=out, in_=in_, identity=ident), R, W, inc)

        def ACT(out, in_, func, R=(), W=(), **kw):
            kb.op("act", lambda e: e.activation(out=out, in_=in_, func=func, **kw), R, W)

        def CPA(out, in_, R=(), W=()):
            kb.op("act", lambda e: e.copy(out=out, in_=in_), R, W)

        def V(eng, name, R=(), W=(), **kw):
            if eng == "pool":
                eng = "dve"
            kb.op(eng, lambda e: getattr(e, name)(**kw), R, W)

        po_i = [0]

        def next_po():
            i = po_i[0] % 2
            po_i[0] += 1
            return PO[i], R_PO[i]

        def A(shape, dt, name):
            return AR.alloc(shape, dt), Res(name)

        idf, R_idf = A([128, 128], F32, "idf")
        idb, R_idb = A([128, 128], BF16, "idb")
        vT, R_vT = A([128, 120], F32, "vT")
        modT, R_modT = A([128, 72], F32, "modT")
        Asc, R_Asc = A([128, 3, 8], F32, "Asc")
        gt_bc, R_gt = A([128, 3, 1024], F32, "gt_bc")
        lng_bc, R_lng = A([128, 512], F32, "lng")
        lnb_bc, R_lnb = A([128, 512], F32, "lnb")
        WsT, R_WsT = A([128, 8, 128], BF16, "WsT")
        patt, R_patt = A([128, SO // 128, 32], F32, "patt")
        cm, R_cm = A([128, 2, 256], BF16, "cm")
        onesc, R_onesc = A([128, 2], F32, "onesc")
        kms, R_kms = A([128, 4, NKT], F32, "kms")
        small, R_small = A([128, 64], F32, "small")
        MARK_P = AR.off
        hx, R_hx = A([128, 4, 1024], F32, "hx")
        tb, R_tb = A([128, 4, 1024], BF16, "tb")
        yT, R_yT = A([128, 8, 512], BF16, "yT")
        aT, R_aT = A([128, NJ, 512], BF16, "aT")
        NWG = 2
        wg = [A([128, 8, 2, 256], BF16, "wg%d" % i) for i in range(NWG)]
        wdn, R_wdn = A([128, NJ, 1024], BF16, "wdn")
        sg = [A([128, 512], F32, "sg%d" % i) for i in range(2)]
        tmpo = [A([128, 512], F32, "tmpo%d" % i) for i in range(2)]
        ss, R_ss = A([128, 16], F32, "ss")
        nsm, _ = A([128, 32], F32, "nsm")
        R_nsq = [Res("nsq%d" % i) for i in range(4)]
        junk, R_junk = A([128, 1024], BF16, "junk")
        MARK0 = AR.off
        MARK1 = MARK0

        kb.dma("sp", idf, ident_d, writes=[R_idf])
        V("dve", "tensor_copy", [R_idf], [R_idb], out=idb, in_=idf)
        R_wgub = [Res("wgu1b"), Res("wgu2b")]
        R_wdnb = [Res("wdn1b"), Res("wdn2b")]
        R_winb = Res("winb")

        def cast_w(dst, src, rows, step, R, sem):
            for r0 in range(0, rows, step):
                kb.dma("pool", dst[r0:r0 + step, :], src[r0:r0 + step, :], writes=[R], sem=sem)

        cast_w(wgu_b[0], wgu_d[0], D, 128, R_wgub[0], "pre_gu1")
        kb.dma("pool", cm, cm_d, writes=[R_cm])

        vecs_sb, R_vecs = A([128, 128], F32, "vecs")
        kb.dma("sp", vecs_sb[0:120, :], vecs_d, writes=[R_vecs])
        kb.dma("sp", lng_bc, lng_d.partition_broadcast(128), writes=[R_lng])
        kb.dma("sp", lnb_bc, lnb_d.partition_broadcast(128), writes=[R_lnb])
        kb.dma("sp", patt.rearrange("p a b -> p (a b)"), patt_d.partition_broadcast(128), writes=[R_patt])
        bg_bc, R_bg = A([128, 3, 1024], F32, "bg_bc")
        for gi in range(3):
            kb.dma("sp", bg_bc[:, gi, :], bgate_d[gi:gi + 1, :].partition_broadcast(128), writes=[R_bg])
        V("dve", "memset", [], [R_onesc], ap=onesc, constant=1.0 / BLK)
        TR(PO[0][:, 0:120], vecs_sb[0:120, :], idf[0:120, 0:120], [R_vecs, R_idf], [R_PO[0]])
        CPA(vT, PO[0][:, 0:120], [R_PO[0]], [R_vT])
        cact, R_cact = A([128, 8], F32, "cact")
        cT, R_cT = A([128, 8], BF16, "cT")
        crep, R_crep = A([128, 8, 128], BF16, "crep")
        ACT(cact, vT[:, 96:104], AF.Silu, [R_vT], [R_cact])
        V("dve", "tensor_copy", [R_cact], [R_cT], out=cT, in_=cact)
        for kc in range(8):
            V("dve", "tensor_copy", [R_cT], [R_crep], out=crep[:, kc, :],
              in_=cT[:, kc:kc + 1].to_broadcast([128, 128]))
        for gi in range(3):
            V("dve", "tensor_scalar", [R_bg], [R_bg], out=bg_bc[:, gi, :], in0=bg_bc[:, gi, :],
              scalar1=(1.0 if gi == 1 else 0.5), scalar2=None, op0=ALU.mult)
        wa = [A([128, 8, 512], BF16, "wa%d" % i) for i in range(4)]
        wada_v = wada_d.rearrange("(kc p) n -> p kc n", p=128)
        PMOD, R_PMOD = PGU[0], R_PGU[0]
        for cg in range(18):
            wab, R_wab = wa[cg % 4]
            for kh in range(2):
                kb.dma("pool", wab[:, 4 * kh:4 * kh + 4, :], wada_v[:, 4 * kh:4 * kh + 4, cg * 512:(cg + 1) * 512],
                       writes=[R_wab])
            for cc in range(4):
                col = cg * 4 + cc
                for kc in range(8):
                    MM(PMOD[:, col:col + 1], wab[:, kc, cc * 128:(cc + 1) * 128], cT[:, kc:kc + 1],
                       kc == 0, kc == 7, [R_wab, R_cT], [R_PMOD], inc=(kc == 7))
            v = cg // 2
            if v in (2, 5, 8):
                gi = (v - 2) // 3
                half = cg % 2
                ps, R_ps = PGU[2 + half], R_PGU[2 + half]
                for kc in range(8):
                    MM(ps, crep[:, kc, :], wab[:, kc, :], kc == 0, kc == 7, [R_wab, R_crep], [R_ps], inc=(kc == 7))
                V("dve", "scalar_tensor_tensor", [R_ps, R_bg], [R_gt], out=gt_bc[:, gi, half * 512:(half + 1) * 512],
                  in0=ps, scalar=(1.0 if gi == 1 else 0.5), in1=bg_bc[:, gi, half * 512:(half + 1) * 512],
                  op0=ALU.mult, op1=ALU.add)
        cast_w(wdn_b[0], wdn_d[0], DFF, 256, R_wdnb[0], "pre_dn1")
        cast_w(win_b, win_d, D, 256, R_winb, "pre_in")
        cast_w(wgu_b[1], wgu_d[1], D, 128, R_wgub[1], "pre_gu2")
        cast_w(wdn_b[1], wdn_d[1], DFF, 256, R_wdnb[1], "pre_dn2")
        V("dve", "tensor_tensor", [R_PMOD, R_vT], [R_modT], out=modT, in0=PMOD[:, 0:72], in1=vT[:, 0:72], op=ALU.add)
        for k in range(3):
            V("dve", "scalar_tensor_tensor", [R_modT, R_vT], [R_Asc], out=Asc[:, k, :],
              in0=modT[:, (3 * k + 1) * 8:(3 * k + 1) * 8 + 8], scalar=1.0, in1=vT[:, 72 + 8 * k:80 + 8 * k],
              op0=ALU.add, op1=ALU.mult)
        ws32, R_ws32 = A([128, 8, 128], F32, "ws32")
        trilm, R_tril = A([128, 128], F32, "tril")
        wsm, R_wsm = A([128, 8, 128], BF16, "wsm")
        kb.dma("sp", ws32, ws_d.rearrange("h t s -> t h s"), writes=[R_ws32])
        kb.dma("sp", trilm, tril_d, writes=[R_tril])
        for h in range(8):
            V("dve", "tensor_tensor", [R_ws32, R_tril], [R_wsm], out=wsm[:, h, :], in0=ws32[:, h, :], in1=trilm, op=ALU.mult)
        for h in range(8):
            TR(PT[0][:, h * 128:(h + 1) * 128], wsm[:, h, :], idb, [R_wsm, R_idb], [R_PT[0][0], R_PT[0][1]], inc=(h == 7))
        V("dve", "tensor_copy", [R_PT[0][0], R_PT[0][1]], [R_WsT], out=WsT.rearrange("p h t -> p (h t)"), in_=PT[0][:, 0:1024])
        kb.barrier()
        AR.off = MARK0
        if stop_after == 0:
            kb.emit()
            return nc

        def rstd_of(ss_ap, n, nfeat, R_in):
            V("dve", "tensor_scalar", [R_in], [R_small], out=small[:, 16:16 + n], in0=ss_ap, scalar1=1.0 / nfeat,
              scalar2=EPS, op0=ALU.mult, op1=ALU.add)
            ACT(small[:, 32:32 + n], small[:, 16:16 + n], AF.Sqrt, [R_small], [R_small])
            V("dve", "reciprocal", [R_small], [R_small], out=small[:, 0:n], in_=small[:, 32:32 + n])
            return small[:, 0:n]

        def norm_T(k):
            norm_stats()
            norm_tr(k)

        def norm_stats():
            V("dve", "memset", [], [R_ss], ap=ss[:, 0:4], constant=0.0)
            KD = 9
            R_sq = [R_nsq[i] for i in range(4)]

            def sq(sub):
                ACT(junk, hx[:, sub, :], AF.Square, [R_hx, R_ss], [R_junk, R_sq[sub]], accum_out=ss[:, sub:sub + 1])

            sq(0)
            for sub in range(4):
                if sub + 1 < 4:
                    sq(sub + 1)
                c0 = 16 + sub
                V("dve", "tensor_scalar", [R_sq[sub], R_ss], [R_sq[sub]], out=nsm[:, c0:c0 + 1], in0=ss[:, sub:sub + 1], scalar1=1.0 / D,
                  scalar2=EPS, op0=ALU.mult, op1=ALU.add)
                ACT(nsm[:, 8 + sub:9 + sub], nsm[:, c0:c0 + 1], AF.Sqrt, [R_sq[sub]], [R_sq[sub]])
                V("dve", "reciprocal", [R_sq[sub]], [R_sq[sub]], out=nsm[:, sub:sub + 1], in_=nsm[:, 8 + sub:9 + sub])
                V("dve", "tensor_scalar", [R_hx, R_sq[sub]], [R_tb], out=tb[:, sub, :], in0=hx[:, sub, :],
                  scalar1=nsm[:, sub:sub + 1], scalar2=None, op0=ALU.mult)

        def norm_tr(k):
            KD = 9
            for c in range(8):
                b, hf = c % 2, 0
                for sub in range(4):
                    TR(PT[b][:, hf * 512 + sub * 128: hf * 512 + (sub + 1) * 128], tb[:, sub, c * 128:(c + 1) * 128], idb,
                       [R_tb, R_idb], [R_PT[b][hf]], inc=(sub == 3))
                if KD == 3:
                    continue
                ACT(yT[:, c, :], PT[b][:, hf * 512:(hf + 1) * 512], AF.Identity, [R_PT[b][hf], R_Asc, R_modT], [R_yT],
                    scale=Asc[:, k, c:c + 1], bias=modT[:, 3 * k * 8 + c:3 * k * 8 + c + 1])

        def wg_load(f, jp):
            wgu_v = wgu_b[f].rearrange("(kc p) (two n) -> p kc two n", p=128, two=2)
            wgb, R_wgb = wg[jp % NWG]
            for two in range(2):
                kb.dma("sp", wgb[:, :, two, :], wgu_v[:, :, two, jp * 256:(jp + 1) * 256], reads=[R_wgub[f]], writes=[R_wgb])

        def ffn_pre(f):
            for jp in range(NWG):
                wg_load(f, jp)

        def wdn_load(f):
            wdn_v = wdn_b[f].rearrange("(j p) d -> p j d", p=128)
            for jh in range(2):
                kb.dma("sp", wdn[:, 11 * jh:11 * jh + 11, :], wdn_v[:, 11 * jh:11 * jh + 11, :], reads=[R_wdnb[f]], writes=[R_wdn])

        def ffn(k, f):
            for jp in range(NJ // 2):
                wgb, R_wgb = wg[jp % NWG]
                if jp >= NWG:
                    wg_load(f, jp)
                for jl in range(2):
                    jj = 2 * jp + jl
                    s = jj % 2
                    pg, R_pg = PGU[2 * s], R_PGU[2 * s]
                    pu, R_pu = PGU[2 * s + 1], R_PGU[2 * s + 1]
                    for kc in range(8):
                        MM(pg, wgb[:, kc, 0, jl * 128:(jl + 1) * 128], yT[:, kc, :], kc == 0, kc == 7, [R_wgb, R_yT], [R_pg], inc=(kc == 7))
                    for kc in range(8):
                        MM(pu, wgb[:, kc, 1, jl * 128:(jl + 1) * 128], yT[:, kc, :], kc == 0, kc == 7, [R_wgb, R_yT], [R_pu], inc=(kc == 7))
                    sgb, R_sgb = sg[s]
                    ACT(sgb, pg, AF.Silu, [R_pg], [R_sgb])
                    V("dve", "tensor_tensor", [R_sgb, R_pu], [R_aT], out=aT[:, jj, :], in0=sgb, in1=pu, op=ALU.mult)
            for sub in range(4):
                for half in range(2):
                    ps, R_ps = next_po()
                    for jj in range(NJ):
                        MM(ps, aT[:, jj, sub * 128:(sub + 1) * 128], wdn[:, jj, half * 512:(half + 1) * 512], jj == 0, jj == NJ - 1,
                           [R_aT, R_wdn], [R_ps], inc=(jj == NJ - 1))
                    tm, R_tm = tmpo[(sub * 2 + half) % 2]
                    V("dve", "tensor_tensor", [R_ps, R_gt], [R_tm], out=tm, in0=ps, in1=gt_bc[:, k, half * 512:(half + 1) * 512], op=ALU.mult)
                    V("pool", "tensor_tensor", [R_tm, R_hx], [R_hx], out=hx[:, sub, half * 512:(half + 1) * 512],
                      in0=hx[:, sub, half * 512:(half + 1) * 512], in1=tm, op=ALU.add)

        wi = [A([128, 8, 512], BF16, "wi%d" % i) for i in range(2)]
        win_v = win_b.rearrange("(kc p) n -> p kc n", p=128)
        cosb, R_cos = A([128, 4, 32], F32, "cosb")
        sinb, R_sin = A([128, 4, 32], F32, "sinb")
        qr, R_qr = A([128, 8, 2, 32], F32, "qr")
        rtmp, R_rtmp = A([128, 8, 32], F32, "rtmp")
        k16, R_k16 = A([128, 512], BF16, "k16")
        qT_st, R_qTst = A([128, 4, 512], F32, "qTst")
        kT_st, R_kTst = A([128, 4, 512], BF16, "kTst")
        v_st, R_vst = A([128, 4, 8, 65], BF16, "vst")
        V("dve", "memset", [], [R_vst], ap=v_st, constant=1.0)
        gmT_st, R_gmTst = A([128, 4, 512], BF16, "gmTst")
        gug, R_gug = A([128, 4, 512], F32, "gug")
        gvg, R_gvg = A([128, 512], F32, "gvg")
        vn, R_vn = A([128, 512], BF16, "vn")
        gm, R_gm = A([128, 512], F32, "gm")
        gmn, R_gmn = A([128, 512], BF16, "gmn")
        bsT = vT[:, 112:120]
        wi_i = [0]

        def rope(ps, dst, R_ps, R_dst, sub):
            p4 = ps.rearrange("p (h t d) -> p h t d", h=8, t=2)
            cb = cosb[:, sub:sub + 1, :].to_broadcast([128, 8, 32])
            sb_ = sinb[:, sub:sub + 1, :].to_broadcast([128, 8, 32])
            V("dve", "tensor_tensor", [R_ps, R_cos], [R_dst], out=dst[:, :, 0, :], in0=p4[:, :, 0, :], in1=cb, op=ALU.mult)
            V("dve", "tensor_tensor", [R_ps, R_sin], [R_rtmp], out=rtmp, in0=p4[:, :, 1, :], in1=sb_, op=ALU.mult)
            V("dve", "tensor_tensor", [R_dst, R_rtmp], [R_dst], out=dst[:, :, 0, :], in0=dst[:, :, 0, :], in1=rtmp, op=ALU.subtract)
            V("dve", "tensor_tensor", [R_ps, R_cos], [R_dst], out=dst[:, :, 1, :], in0=p4[:, :, 1, :], in1=cb, op=ALU.mult)
            V("dve", "tensor_tensor", [R_ps, R_sin], [R_rtmp], out=rtmp, in0=p4[:, :, 0, :], in1=sb_, op=ALU.mult)
            V("dve", "tensor_tensor", [R_dst, R_rtmp], [R_dst], out=dst[:, :, 1, :], in0=dst[:, :, 1, :], in1=rtmp, op=ALU.add)

        ALLPO = [(PO[0], R_PO[0]), (PO[1], R_PO[1])] + [(PGU[i], R_PGU[i]) for i in range(4)]
        po6 = [0]

        def next_po6():
            i = po6[0] % 6
            po6[0] += 1
            return ALLPO[i]

        def load_x(t):
            pr, which = t // 2, t % 2
            kb.dma("sp", hx, x_d[which, pr * T:(pr + 1) * T, :].rearrange("(s p) d -> p s d", p=128), writes=[R_hx])

        def load_cs(t):
            pr, which = t // 2, t % 2
            kb.dma("sp", cosb, cos_d[which, pr * T:(pr + 1) * T, :].rearrange("(s p) d -> p s d", p=128), writes=[R_cos])
            kb.dma("sp", sinb, sin_d[which, pr * T:(pr + 1) * T, :].rearrange("(s p) d -> p s d", p=128), writes=[R_sin])

        def stage_b(t, grp, sub, ps, R_ps):
            sl = slice(sub * 128, (sub + 1) * 128)
            st_i = t * 4 + sub
            if grp == 0:
                rope(ps, qr, R_ps, R_qr, sub)
                qf = qr.rearrange("p h t d -> p (h t d)")
                ps2, R_ps2 = next_po6()
                for hp in range(4):
                    TR(ps2[:, hp * 128:(hp + 1) * 128], qf[:, hp * 128:(hp + 1) * 128], idf, [R_qr, R_idf], [R_ps2], inc=(hp == 3))
                CPA(qT_st[:, :, sl], ps2.rearrange("p (h s) -> p h s", h=4), [R_ps2], [R_qTst])
            elif grp == 1:
                rope(ps, qr, R_ps, R_qr, sub)
                kf = qr.rearrange("p h t d -> p (h t d)")
                ps2, R_ps2 = next_po6()
                for hp in range(4):
                    MM(ps2[:, hp:hp + 1], kf[:, hp * 128:(hp + 1) * 128], onesc[:, 0:1], True, True, [R_qr, R_onesc], [R_ps2], inc=(hp == 3))
                V("dve", "tensor_copy", [R_ps2], [R_kms], out=kms[:, :, st_i], in_=ps2[:, 0:4])
                V("dve", "tensor_copy", [R_qr], [R_k16], out=k16, in_=kf)
                for hp in range(4):
                    TR(PT[0][:, hp * 128:(hp + 1) * 128], k16[:, hp * 128:(hp + 1) * 128], idb, [R_k16, R_idb], [R_PT[0][0]], inc=(hp == 3))
                CPA(kT_st[:, :, sl], PT[0][:, 0:512].rearrange("p (h s) -> p h s", h=4), [R_PT[0][0]], [R_kTst])
            elif grp == 2:
                CPA(v_st[:, sub, :, 0:64], ps.rearrange("p (h d) -> p h d", h=8), [R_ps], [R_vst])
            elif grp == 3:
                ACT(gug[:, sub, :], ps, AF.Gelu_apprx_tanh, [R_ps], [R_gug])
            else:
                V("dve", "memset", [], [R_ss], ap=ss[:, 4:8], constant=0.0)
                ACT(gvg, ps, AF.Gelu_apprx_tanh, [R_ps], [R_gvg, R_ss], accum_out=ss[:, 4:5])
                ACT(junk[:, 0:512], gvg, AF.Square, [R_gvg], [R_junk, R_ss], accum_out=ss[:, 5:6])
                V("dve", "tensor_scalar", [R_ss], [R_ss], out=ss[:, 8:9], in0=ss[:, 4:5], scalar1=1.0 / 512, scalar2=None, op0=ALU.mult)
                V("dve", "tensor_tensor", [R_ss], [R_ss], out=ss[:, 9:10], in0=ss[:, 8:9], in1=ss[:, 8:9], op=ALU.mult)
                V("dve", "scalar_tensor_tensor", [R_ss], [R_ss], out=ss[:, 10:11], in0=ss[:, 5:6], scalar=1.0 / 512, in1=ss[:, 9:10],
                  op0=ALU.mult, op1=ALU.subtract)
                V("dve", "tensor_scalar", [R_ss], [R_ss], out=ss[:, 11:12], in0=ss[:, 10:11], scalar1=EPS, scalar2=None, op0=ALU.add)
                ACT(ss[:, 12:13], ss[:, 11:12], AF.Sqrt, [R_ss], [R_ss])
                V("dve", "reciprocal", [R_ss], [R_ss], out=ss[:, 13:14], in_=ss[:, 12:13])
                V("dve", "scalar_tensor_tensor", [R_ss], [R_ss], out=ss[:, 14:15], in0=ss[:, 8:9], scalar=-1.0, in1=ss[:, 13:14],
                  op0=ALU.mult, op1=ALU.mult)
                ACT(gvg, gvg, AF.Identity, [R_gvg, R_ss], [R_gvg], scale=ss[:, 13:14], bias=ss[:, 14:15])
                V("dve", "tensor_tensor", [R_gvg, R_lng], [R_gvg], out=gvg, in0=gvg, in1=lng_bc, op=ALU.mult)
                V("dve", "tensor_tensor", [R_gvg, R_lnb], [R_vn], out=vn, in0=gvg, in1=lnb_bc, op=ALU.add)
                ps3, R_ps3 = next_po6()
                for hg in range(8):
                    MM(ps3[:, hg * 64:(hg + 1) * 64], WsT[:, hg, :], vn[:, hg * 64:(hg + 1) * 64], True, True, [R_WsT, R_vn], [R_ps3], inc=(hg == 7))
                V("dve", "tensor_tensor", [R_ps3, R_vT], [R_gm], out=gm.rearrange("p (h d) -> p h d", h=8),
                  in0=ps3.rearrange("p (h d) -> p h d", h=8), in1=bsT.unsqueeze(2).to_broadcast([128, 8, 64]), op=ALU.add)
                V("dve", "tensor_tensor", [R_gm, R_gug], [R_gm], out=gm, in0=gm, in1=gug[:, sub, :], op=ALU.mult)
                ACT(junk[:, 0:512], gm, AF.Square, [R_gm], [R_junk, R_ss], accum_out=ss[:, 6:7])
                rs = rstd_of(ss[:, 6:7], 1, 512, R_ss)
                V("dve", "tensor_scalar", [R_gm, R_small], [R_gmn], out=gmn, in0=gm, scalar1=rs[:, 0:1], scalar2=None, op0=ALU.mult)
                for fc in range(4):
                    TR(PT[1][:, fc * 128:(fc + 1) * 128], gmn[:, fc * 128:(fc + 1) * 128], idb, [R_gmn, R_idb], [R_PT[1][0]], inc=(fc == 3))
                CPA(gmT_st[:, :, sl], PT[1][:, 0:512].rearrange("p (h s) -> p h s", h=4), [R_PT[1][0]], [R_gmTst])

        load_x(0)
        load_cs(0)
        for t in range(NT):
            pr, which = t // 2, t % 2
            own_t = (which == 0)
            tok0 = t * T
            tsl = slice(tok0, tok0 + T)
            osl = slice(pr * T, (pr + 1) * T)
            ffn_pre(0)
            if t == 0:
                norm_stats()
            norm_tr(0)
            wdn_load(0)
            ffn(0, 0)
            if own_t:
                kb.dma("sp", h1_scr[osl, :].rearrange("(s p) d -> p s d", p=128), hx, reads=[R_hx])
            grps = list(range(5)) if own_t else [1, 2]

            def wi_load(grp_):
                wib_, R_wib_ = wi[wi_i[0] % 2]
                wi_i[0] += 1
                for kh in range(2):
                    kb.dma("sp", wib_[:, 4 * kh:4 * kh + 4, :], win_v[:, 4 * kh:4 * kh + 4, grp_ * 512:(grp_ + 1) * 512], reads=[R_winb], writes=[R_wib_])
                return wib_, R_wib_

            nxt_w = wi_load(grps[0])
            norm_T(1)
            if t + 1 < NT:
                load_x(t + 1)
            for gi_, grp in enumerate(grps):
                wib, R_wib = nxt_w
                if gi_ + 1 < len(grps):
                    nxt_w = wi_load(grps[gi_ + 1])
                if gi_ == len(grps) - 1 and t + 1 < NT:
                    norm_stats()
                pend = None
                for sub in range(4):
                    sl = slice(sub * 128, (sub + 1) * 128)
                    ps, R_ps = next_po6()
                    for kc in range(8):
                        MM(ps, yT[:, kc, sl], wib[:, kc, :], kc == 0, kc == 7, [R_yT, R_wib], [R_ps], inc=(kc == 7))
                    if pend is not None:
                        stage_b(t, grp, *pend)
                    pend = (sub, ps, R_ps)
                stage_b(t, grp, *pend)
                if grp == 0:
                    kb.dma("sp", q_scr.rearrange("h p s -> p h s")[:, :, osl], qT_st, reads=[R_qTst])
                elif grp == 1:
                    kb.dma("sp", k_scr.rearrange("h p s -> p h s")[:, :, tsl], kT_st, reads=[R_kTst])
                    if t + 1 < NT:
                        load_cs(t + 1)
                elif grp == 2:
                    for sub_ in range(4):
                        kb.dma("act", v_scr.rearrange("h p k d -> p k h d")[:, 4 * t + sub_, :, :], v_st[:, sub_, :, :], reads=[R_vst])
                elif grp == 4:
                    kb.dma("sp", gmT_scr.rearrange("h p s -> p h s")[:, :, osl], gmT_st, reads=[R_gmTst])
        kmv = kms.rearrange("p h (b two) -> p h b two", two=2)
        kmb, R_kmb = A([128, 4, NB], F32, "kmb")
        V("dve", "tensor_tensor", [R_kms], [R_kmb], out=kmb, in0=kmv[:, :, :, 0], in1=kmv[:, :, :, 1], op=ALU.add)
        kb.dma("sp", km_scr.rearrange("h p b -> p h b"), kmb, reads=[R_kmb])
        kb.barrier()
        AR.off = MARK1
        if stop_after == 1:
            kb.emit()
            return nc

        NG = NP
        AR.off = MARK_P
        Kaug = [A([128, S], BF16, "kaug%d" % i) for i in range(2)]
        Vaug = [A([128, NKT, 65], BF16, "vaug%d" % i) for i in range(2)]
        kmh = [A([128, NB], F32, "kmh%d" % i) for i in range(2)]
        Q32 = [A([128, 512], F32, "q32_%d" % i) for i in range(3)]
        Qaug = [A([128, 512], BF16, "qaug%d" % i) for i in range(2)]
        NPB = 3
        pT = [A([128, 512], BF16, "pT%d" % i) for i in range(NPB)]
        osb, R_osb = A([128, 512], F32, "osb")
        stg, R_stg = A([128, 4, 96], BF16, "stg")
        gsb, R_gsb = A([128, 32], F32, "gsb")
        mx8, R_mx8 = A([128, 8], F32, "mx8")
        mtmp, R_mtmp = A([128, 32], F32, "mtmp")
        rec, R_rec = A([128, 4], F32, "rec")
        attn_st = [A([128, 4, 64], F32, "attnst%d" % i) for i in range(2)]
        V("dve", "memset", [], [R_stg], ap=stg, constant=0.0)
        V("dve", "memset", [], [R_gsb], ap=gsb, constant=-1e30)
        for i in range(2):
            kb.dma("pool", Kaug[i][0][64:96, :], kind_d, writes=[Kaug[i][1]], sem="kind%d" % i)
        SB = [(PGU[0], R_PGU[0]), (PGU[1], R_PGU[1]), (PGU[2], R_PGU[2])]
        PM, R_PM = PGU[3], R_PGU[3]
        attn_v = attn_scr.rearrange("(s p) f -> p s f", p=128)

        def load_head(h):
            hp, r0 = h // 2, (h % 2) * 64
            hb = h % 2
            kb.dma("act", Kaug[hb][0][0:64, :], k_scr[hp, r0:r0 + 64, :], writes=[Kaug[hb][1]])
            kb.dma("act", Vaug[hb][0], v_scr[h], writes=[Vaug[hb][1]])
            kb.dma("act", kmh[hb][0][0:64, :], km_scr[hp, r0:r0 + 64, :], writes=[kmh[hb][1]])

        items = [(h, g) for h in range(H) for g in range(NG)]

        def q_load(i):
            h, g = items[i]
            hp, r0 = h // 2, (h % 2) * 64
            Q3, R_Q3 = Q32[i % 3]
            kb.dma("sp", Q3[0:64, :], q_scr[hp, r0:r0 + 64, g * 512:(g + 1) * 512], writes=[R_Q3])

        def S1(i):
            h, g = items[i]
            km, R_km = kmh[h % 2]
            Q3, R_Q3 = Q32[i % 3]
            Qa, R_Qa = Qaug[i % 2]
            V("dve", "tensor_copy", [R_Q3], [R_Qa], out=Qa[0:64, :], in_=Q3[0:64, :])
            for sub in range(4):
                MM(PM[:, sub * 32:sub * 32 + NB], Q3[0:64, sub * 128:(sub + 1) * 128], km[0:64, 0:NB], True, True, [R_Q3, R_km], [R_PM], inc=(sub == 3))
            for sub in range(4):
                V("dve", "tensor_tensor", [R_PM, R_patt], [R_gsb], out=gsb[:, 0:NB], in0=PM[:, sub * 32:sub * 32 + NB], in1=patt[:, 4 * g + sub, 0:NB], op=ALU.add)
                V("dve", "max", [R_gsb], [R_mx8], out=mx8, in_=gsb)
                V("dve", "tensor_scalar", [R_mx8], [R_mx8], out=mx8[:, 4:5], in0=mx8[:, 3:4], scalar1=-1e29, scalar2=None, op0=ALU.max)
                V("dve", "tensor_scalar", [R_gsb, R_mx8], [R_mtmp], out=mtmp, in0=gsb, scalar1=mx8[:, 4:5], scalar2=BIG,
                  op0=ALU.is_ge, op1=ALU.mult)
                V("dve", "tensor_scalar", [R_mtmp], [R_stg], out=stg[:, sub, 64:96], in0=mtmp, scalar1=-BIG, scalar2=None, op0=ALU.add)

        def S2(i):
            Qa, R_Qa = Qaug[i % 2]
            for sub in range(4):
                TR(PT[0][0:96, sub * 128:(sub + 1) * 128], stg[:, sub, :], idb, [R_stg, R_idb], [R_PT[0][0]], inc=(sub == 3))
            V("dve", "tensor_copy", [R_PT[0][0]], [R_Qa], out=Qa[64:96, :], in_=PT[0][64:96, 0:512])

        sbi = 0
        pbi = 0
        load_head(0)
        q_load(0)
        if len(items) > 1:
            q_load(1)
        S1(0)
        S2(0)
        for i, (h, g) in enumerate(items):
            hb = h % 2
            Ka, R_Ka = Kaug[hb]
            Va, R_Va = Vaug[hb]
            Qa, R_Qa = Qaug[i % 2]
            if g == 0 and h + 1 < H:
                load_head(h + 1)
            if i + 2 < len(items):
                q_load(i + 2)
            if i + 1 < len(items):
                S1(i + 1)
            nkt = min(NKT, 8 * (g + 1))
            oacc, R_oacc = PO[i % 2], R_PO[i % 2]
            LA = 2
            slots = {}
            for it_ in range(nkt + LA):
                if it_ < nkt:
                    kt = it_
                    sps, R_sps = SB[sbi % 3]
                    sbi += 1
                    slots[kt] = (sps, R_sps)
                    MM(sps, Ka[0:96, kt * 128:(kt + 1) * 128], Qa[0:96, :], True, True, [R_Ka, R_Qa], [R_sps])
                kt = it_ - LA
                if kt >= 0:
                    sps, R_sps = slots.pop(kt)
                    pb, R_pb = pT[pbi % NPB]
                    pbi += 1
                    ACT(pb, sps, AF.Exp, [R_sps], [R_pb], scale=DH ** -0.5)
                    blk = kt // 2
                    if blk in (4 * g, 4 * g + 1):
                        c0 = (blk - 4 * g) * 256
                        V("dve", "tensor_tensor", [R_pb, R_cm], [R_pb], out=pb[:, c0:c0 + 256], in0=pb[:, c0:c0 + 256], in1=cm[:, kt % 2, :], op=ALU.mult)
                    MM(oacc[0:65, :], Va[:, kt, 0:65], pb, kt == 0, kt == nkt - 1, [R_Va, R_pb], [R_oacc], inc=True)
                if it_ == nkt // 2 and i + 1 < len(items):
                    S2(i + 1)
            V("dve", "tensor_copy", [R_oacc], [R_osb], out=osb[0:65, :], in_=oacc[0:65, :])
            for sub in range(4):
                TR(PM[:, sub * 128:sub * 128 + 65], osb[0:65, sub * 128:(sub + 1) * 128], idf[0:65, 0:65], [R_osb, R_idf], [R_PM], inc=(sub == 3))
            pm3 = PM.rearrange("p (s c) -> p s c", c=128)
            V("dve", "reciprocal", [R_PM], [R_rec], out=rec.unsqueeze(2), in_=pm3[:, :, 64:65])
            ast, R_ast = attn_st[i % 2]
            V("dve", "tensor_tensor", [R_PM, R_rec], [R_ast], out=ast, in0=pm3[:, :, 0:64], in1=rec.unsqueeze(2).to_broadcast([128, 4, 64]), op=ALU.mult)
            kb.dma("sp", attn_v[:, 4 * g:4 * g + 4, h * 64:(h + 1) * 64], ast, reads=[R_ast])
        kb.barrier()
        AR.off = MARK1
        if stop_after == 2:
            kb.emit()
            return nc

        wo_sb, R_wo = A([128, 8, 1024], BF16, "wo")
        wo32 = [A([128, 1024], F32, "wo32_%d" % i) for i in range(1)]
        at, R_at = A([128, 4, 512], F32, "at")
        an, R_an = A([128, 4, 512], BF16, "an")
        anT, R_anT = A([128, 4, 512], BF16, "anT")
        gmT, R_gmT = A([128, 4, 512], BF16, "gmT")
        obs = [A([128, 1024], F32, "ob%d" % i) for i in range(2)]
        nf_bc, R_nf = A([128, 1024], F32, "nf_bc")
        kb.dma("sp", nf_bc, nfin_d.partition_broadcast(128), writes=[R_nf])
        wout_v = wout_d.rearrange("(kc p) n -> p kc n", p=128)
        for kc in range(8):
            w32, R_w32 = wo32[0]
            kb.dma("sp", w32, wout_v[:, kc, :], writes=[R_w32])
            V("dve", "tensor_scalar", [R_w32, R_vT], [R_wo], out=wo_sb[:, kc, :], in0=w32, scalar1=vT[:, 104 + kc:105 + kc], scalar2=None, op0=ALU.mult)
        def load_at(t):
            tsl_ = slice(t * T, (t + 1) * T)
            kb.dma("sp", at, attn_scr[tsl_, :].rearrange("(s p) f -> p s f", p=128), writes=[R_at])
            kb.dma("sp", gmT, gmT_scr.rearrange("h p s -> p h s")[:, :, tsl_], writes=[R_gmT])

        load_at(0)
        for t in range(NP):
            tok0 = t * T
            tsl = slice(tok0, tok0 + T)
            kb.dma("sp", hx, h1_scr[tsl, :].rearrange("(s p) d -> p s d", p=128), writes=[R_hx])
            V("dve", "memset", [], [R_ss], ap=ss[:, 4:8], constant=0.0)
            for sub in range(4):
                ACT(junk[:, 0:512], at[:, sub, :], AF.Square, [R_at], [R_junk, R_ss], accum_out=ss[:, 4 + sub:5 + sub])
            rs = rstd_of(ss[:, 4:8], 4, 512, R_ss)
            for sub in range(4):
                V("pool", "tensor_scalar", [R_at, R_small], [R_an], out=an[:, sub, :], in0=at[:, sub, :], scalar1=rs[:, sub:sub + 1], scalar2=None, op0=ALU.mult)
            for fc in range(4):
                b, hf = fc % 2, 0
                for sub in range(4):
                    TR(PT[b][:, hf * 512 + sub * 128:hf * 512 + (sub + 1) * 128], an[:, sub, fc * 128:(fc + 1) * 128], idb, [R_an, R_idb], [R_PT[b][hf]], inc=(sub == 3))
                CPA(anT[:, fc, :], PT[b][:, hf * 512:(hf + 1) * 512], [R_PT[b][hf]], [R_anT])
            for sub in range(4):
                sl = slice(sub * 128, (sub + 1) * 128)
                for half in range(2):
                    hs = slice(half * 512, (half + 1) * 512)
                    ps, R_ps = next_po()
                    for fc in range(4):
                        MM(ps, anT[:, fc, sl], wo_sb[:, fc, hs], fc == 0, False, [R_anT, R_wo], [R_ps], inc=False)
                    for fc in range(4):
                        MM(ps, gmT[:, fc, sl], wo_sb[:, 4 + fc, hs], False, fc == 3, [R_gmT, R_wo], [R_ps], inc=(fc == 3))
                    tm, R_tm = tmpo[(sub * 2 + half) % 2]
                    V("dve", "tensor_tensor", [R_ps, R_gt], [R_tm], out=tm, in0=ps, in1=gt_bc[:, 1, hs], op=ALU.mult)
                    V("pool", "tensor_tensor", [R_tm, R_hx], [R_hx], out=hx[:, sub, hs], in0=hx[:, sub, hs], in1=tm, op=ALU.add)
            if t + 1 < NP:
                load_at(t + 1)
            ffn_pre(1)
            norm_T(2)
            wdn_load(1)
            ffn(2, 1)
            V("dve", "memset", [], [R_ss], ap=ss[:, 0:4], constant=0.0)
            for sub in range(4):
                ACT(junk, hx[:, sub, :], AF.Square, [R_hx], [R_junk, R_ss], accum_out=ss[:, sub:sub + 1])
            rs = rstd_of(ss[:, 0:4], 4, D, R_ss)
            for sub in range(4):
                ob, R_ob = obs[sub % 2]
                ACT(ob, hx[:, sub, :], AF.Identity, [R_hx, R_small], [R_ob], scale=rs[:, sub:sub + 1])
                V("dve", "tensor_tensor", [R_ob, R_nf], [R_ob], out=ob, in0=ob, in1=nf_bc, op=ALU.mult)
                kb.dma("sp", out_d[tok0 + sub * 128:tok0 + (sub + 1) * 128, :], ob, reads=[R_ob])
        kb.barrier()
        kb.emit()
    return nc


def _consts(S, r):
    half = 32
    inv_freq = (np.float32(10000.0) ** (-np.arange(half, dtype=np.float32) / np.float32(half))).astype(np.float32)
    pos = np.arange(S, dtype=np.float32)
    ang = (pos[:, None] * inv_freq[None, :]).astype(np.float32)
    cosG = np.cos(ang).astype(np.float32)
    sinG = np.sin(ang).astype(np.float32)
    NT = S // T
    NP = NT // 2
    own = [2 * p + r for p in range(NP)]
    oth = [2 * p + 1 - r for p in range(NP)]
    gat = lambda tab, tiles: np.concatenate([tab[t * T:(t + 1) * T] for t in tiles], axis=0)
    cosT = np.ascontiguousarray(np.stack([gat(cosG, own), gat(cosG, oth)], axis=0))
    sinT = np.ascontiguousarray(np.stack([gat(sinG, own), gat(sinG, oth)], axis=0))
    NB = S // BLK
    kind = np.zeros((32, S), np.float32)
    for j in range(NB):
        kind[j, j * BLK:(j + 1) * BLK] = 1.0
    k = np.arange(128)[:, None]
    q = np.arange(256)[None, :]
    cmask = np.stack([(q >= k), (q >= k + 128)], axis=1).astype(np.float32)
    nsub = (S // 2) // 128
    patt = np.full((nsub, 32), -1e30, np.float32)
    for s_ in range(nsub):
        p, ib = s_ // 4, (s_ % 4) // 2
        L = 4 * p + ib
        G = 2 * (2 * p + r) + ib
        for j in range(NB):
            pj, which, ibj = j // 4, (j % 4) // 2, j % 2
            gt = 2 * pj + (r if which == 0 else 1 - r)
            gb = 2 * gt + ibj
            if j == L:
                patt[s_, j] = 1e30
            elif gb < G:
                patt[s_, j] = 0.0
    tt = np.arange(128)[:, None]
    s2 = np.arange(128)[None, :]
    tril = (s2 <= tt).astype(np.float32)
    return dict(ident=np.eye(128, dtype=np.float32), cosT=cosT, sinT=sinT, kind=kind, cmask=cmask,
                patt=np.ascontiguousarray(patt.reshape(1, -1)), tril=tril), own, oth


def make_in_maps(inputs, S, cores):
    f = lambda a: np.ascontiguousarray(np.asarray(a, dtype=np.float32))
    b_ada = f(inputs["b_ada"])[0]
    shared = dict(
        bgate=f(b_ada.reshape(9, D)[[2, 5, 8]]),
        nfin=f(inputs["norm_final"]).reshape(1, D),
        lng=f(inputs["gmlp_ln_g"])[0].reshape(1, 512),
        lnb=f(inputs["gmlp_ln_b"])[0].reshape(1, 512),
        w_s=f(inputs["gmlp_w_s"])[0],
        w_ada=f(inputs["w_ada"])[0],
        w_gu1=f(inputs["w_ffn1_gu"])[0], w_gu2=f(inputs["w_ffn2_gu"])[0],
        w_dn1=f(inputs["w_ffn1_down"])[0], w_dn2=f(inputs["w_ffn2_down"])[0],
        w_in=f(inputs["w_in"])[0], w_out=f(inputs["w_out"])[0],
    )
    x = f(inputs["x"])
    c = f(inputs["c"])
    cst = {r: _consts(S, r) for r in (0, 1)}
    maps = []
    for (b, r) in cores:
        vecs = np.concatenate([
            b_ada.reshape(72, 128),
            f(inputs["norm_ffn1"])[0].reshape(8, 128),
            f(inputs["norm_mix"])[0].reshape(8, 128),
            f(inputs["norm_ffn2"])[0].reshape(8, 128),
            c[b].reshape(8, 128),
            f(inputs["g_attn_out"])[0].reshape(4, 128),
            f(inputs["g_gmlp_out"])[0].reshape(4, 128),
            f(inputs["gmlp_b_s"])[0].reshape(8, 128),
        ], axis=0)
        cd, own, oth = cst[r]
        m = dict(shared)
        m.update(cd)
        xb = x[b, :S]
        gat = lambda tiles: np.concatenate([xb[t * T:(t + 1) * T] for t in tiles], axis=0)
        m["x"] = np.ascontiguousarray(np.stack([gat(own), gat(oth)], axis=0))
        m["vecs"] = np.ascontiguousarray(vecs)
        maps.append(m)
    return maps


def assemble(results, S, cores, nbatch):
    out = np.zeros((nbatch, S, D), np.float32)
    NP = (S // T) // 2
    for (b, r), res in zip(cores, results):
        o = np.asarray(res["out"], dtype=np.float32)
        for p in range(NP):
            t = 2 * p + r
            out[b, t * T:(t + 1) * T] = o[p * T:(p + 1) * T]
    return out


_NC_CACHE = {}


def kernel(**inputs):
    S = 8192
    if S not in _NC_CACHE:
        _NC_CACHE[S] = build(S)
    nc = _NC_CACHE[S]
    cores = [(c // 2, c % 2) for c in range(8)]
    maps = make_in_maps(inputs, S, cores)
    res = run_bass_kernel_spmd(nc, maps, core_ids=list(range(8)))
    return assemble(res.results, S, cores, 4)
```

```python
import contextlib
import os
import numpy as np
import concourse.bass as bass
import concourse.mybir as mybir
from concourse.bass_utils import run_bass_kernel_spmd

F32 = mybir.dt.float32
BF16 = mybir.dt.bfloat16
AF = mybir.ActivationFunctionType
ALU = mybir.AluOpType
AX = mybir.AxisListType

ENGS = ("pe", "act", "dve", "pool", "sp")
SELF_SYNC = {"pe": False, "act": True, "dve": True, "pool": True, "sp": False}


class Res:
    __slots__ = ("name", "w", "r", "dsem")

    def __init__(self, name):
        self.name = name
        self.w = {}
        self.r = {}
        self.dsem = None


class KB:
    def __init__(self, nc):
        self.nc = nc
        self.q = {e: [] for e in ENGS}
        self.cnt = {e: 0 for e in ENGS}
        self.seen = {e: {} for e in ENGS}
        self.pend = {e: ([], []) for e in ENGS}
        self.dcnt = {}
        self.sems = {}
        self.nd = 0

    def _need(self, eng, waits, sem, c):
        if sem == eng and not SELF_SYNC[eng]:
            return
        if self.seen[eng].get(sem, 0) >= c:
            return
        if waits.get(sem, 0) < c:
            waits[sem] = c

    def _deps(self, eng, reads, writes):
        waits = {}
        for r in reads:
            for s, c in r.w.items():
                self._need(eng, waits, s, c)
        for w in writes:
            for s, c in w.w.items():
                self._need(eng, waits, s, c)
            for s, c in w.r.items():
                self._need(eng, waits, s, c)
        for s, c in waits.items():
            self.q[eng].append(("wait", s, c))
            self.seen[eng][s] = c

    def _mark(self, ev, reads, writes):
        s, c = ev
        for r in reads:
            if r.r.get(s, 0) < c:
                r.r[s] = c
        for w in writes:
            w.w = {s: c}
            w.r = {}

    def op(self, eng, fn, reads=(), writes=(), inc=True):
        for r in list(reads) + list(writes):
            for e2 in ENGS:
                if e2 != eng and (r in self.pend[e2][1]):
                    raise RuntimeError("resource %s pending on %s" % (r.name, e2))
        for w in writes:
            for e2 in ENGS:
                if e2 != eng and (w in self.pend[e2][0]):
                    raise RuntimeError("resource %s pending-read on %s" % (w.name, e2))
        self._deps(eng, reads, writes)
        self.q[eng].append(("op", fn, inc))
        if inc:
            self.cnt[eng] += 1
            ev = (eng, self.cnt[eng])
            pr, pw = self.pend[eng]
            self._mark(ev, pr, pw)
            self.pend[eng] = ([], [])
            self._mark(ev, reads, writes)
        else:
            self.pend[eng][0].extend(reads)
            self.pend[eng][1].extend(writes)

    def dma(self, eng, out, in_, reads=(), writes=(), sem=None, **kw):
        if sem is None:
            sem = (list(writes) + list(reads))[0]
        if isinstance(sem, Res):
            if sem.dsem is None:
                sem.dsem = "d%d_%s" % (self.nd, sem.name)
                self.nd += 1
            sem = sem.dsem
        self._deps(eng, reads, writes)
        self.dcnt[sem] = self.dcnt.get(sem, 0) + 16
        self.q[eng].append(("dma", out, in_, sem, kw))
        self._mark((sem, self.dcnt[sem]), reads, writes)

    def barrier(self, skip_prefix=None):
        for e in ENGS:
            assert not self.pend[e][0] and not self.pend[e][1], e
        allev = [(e, self.cnt[e]) for e in ENGS if self.cnt[e] > 0]
        allev += [(k_, v_) for k_, v_ in self.dcnt.items() if not (skip_prefix and k_.startswith(skip_prefix))]
        for e in ENGS:
            for s, c in allev:
                if s == e or c == 0:
                    continue
                if self.seen[e].get(s, 0) < c:
                    self.q[e].append(("wait", s, c))
                    self.seen[e][s] = c

    def emit(self):
        nc = self.nc
        names = list(ENGS) + list(self.dcnt.keys())
        import contextlib
        with contextlib.ExitStack() as es:
            for n in names:
                self.sems[n] = es.enter_context(nc.semaphore("s_" + n))
            block = es.enter_context(nc.Block())

            def run(e, eng):
                for it in self.q[e]:
                    if it[0] == "wait":
                        eng.wait_ge(self.sems[it[1]], it[2])
                    elif it[0] == "op":
                        ins = it[1](eng)
                        if it[2]:
                            ins.then_inc(self.sems[e], 1)
                    else:
                        _, out, in_, sem, kw = it
                        eng.dma_start(out=out, in_=in_, **kw).then_inc(self.sems[sem], 16)

            @block.tensor
            def _(eng):
                run("pe", eng)

            @block.scalar
            def _(eng):
                run("act", eng)

            @block.vector
            def _(eng):
                run("dve", eng)

            @block.gpsimd
            def _(eng):
                run("pool", eng)

            @block.sync
            def _(eng):
                run("sp", eng)


D = 1024
DFF = 2816
NJ = DFF // 128
H = 8
DH = 64
T = 512
BLK = 256
EPS = 1e-6
BIG = 30000.0
CW = 53000


class Arena:
    def __init__(self, ap):
        self.ap = ap
        self.off = 0

    def alloc(self, shape, dt):
        n = 1
        for s in shape[1:]:
            n *= s
        nf = n if dt == F32 else (n + 1) // 2
        a = self.ap[:, self.off:self.off + nf]
        self.off += nf
        assert self.off <= CW, ("arena overflow", self.off)
        v = a if dt == F32 else a.bitcast(BF16)[:, 0:n]
        if len(shape) == 3:
            v = v.rearrange("p (a b) -> p a b", a=shape[1])
        elif len(shape) == 4:
            v = v.rearrange("p (a b c) -> p a b c", a=shape[1], b=shape[2])
        return v


def build(S, debug=False, stop_after=9):
    NT = S // T
    NP = NT // 2
    SO = S // 2
    NKT = S // 128
    NB = S // BLK
    nc = bass.Bass("TRN2", target_bir_lowering=False)

    def din(name, shape, dt=F32):
        return nc.dram_tensor(name, list(shape), dt, kind="ExternalInput").ap()

    def dscr(name, shape, dt, dbg=False):
        kind = "ExternalOutput" if (debug and dbg) else "Internal"
        return nc.dram_tensor(name, list(shape), dt, kind=kind).ap()

    x_d = din("x", [2, SO, D])
    vecs_d = din("vecs", [120, 128])
    bgate_d = din("bgate", [3, D])
    nfin_d = din("nfin", [1, D])
    lng_d = din("lng", [1, 512])
    lnb_d = din("lnb", [1, 512])
    ws_d = din("w_s", [8, 128, 128])
    wada_d = din("w_ada", [D, 9 * D])
    wgu_d = [din("w_gu1", [D, 2 * DFF]), din("w_gu2", [D, 2 * DFF])]
    wdn_d = [din("w_dn1", [DFF, D]), din("w_dn2", [DFF, D])]
    win_d = din("w_in", [D, 2560])
    wout_d = din("w_out", [D, D])
    ident_d = din("ident", [128, 128])
    cos_d = din("cosT", [2, SO, 32])
    sin_d = din("sinT", [2, SO, 32])
    kind_d = din("kind", [32, S])
    cm_d = din("cmask", [128, 2, 256])
    patt_d = din("patt", [1, (SO // 128) * 32])
    tril_d = din("tril", [128, 128])
    out_d = nc.dram_tensor("out", [SO, D], F32, kind="ExternalOutput").ap()

    wgu_b = [dscr("wgu1_b", [D, 2 * DFF], BF16), dscr("wgu2_b", [D, 2 * DFF], BF16)]
    wdn_b = [dscr("wdn1_b", [DFF, D], BF16), dscr("wdn2_b", [DFF, D], BF16)]
    win_b = dscr("win_b", [D, 2560], BF16)
    h1_scr = dscr("h1_scr", [SO, D], F32, True)
    q_scr = dscr("q_scr", [4, 128, SO], F32, True)
    k_scr = dscr("k_scr", [4, 128, S], BF16)
    v_scr = dscr("v_scr", [8, 128, NKT, 65], BF16)
    gmT_scr = dscr("gmT_scr", [4, 128, SO], BF16)
    km_scr = dscr("km_scr", [4, 128, NB], F32, True)
    attn_scr = dscr("attn_scr", [SO, 512], F32, True)

    es = contextlib.ExitStack()
    with es:
        arena_t = es.enter_context(nc.sbuf_tensor("arena", [128, CW], F32))
        AR = Arena(arena_t[:, :])
        PGU = [es.enter_context(nc.psum_tensor("pgu%d" % i, [128, 512], F32))[:, :] for i in range(4)]
        PO = [es.enter_context(nc.psum_tensor("po%d" % i, [128, 512], F32))[:, :] for i in range(2)]
        PT = [es.enter_context(nc.psum_tensor("pt%d" % i, [128, 1024], BF16))[:, :] for i in range(2)]
        R_PGU = [Res("pgu%d" % i) for i in range(4)]
        R_PO = [Res("po%d" % i) for i in range(2)]
        R_PT = []
        for i in range(2):
            r_ = Res("pt%d" % i)
            R_PT.append([r_, r_])
        kb = KB(nc)

        def MM(out, lhsT, rhs, st, sp, R=(), W=(), inc=True):
            kb.op("pe", lambda e: e.matmul(out, lhsT=lhsT, rhs=rhs, start=st, stop=sp), R, W, inc)

        def TR(out, in_, ident, R=(), W=(), inc=True):
            kb.op("pe", lambda e: e.transpose(out=out, in_=in_, identity=ident), R, W, inc)

        def ACT(out, in_, func, R=(), W=(), **kw):
            kb.op("act", lambda e: e.activation(out=out, in_=in_, func=func, **kw), R, W)

        def CPA(out, in_, R=(), W=()):
            kb.op("act", lambda e: e.copy(out=out, in_=in_), R, W)

        def V(eng, name, R=(), W=(), **kw):
            if eng == "pool":
                eng = "dve"
            kb.op(eng, lambda e: getattr(e, name)(**kw), R, W)

        po_i = [0]

        def next_po():
            i = po_i[0] % 2
            po_i[0] += 1
            return PO[i], R_PO[i]

        def A(shape, dt, name):
            return AR.alloc(shape, dt), Res(name)

        idf, R_idf = A([128, 128], F32, "idf")
        idb, R_idb = A([128, 128], BF16, "idb")
        vT, R_vT = A([128, 120], F32, "vT")
        modT, R_modT = A([128, 72], F32, "modT")
        Asc, R_Asc = A([128, 3, 8], F32, "Asc")
        gt_bc, R_gt = A([128, 3, 1024], F32, "gt_bc")
        lng_bc, R_lng = A([128, 512], F32, "lng")
        lnb_bc, R_lnb = A([128, 512], F32, "lnb")
        WsT, R_WsT = A([128, 8, 128], BF16, "WsT")
        patt, R_patt = A([128, SO // 128, 32], F32, "patt")
        cm, R_cm = A([128, 2, 256], BF16, "cm")
        onesc, R_onesc = A([128, 2], F32, "onesc")
        kms, R_kms = A([128, 4, NKT], F32, "kms")
        small, R_small = A([128, 64], F32, "small")
        MARK_P = AR.off
        hx, R_hx = A([128, 4, 1024], F32, "hx")
        tb, R_tb = A([128, 4, 1024], BF16, "tb")
        yT, R_yT = A([128, 8, 512], BF16, "yT")
        aT, R_aT = A([128, NJ, 512], BF16, "aT")
        NWG = 2
        wg = [A([128, 8, 2, 256], BF16, "wg%d" % i) for i in range(NWG)]
        wdn, R_wdn = A([128, NJ, 1024], BF16, "wdn")
        sg = [A([128, 512], F32, "sg%d" % i) for i in range(2)]
        tmpo = [A([128, 512], F32, "tmpo%d" % i) for i in range(2)]
        ss, R_ss = A([128, 16], F32, "ss")
        nsm, _ = A([128, 32], F32, "nsm")
        R_nsq = [Res("nsq%d" % i) for i in range(4)]
        junk, R_junk = A([128, 1024], BF16, "junk")
        MARK0 = AR.off
        MARK1 = MARK0

        kb.dma("sp", idf, ident_d, writes=[R_idf])
        V("dve", "tensor_copy", [R_idf], [R_idb], out=idb, in_=idf)
        R_wgub = [Res("wgu1b"), Res("wgu2b")]
        R_wdnb = [Res("wdn1b"), Res("wdn2b")]
        R_winb = Res("winb")

        def cast_w(dst, src, rows, step, R, sem):
            for r0 in range(0, rows, step):
                kb.dma("pool", dst[r0:r0 + step, :], src[r0:r0 + step, :], writes=[R], sem=sem)

        cast_w(wgu_b[0], wgu_d[0], D, 128, R_wgub[0], "pre_gu1")
        kb.dma("pool", cm, cm_d, writes=[R_cm])

        vecs_sb, R_vecs = A([128, 128], F32, "vecs")
        kb.dma("sp", vecs_sb[0:120, :], vecs_d, writes=[R_vecs])
        kb.dma("sp", lng_bc, lng_d.partition_broadcast(128), writes=[R_lng])
        kb.dma("sp", lnb_bc, lnb_d.partition_broadcast(128), writes=[R_lnb])
        kb.dma("sp", patt.rearrange("p a b -> p (a b)"), patt_d.partition_broadcast(128), writes=[R_patt])
        bg_bc, R_bg = A([128, 3, 1024], F32, "bg_bc")
        for gi in range(3):
            kb.dma("sp", bg_bc[:, gi, :], bgate_d[gi:gi + 1, :].partition_broadcast(128), writes=[R_bg])
        V("dve", "memset", [], [R_onesc], ap=onesc, constant=1.0 / BLK)
        TR(PO[0][:, 0:120], vecs_sb[0:120, :], idf[0:120, 0:120], [R_vecs, R_idf], [R_PO[0]])
        CPA(vT, PO[0][:, 0:120], [R_PO[0]], [R_vT])
        cact, R_cact = A([128, 8], F32, "cact")
        cT, R_cT = A([128, 8], BF16, "cT")
        crep, R_crep = A([128, 8, 128], BF16, "crep")
        ACT(cact, vT[:, 96:104], AF.Silu, [R_vT], [R_cact])
        V("dve", "tensor_copy", [R_cact], [R_cT], out=cT, in_=cact)
        for kc in range(8):
            V("dve", "tensor_copy", [R_cT], [R_crep], out=crep[:, kc, :],
              in_=cT[:, kc:kc + 1].to_broadcast([128, 128]))
        for gi in range(3):
            V("dve", "tensor_scalar", [R_bg], [R_bg], out=bg_bc[:, gi, :], in0=bg_bc[:, gi, :],
              scalar1=(1.0 if gi == 1 else 0.5), scalar2=None, op0=ALU.mult)
        wa = [A([128, 8, 512], BF16, "wa%d" % i) for i in range(4)]
        wada_v = wada_d.rearrange("(kc p) n -> p kc n", p=128)
        PMOD, R_PMOD = PGU[0], R_PGU[0]
        for cg in range(18):
            wab, R_wab = wa[cg % 4]
            for kh in range(2):
                kb.dma("pool", wab[:, 4 * kh:4 * kh + 4, :], wada_v[:, 4 * kh:4 * kh + 4, cg * 512:(cg + 1) * 512],
                       writes=[R_wab])
            for cc in range(4):
                col = cg * 4 + cc
                for kc in range(8):
                    MM(PMOD[:, col:col + 1], wab[:, kc, cc * 128:(cc + 1) * 128], cT[:, kc:kc + 1],
                       kc == 0, kc == 7, [R_wab, R_cT], [R_PMOD], inc=(kc == 7))
            v = cg // 2
            if v in (2, 5, 8):
                gi = (v - 2) // 3
                half = cg % 2
                ps, R_ps = PGU[2 + half], R_PGU[2 + half]
                for kc in range(8):
                    MM(ps, crep[:, kc, :], wab[:, kc, :], kc == 0, kc == 7, [R_wab, R_crep], [R_ps], inc=(kc == 7))
                V("dve", "scalar_tensor_tensor", [R_ps, R_bg], [R_gt], out=gt_bc[:, gi, half * 512:(half + 1) * 512],
                  in0=ps, scalar=(1.0 if gi == 1 else 0.5), in1=bg_bc[:, gi, half * 512:(half + 1) * 512],
                  op0=ALU.mult, op1=ALU.add)
        cast_w(wdn_b[0], wdn_d[0], DFF, 256, R_wdnb[0], "pre_dn1")
        cast_w(win_b, win_d, D, 256, R_winb, "pre_in")
        cast_w(wgu_b[1], wgu_d[1], D, 128, R_wgub[1], "pre_gu2")
        cast_w(wdn_b[1], wdn_d[1], DFF, 256, R_wdnb[1], "pre_dn2")
        V("dve", "tensor_tensor", [R_PMOD, R_vT], [R_modT], out=modT, in0=PMOD[:, 0:72], in1=vT[:, 0:72], op=ALU.add)
        for k in range(3):
            V("dve", "scalar_tensor_tensor", [R_modT, R_vT], [R_Asc], out=Asc[:, k, :],
              in0=modT[:, (3 * k + 1) * 8:(3 * k + 1) * 8 + 8], scalar=1.0, in1=vT[:, 72 + 8 * k:80 + 8 * k],
              op0=ALU.add, op1=ALU.mult)
        ws32, R_ws32 = A([128, 8, 128], F32, "ws32")
        trilm, R_tril = A([128, 128], F32, "tril")
        wsm, R_wsm = A([128, 8, 128], BF16, "wsm")
        kb.dma("sp", ws32, ws_d.rearrange("h t s -> t h s"), writes=[R_ws32])
        kb.dma("sp", trilm, tril_d, writes=[R_tril])
        for h in range(8):
            V("dve", "tensor_tensor", [R_ws32, R_tril], [R_wsm], out=wsm[:, h, :], in0=ws32[:, h, :], in1=trilm, op=ALU.mult)
        for h in range(8):
            TR(PT[0][:, h * 128:(h + 1) * 128], wsm[:, h, :], idb, [R_wsm, R_idb], [R_PT[0][0], R_PT[0][1]], inc=(h == 7))
        V("dve", "tensor_copy", [R_PT[0][0], R_PT[0][1]], [R_WsT], out=WsT.rearrange("p h t -> p (h t)"), in_=PT[0][:, 0:1024])
        kb.barrier()
        AR.off = MARK0
        if stop_after == 0:
            kb.emit()
            return nc

        def rstd_of(ss_ap, n, nfeat, R_in):
            V("dve", "tensor_scalar", [R_in], [R_small], out=small[:, 16:16 + n], in0=ss_ap, scalar1=1.0 / nfeat,
              scalar2=EPS, op0=ALU.mult, op1=ALU.add)
            ACT(small[:, 32:32 + n], small[:, 16:16 + n], AF.Sqrt, [R_small], [R_small])
            V("dve", "reciprocal", [R_small], [R_small], out=small[:, 0:n], in_=small[:, 32:32 + n])
            return small[:, 0:n]

        def norm_T(k):
            norm_stats()
            norm_tr(k)

        def norm_stats():
            V("dve", "memset", [], [R_ss], ap=ss[:, 0:4], constant=0.0)
            KD = 9
            R_sq = [R_nsq[i] for i in range(4)]

            def sq(sub):
                ACT(junk, hx[:, sub, :], AF.Square, [R_hx, R_ss], [R_junk, R_sq[sub]], accum_out=ss[:, sub:sub + 1])

            sq(0)
            for sub in range(4):
                if sub + 1 < 4:
                    sq(sub + 1)
                c0 = 16 + sub
                V("dve", "tensor_scalar", [R_sq[sub], R_ss], [R_sq[sub]], out=nsm[:, c0:c0 + 1], in0=ss[:, sub:sub + 1], scalar1=1.0 / D,
                  scalar2=EPS, op0=ALU.mult, op1=ALU.add)
                ACT(nsm[:, 8 + sub:9 + sub], nsm[:, c0:c0 + 1], AF.Sqrt, [R_sq[sub]], [R_sq[sub]])
                V("dve", "reciprocal", [R_sq[sub]], [R_sq[sub]], out=nsm[:, sub:sub + 1], in_=nsm[:, 8 + sub:9 + sub])
                V("dve", "tensor_scalar", [R_hx, R_sq[sub]], [R_tb], out=tb[:, sub, :], in0=hx[:, sub, :],
                  scalar1=nsm[:, sub:sub + 1], scalar2=None, op0=ALU.mult)

        def norm_tr(k):
            KD = 9
            for c in range(8):
                b, hf = c % 2, 0
                for sub in range(4):
                    TR(PT[b][:, hf * 512 + sub * 128: hf * 512 + (sub + 1) * 128], tb[:, sub, c * 128:(c + 1) * 128], idb,
                       [R_tb, R_idb], [R_PT[b][hf]], inc=(sub == 3))
                if KD == 3:
                    continue
                if c % 2 == 0:
                    ACT(yT[:, c, :], PT[b][:, hf * 512:(hf + 1) * 512], AF.Identity, [R_PT[b][hf], R_Asc, R_modT], [R_yT],
                        scale=Asc[:, k, c:c + 1], bias=modT[:, 3 * k * 8 + c:3 * k * 8 + c + 1])
                else:
                    V("dve", "tensor_scalar", [R_PT[b][hf], R_Asc, R_modT], [R_yT], out=yT[:, c, :], in0=PT[b][:, hf * 512:(hf + 1) * 512],
                      scalar1=Asc[:, k, c:c + 1], scalar2=modT[:, 3 * k * 8 + c:3 * k * 8 + c + 1], op0=ALU.mult, op1=ALU.add)

        def wg_load(f, jp):
            wgu_v = wgu_b[f].rearrange("(kc p) (two n) -> p kc two n", p=128, two=2)
            wgb, R_wgb = wg[jp % NWG]
            for two in range(2):
                kb.dma("sp", wgb[:, :, two, :], wgu_v[:, :, two, jp * 256:(jp + 1) * 256], reads=[R_wgub[f]], writes=[R_wgb])

        def ffn_pre(f):
            for jp in range(NWG):
                wg_load(f, jp)

        def wdn_load(f):
            wdn_v = wdn_b[f].rearrange("(j p) d -> p j d", p=128)
            for jh in range(2):
                kb.dma("sp", wdn[:, 11 * jh:11 * jh + 11, :], wdn_v[:, 11 * jh:11 * jh + 11, :], reads=[R_wdnb[f]], writes=[R_wdn])

        def ffn(k, f):
            for jp in range(NJ // 2):
                wgb, R_wgb = wg[jp % NWG]
                if jp >= NWG:
                    wg_load(f, jp)
                for jl in range(2):
                    jj = 2 * jp + jl
                    s = jj % 2
                    pg, R_pg = PGU[2 * s], R_PGU[2 * s]
                    pu, R_pu = PGU[2 * s + 1], R_PGU[2 * s + 1]
                    for kc in range(8):
                        MM(pg, wgb[:, kc, 0, jl * 128:(jl + 1) * 128], yT[:, kc, :], kc == 0, kc == 7, [R_wgb, R_yT], [R_pg], inc=(kc == 7))
                    for kc in range(8):
                        MM(pu, wgb[:, kc, 1, jl * 128:(jl + 1) * 128], yT[:, kc, :], kc == 0, kc == 7, [R_wgb, R_yT], [R_pu], inc=(kc == 7))
                    sgb, R_sgb = sg[s]
                    ACT(sgb, pg, AF.Silu, [R_pg], [R_sgb])
                    V("dve", "tensor_tensor", [R_sgb, R_pu], [R_aT], out=aT[:, jj, :], in0=sgb, in1=pu, op=ALU.mult)
            for sub in range(4):
                for half in range(2):
                    ps, R_ps = next_po()
                    for jj in range(NJ):
                        MM(ps, aT[:, jj, sub * 128:(sub + 1) * 128], wdn[:, jj, half * 512:(half + 1) * 512], jj == 0, jj == NJ - 1,
                           [R_aT, R_wdn], [R_ps], inc=(jj == NJ - 1))
                    tm, R_tm = tmpo[(sub * 2 + half) % 2]
                    V("dve", "tensor_tensor", [R_ps, R_gt], [R_tm], out=tm, in0=ps, in1=gt_bc[:, k, half * 512:(half + 1) * 512], op=ALU.mult)
                    V("pool", "tensor_tensor", [R_tm, R_hx], [R_hx], out=hx[:, sub, half * 512:(half + 1) * 512],
                      in0=hx[:, sub, half * 512:(half + 1) * 512], in1=tm, op=ALU.add)

        wi = [A([128, 8, 512], BF16, "wi%d" % i) for i in range(2)]
        win_v = win_b.rearrange("(kc p) n -> p kc n", p=128)
        cosb, R_cos = A([128, 4, 32], F32, "cosb")
        sinb, R_sin = A([128, 4, 32], F32, "sinb")
        qr, R_qr = A([128, 8, 2, 32], F32, "qr")
        rtmp, R_rtmp = A([128, 8, 32], F32, "rtmp")
        k16, R_k16 = A([128, 512], BF16, "k16")
        qT_st, R_qTst = A([128, 4, 512], F32, "qTst")
        kT_st, R_kTst = A([128, 4, 512], BF16, "kTst")
        v_st, R_vst = A([128, 4, 8, 65], BF16, "vst")
        V("dve", "memset", [], [R_vst], ap=v_st, constant=1.0)
        gmT_st, R_gmTst = A([128, 4, 512], BF16, "gmTst")
        gug, R_gug = A([128, 4, 512], F32, "gug")
        gvg, R_gvg = A([128, 512], F32, "gvg")
        vn, R_vn = A([128, 512], BF16, "vn")
        gm, R_gm = A([128, 512], F32, "gm")
        gmn, R_gmn = A([128, 512], BF16, "gmn")
        bsT = vT[:, 112:120]
        wi_i = [0]

        def rope(ps, dst, R_ps, R_dst, sub):
            p4 = ps.rearrange("p (h t d) -> p h t d", h=8, t=2)
            cb = cosb[:, sub:sub + 1, :].to_broadcast([128, 8, 32])
            sb_ = sinb[:, sub:sub + 1, :].to_broadcast([128, 8, 32])
            V("dve", "tensor_tensor", [R_ps, R_cos], [R_dst], out=dst[:, :, 0, :], in0=p4[:, :, 0, :], in1=cb, op=ALU.mult)
            V("dve", "tensor_tensor", [R_ps, R_sin], [R_rtmp], out=rtmp, in0=p4[:, :, 1, :], in1=sb_, op=ALU.mult)
            V("dve", "tensor_tensor", [R_dst, R_rtmp], [R_dst], out=dst[:, :, 0, :], in0=dst[:, :, 0, :], in1=rtmp, op=ALU.subtract)
            V("dve", "tensor_tensor", [R_ps, R_cos], [R_dst], out=dst[:, :, 1, :], in0=p4[:, :, 1, :], in1=cb, op=ALU.mult)
            V("dve", "tensor_tensor", [R_ps, R_sin], [R_rtmp], out=rtmp, in0=p4[:, :, 0, :], in1=sb_, op=ALU.mult)
            V("dve", "tensor_tensor", [R_dst, R_rtmp], [R_dst], out=dst[:, :, 1, :], in0=dst[:, :, 1, :], in1=rtmp, op=ALU.add)

        ALLPO = [(PO[0], R_PO[0]), (PO[1], R_PO[1])] + [(PGU[i], R_PGU[i]) for i in range(4)]
        po6 = [0]

        def next_po6():
            i = po6[0] % 6
            po6[0] += 1
            return ALLPO[i]

        def load_x(t):
            pr, which = t // 2, t % 2
            kb.dma("sp", hx, x_d[which, pr * T:(pr + 1) * T, :].rearrange("(s p) d -> p s d", p=128), writes=[R_hx])

        def load_cs(t):
            pr, which = t // 2, t % 2
            kb.dma("sp", cosb, cos_d[which, pr * T:(pr + 1) * T, :].rearrange("(s p) d -> p s d", p=128), writes=[R_cos])
            kb.dma("sp", sinb, sin_d[which, pr * T:(pr + 1) * T, :].rearrange("(s p) d -> p s d", p=128), writes=[R_sin])

        def stage_b(t, grp, sub, ps, R_ps):
            sl = slice(sub * 128, (sub + 1) * 128)
            st_i = t * 4 + sub
            if grp == 0:
                rope(ps, qr, R_ps, R_qr, sub)
                qf = qr.rearrange("p h t d -> p (h t d)")
                ps2, R_ps2 = next_po6()
                for hp in range(4):
                    TR(ps2[:, hp * 128:(hp + 1) * 128], qf[:, hp * 128:(hp + 1) * 128], idf, [R_qr, R_idf], [R_ps2], inc=(hp == 3))
                CPA(qT_st[:, :, sl], ps2.rearrange("p (h s) -> p h s", h=4), [R_ps2], [R_qTst])
            elif grp == 1:
                rope(ps, qr, R_ps, R_qr, sub)
                kf = qr.rearrange("p h t d -> p (h t d)")
                ps2, R_ps2 = next_po6()
                for hp in range(4):
                    MM(ps2[:, hp:hp + 1], kf[:, hp * 128:(hp + 1) * 128], onesc[:, 0:1], True, True, [R_qr, R_onesc], [R_ps2], inc=(hp == 3))
                V("dve", "tensor_copy", [R_ps2], [R_kms], out=kms[:, :, st_i], in_=ps2[:, 0:4])
                V("dve", "tensor_copy", [R_qr], [R_k16], out=k16, in_=kf)
                for hp in range(4):
                    TR(PT[0][:, hp * 128:(hp + 1) * 128], k16[:, hp * 128:(hp + 1) * 128], idb, [R_k16, R_idb], [R_PT[0][0]], inc=(hp == 3))
                CPA(kT_st[:, :, sl], PT[0][:, 0:512].rearrange("p (h s) -> p h s", h=4), [R_PT[0][0]], [R_kTst])
            elif grp == 2:
                CPA(v_st[:, sub, :, 0:64], ps.rearrange("p (h d) -> p h d", h=8), [R_ps], [R_vst])
            elif grp == 3:
                ACT(gug[:, sub, :], ps, AF.Gelu_apprx_tanh, [R_ps], [R_gug])
            else:
                V("dve", "memset", [], [R_ss], ap=ss[:, 4:8], constant=0.0)
                ACT(gvg, ps, AF.Gelu_apprx_tanh, [R_ps], [R_gvg, R_ss], accum_out=ss[:, 4:5])
                ACT(junk[:, 0:512], gvg, AF.Square, [R_gvg], [R_junk, R_ss], accum_out=ss[:, 5:6])
                V("dve", "tensor_scalar", [R_ss], [R_ss], out=ss[:, 8:9], in0=ss[:, 4:5], scalar1=1.0 / 512, scalar2=None, op0=ALU.mult)
                V("dve", "tensor_tensor", [R_ss], [R_ss], out=ss[:, 9:10], in0=ss[:, 8:9], in1=ss[:, 8:9], op=ALU.mult)
                V("dve", "scalar_tensor_tensor", [R_ss], [R_ss], out=ss[:, 10:11], in0=ss[:, 5:6], scalar=1.0 / 512, in1=ss[:, 9:10],
                  op0=ALU.mult, op1=ALU.subtract)
                V("dve", "tensor_scalar", [R_ss], [R_ss], out=ss[:, 11:12], in0=ss[:, 10:11], scalar1=EPS, scalar2=None, op0=ALU.add)
                ACT(ss[:, 12:13], ss[:, 11:12], AF.Sqrt, [R_ss], [R_ss])
                V("dve", "reciprocal", [R_ss], [R_ss], out=ss[:, 13:14], in_=ss[:, 12:13])
                V("dve", "scalar_tensor_tensor", [R_ss], [R_ss], out=ss[:, 14:15], in0=ss[:, 8:9], scalar=-1.0, in1=ss[:, 13:14],
                  op0=ALU.mult, op1=ALU.mult)
                ACT(gvg, gvg, AF.Identity, [R_gvg, R_ss], [R_gvg], scale=ss[:, 13:14], bias=ss[:, 14:15])
                V("dve", "tensor_tensor", [R_gvg, R_lng], [R_gvg], out=gvg, in0=gvg, in1=lng_bc, op=ALU.mult)
                V("dve", "tensor_tensor", [R_gvg, R_lnb], [R_vn], out=vn, in0=gvg, in1=lnb_bc, op=ALU.add)
                ps3, R_ps3 = next_po6()
                for hg in range(8):
                    MM(ps3[:, hg * 64:(hg + 1) * 64], WsT[:, hg, :], vn[:, hg * 64:(hg + 1) * 64], True, True, [R_WsT, R_vn], [R_ps3], inc=(hg == 7))
                V("dve", "tensor_tensor", [R_ps3, R_vT], [R_gm], out=gm.rearrange("p (h d) -> p h d", h=8),
                  in0=ps3.rearrange("p (h d) -> p h d", h=8), in1=bsT.unsqueeze(2).to_broadcast([128, 8, 64]), op=ALU.add)
                V("dve", "tensor_tensor", [R_gm, R_gug], [R_gm], out=gm, in0=gm, in1=gug[:, sub, :], op=ALU.mult)
                ACT(junk[:, 0:512], gm, AF.Square, [R_gm], [R_junk, R_ss], accum_out=ss[:, 6:7])
                rs = rstd_of(ss[:, 6:7], 1, 512, R_ss)
                V("dve", "tensor_scalar", [R_gm, R_small], [R_gmn], out=gmn, in0=gm, scalar1=rs[:, 0:1], scalar2=None, op0=ALU.mult)
                for fc in range(4):
                    TR(PT[1][:, fc * 128:(fc + 1) * 128], gmn[:, fc * 128:(fc + 1) * 128], idb, [R_gmn, R_idb], [R_PT[1][0]], inc=(fc == 3))
                CPA(gmT_st[:, :, sl], PT[1][:, 0:512].rearrange("p (h s) -> p h s", h=4), [R_PT[1][0]], [R_gmTst])

        load_x(0)
        load_cs(0)
        for t in range(NT):
            pr, which = t // 2, t % 2
            own_t = (which == 0)
            tok0 = t * T
            tsl = slice(tok0, tok0 + T)
            osl = slice(pr * T, (pr + 1) * T)
            ffn_pre(0)
            if t == 0:
                norm_stats()
            norm_tr(0)
            wdn_load(0)
            ffn(0, 0)
            if own_t:
                kb.dma("sp", h1_scr[osl, :].rearrange("(s p) d -> p s d", p=128), hx, reads=[R_hx])
            grps = list(range(5)) if own_t else [1, 2]

            def wi_load(grp_):
                wib_, R_wib_ = wi[wi_i[0] % 2]
                wi_i[0] += 1
                for kh in range(2):
                    kb.dma("sp", wib_[:, 4 * kh:4 * kh + 4, :], win_v[:, 4 * kh:4 * kh + 4, grp_ * 512:(grp_ + 1) * 512], reads=[R_winb], writes=[R_wib_])
                return wib_, R_wib_

            nxt_w = wi_load(grps[0])
            norm_T(1)
            if t + 1 < NT:
                load_x(t + 1)
            for gi_, grp in enumerate(grps):
                wib, R_wib = nxt_w
                if gi_ + 1 < len(grps):
                    nxt_w = wi_load(grps[gi_ + 1])
                if gi_ == len(grps) - 1 and t + 1 < NT:
                    norm_stats()
                pend = None
                for sub in range(4):
                    sl = slice(sub * 128, (sub + 1) * 128)
                    ps, R_ps = next_po6()
                    for kc in range(8):
                        MM(ps, yT[:, kc, sl], wib[:, kc, :], kc == 0, kc == 7, [R_yT, R_wib], [R_ps], inc=(kc == 7))
                    if pend is not None:
                        stage_b(t, grp, *pend)
                    pend = (sub, ps, R_ps)
                stage_b(t, grp, *pend)
                if grp == 0:
                    kb.dma("sp", q_scr.rearrange("h p s -> p h s")[:, :, osl], qT_st, reads=[R_qTst])
                elif grp == 1:
                    kb.dma("sp", k_scr.rearrange("h p s -> p h s")[:, :, tsl], kT_st, reads=[R_kTst])
                    if t + 1 < NT:
                        load_cs(t + 1)
                elif grp == 2:
                    for sub_ in range(4):
                        kb.dma("act", v_scr.rearrange("h p k d -> p k h d")[:, 4 * t + sub_, :, :], v_st[:, sub_, :, :], reads=[R_vst])
                elif grp == 4:
                    kb.dma("sp", gmT_scr.rearrange("h p s -> p h s")[:, :, osl], gmT_st, reads=[R_gmTst])
        kmv = kms.rearrange("p h (b two) -> p h b two", two=2)
        kmb, R_kmb = A([128, 4, NB], F32, "kmb")
        V("dve", "tensor_tensor", [R_kms], [R_kmb], out=kmb, in0=kmv[:, :, :, 0], in1=kmv[:, :, :, 1], op=ALU.add)
        kb.dma("sp", km_scr.rearrange("h p b -> p h b"), kmb, reads=[R_kmb])
        kb.barrier()
        AR.off = MARK1
        if stop_after == 1:
            kb.emit()
            return nc

        NG = NP
        AR.off = MARK_P
        Kaug = [A([128, S], BF16, "kaug%d" % i) for i in range(2)]
        Vaug = [A([128, NKT, 65], BF16, "vaug%d" % i) for i in range(2)]
        kmh = [A([128, NB], F32, "kmh%d" % i) for i in range(2)]
        Q32 = [A([128, 512], F32, "q32_%d" % i) for i in range(3)]
        Qaug = [A([128, 512], BF16, "qaug%d" % i) for i in range(2)]
        NPB = 3
        pT = [A([128, 512], BF16, "pT%d" % i) for i in range(NPB)]
        osb, R_osb = A([128, 512], F32, "osb")
        stg, R_stg = A([128, 4, 96], BF16, "stg")
        gsb, R_gsb = A([128, 32], F32, "gsb")
        mx8, R_mx8 = A([128, 8], F32, "mx8")
        mtmp, R_mtmp = A([128, 32], F32, "mtmp")
        rec, R_rec = A([128, 4], F32, "rec")
        attn_st = [A([128, 4, 64], F32, "attnst%d" % i) for i in range(2)]
        V("dve", "memset", [], [R_stg], ap=stg, constant=0.0)
        V("dve", "memset", [], [R_gsb], ap=gsb, constant=-1e30)
        for i in range(2):
            kb.dma("pool", Kaug[i][0][64:96, :], kind_d, writes=[Kaug[i][1]], sem="kind%d" % i)
        SB = [(PGU[0], R_PGU[0]), (PGU[1], R_PGU[1]), (PGU[2], R_PGU[2])]
        PM, R_PM = PGU[3], R_PGU[3]
        attn_v = attn_scr.rearrange("(s p) f -> p s f", p=128)

        def load_head(h):
            hp, r0 = h // 2, (h % 2) * 64
            hb = h % 2
            kb.dma("act", Kaug[hb][0][0:64, :], k_scr[hp, r0:r0 + 64, :], writes=[Kaug[hb][1]])
            kb.dma("act", Vaug[hb][0], v_scr[h], writes=[Vaug[hb][1]])
            kb.dma("act", kmh[hb][0][0:64, :], km_scr[hp, r0:r0 + 64, :], writes=[kmh[hb][1]])

        items = [(h, g) for h in range(H) for g in range(NG)]

        def q_load(i):
            h, g = items[i]
            hp, r0 = h // 2, (h % 2) * 64
            Q3, R_Q3 = Q32[i % 3]
            kb.dma("sp", Q3[0:64, :], q_scr[hp, r0:r0 + 64, g * 512:(g + 1) * 512], writes=[R_Q3])

        def S1(i):
            h, g = items[i]
            km, R_km = kmh[h % 2]
            Q3, R_Q3 = Q32[i % 3]
            Qa, R_Qa = Qaug[i % 2]
            V("dve", "tensor_copy", [R_Q3], [R_Qa], out=Qa[0:64, :], in_=Q3[0:64, :])
            for sub in range(4):
                MM(PM[:, sub * 32:sub * 32 + NB], Q3[0:64, sub * 128:(sub + 1) * 128], km[0:64, 0:NB], True, True, [R_Q3, R_km], [R_PM], inc=(sub == 3))
            for sub in range(4):
                V("dve", "tensor_tensor", [R_PM, R_patt], [R_gsb], out=gsb[:, 0:NB], in0=PM[:, sub * 32:sub * 32 + NB], in1=patt[:, 4 * g + sub, 0:NB], op=ALU.add)
                V("dve", "max", [R_gsb], [R_mx8], out=mx8, in_=gsb)
                V("dve", "tensor_scalar", [R_mx8], [R_mx8], out=mx8[:, 4:5], in0=mx8[:, 3:4], scalar1=-1e29, scalar2=None, op0=ALU.max)
                V("dve", "tensor_scalar", [R_gsb, R_mx8], [R_mtmp], out=mtmp, in0=gsb, scalar1=mx8[:, 4:5], scalar2=BIG,
                  op0=ALU.is_ge, op1=ALU.mult)
                V("dve", "tensor_scalar", [R_mtmp], [R_stg], out=stg[:, sub, 64:96], in0=mtmp, scalar1=-BIG, scalar2=None, op0=ALU.add)

        def S2(i):
            Qa, R_Qa = Qaug[i % 2]
            for sub in range(4):
                TR(PT[0][0:96, sub * 128:(sub + 1) * 128], stg[:, sub, :], idb, [R_stg, R_idb], [R_PT[0][0]], inc=(sub == 3))
            V("dve", "tensor_copy", [R_PT[0][0]], [R_Qa], out=Qa[64:96, :], in_=PT[0][64:96, 0:512])

        sbi = 0
        pbi = 0
        load_head(0)
        q_load(0)
        if len(items) > 1:
            q_load(1)
        S1(0)
        S2(0)
        for i, (h, g) in enumerate(items):
            hb = h % 2
            Ka, R_Ka = Kaug[hb]
            Va, R_Va = Vaug[hb]
            Qa, R_Qa = Qaug[i % 2]
            if g == 0 and h + 1 < H:
                load_head(h + 1)
            if i + 2 < len(items):
                q_load(i + 2)
            if i + 1 < len(items):
                S1(i + 1)
            nkt = min(NKT, 8 * (g + 1))
            oacc, R_oacc = PO[i % 2], R_PO[i % 2]
            LA = 2
            slots = {}
            for it_ in range(nkt + LA):
                if it_ < nkt:
                    kt = it_
                    sps, R_sps = SB[sbi % 3]
                    sbi += 1
                    slots[kt] = (sps, R_sps)
                    MM(sps, Ka[0:96, kt * 128:(kt + 1) * 128], Qa[0:96, :], True, True, [R_Ka, R_Qa], [R_sps])
                kt = it_ - LA
                if kt >= 0:
                    sps, R_sps = slots.pop(kt)
                    pb, R_pb = pT[pbi % NPB]
                    pbi += 1
                    ACT(pb, sps, AF.Exp, [R_sps], [R_pb], scale=DH ** -0.5)
                    blk = kt // 2
                    if blk in (4 * g, 4 * g + 1):
                        c0 = (blk - 4 * g) * 256
                        V("dve", "tensor_tensor", [R_pb, R_cm], [R_pb], out=pb[:, c0:c0 + 256], in0=pb[:, c0:c0 + 256], in1=cm[:, kt % 2, :], op=ALU.mult)
                    MM(oacc[0:65, :], Va[:, kt, 0:65], pb, kt == 0, kt == nkt - 1, [R_Va, R_pb], [R_oacc], inc=True)
                if it_ == nkt // 2 and i + 1 < len(items):
                    S2(i + 1)
            V("dve", "tensor_copy", [R_oacc], [R_osb], out=osb[0:65, :], in_=oacc[0:65, :])
            for sub in range(4):
                TR(PM[:, sub * 128:sub * 128 + 65], osb[0:65, sub * 128:(sub + 1) * 128], idf[0:65, 0:65], [R_osb, R_idf], [R_PM], inc=(sub == 3))
            pm3 = PM.rearrange("p (s c) -> p s c", c=128)
            V("dve", "reciprocal", [R_PM], [R_rec], out=rec.unsqueeze(2), in_=pm3[:, :, 64:65])
            ast, R_ast = attn_st[i % 2]
            V("dve", "tensor_tensor", [R_PM, R_rec], [R_ast], out=ast, in0=pm3[:, :, 0:64], in1=rec.unsqueeze(2).to_broadcast([128, 4, 64]), op=ALU.mult)
            kb.dma("sp", attn_v[:, 4 * g:4 * g + 4, h * 64:(h + 1) * 64], ast, reads=[R_ast])
        kb.barrier()
        AR.off = MARK1
        if stop_after == 2:
            kb.emit()
            return nc

        wo_sb, R_wo = A([128, 8, 1024], BF16, "wo")
        wo32 = [A([128, 1024], F32, "wo32_%d" % i) for i in range(1)]
        at, R_at = A([128, 4, 512], F32, "at")
        an, R_an = A([128, 4, 512], BF16, "an")
        anT, R_anT = A([128, 4, 512], BF16, "anT")
        gmT, R_gmT = A([128, 4, 512], BF16, "gmT")
        obs = [A([128, 1024], F32, "ob%d" % i) for i in range(2)]
        nf_bc, R_nf = A([128, 1024], F32, "nf_bc")
        kb.dma("sp", nf_bc, nfin_d.partition_broadcast(128), writes=[R_nf])
        wout_v = wout_d.rearrange("(kc p) n -> p kc n", p=128)
        for kc in range(8):
            w32, R_w32 = wo32[0]
            kb.dma("sp", w32, wout_v[:, kc, :], writes=[R_w32])
            V("dve", "tensor_scalar", [R_w32, R_vT], [R_wo], out=wo_sb[:, kc, :], in0=w32, scalar1=vT[:, 104 + kc:105 + kc], scalar2=None, op0=ALU.mult)
        def load_at(t):
            tsl_ = slice(t * T, (t + 1) * T)
            kb.dma("sp", at, attn_scr[tsl_, :].rearrange("(s p) f -> p s f", p=128), writes=[R_at])
            kb.dma("sp", gmT, gmT_scr.rearrange("h p s -> p h s")[:, :, tsl_], writes=[R_gmT])

        load_at(0)
        for t in range(NP):
            tok0 = t * T
            tsl = slice(tok0, tok0 + T)
            kb.dma("sp", hx, h1_scr[tsl, :].rearrange("(s p) d -> p s d", p=128), writes=[R_hx])
            V("dve", "memset", [], [R_ss], ap=ss[:, 4:8], constant=0.0)
            for sub in range(4):
                ACT(junk[:, 0:512], at[:, sub, :], AF.Square, [R_at], [R_junk, R_ss], accum_out=ss[:, 4 + sub:5 + sub])
            rs = rstd_of(ss[:, 4:8], 4, 512, R_ss)
            for sub in range(4):
                V("pool", "tensor_scalar", [R_at, R_small], [R_an], out=an[:, sub, :], in0=at[:, sub, :], scalar1=rs[:, sub:sub + 1], scalar2=None, op0=ALU.mult)
            for fc in range(4):
                b, hf = fc % 2, 0
                for sub in range(4):
                    TR(PT[b][:, hf * 512 + sub * 128:hf * 512 + (sub + 1) * 128], an[:, sub, fc * 128:(fc + 1) * 128], idb, [R_an, R_idb], [R_PT[b][hf]], inc=(sub == 3))
                CPA(anT[:, fc, :], PT[b][:, hf * 512:(hf + 1) * 512], [R_PT[b][hf]], [R_anT])
            for sub in range(4):
                sl = slice(sub * 128, (sub + 1) * 128)
                for half in range(2):
                    hs = slice(half * 512, (half + 1) * 512)
                    ps, R_ps = next_po()
                    for fc in range(4):
                        MM(ps, anT[:, fc, sl], wo_sb[:, fc, hs], fc == 0, False, [R_anT, R_wo], [R_ps], inc=False)
                    for fc in range(4):
                        MM(ps, gmT[:, fc, sl], wo_sb[:, 4 + fc, hs], False, fc == 3, [R_gmT, R_wo], [R_ps], inc=(fc == 3))
                    tm, R_tm = tmpo[(sub * 2 + half) % 2]
                    V("dve", "tensor_tensor", [R_ps, R_gt], [R_tm], out=tm, in0=ps, in1=gt_bc[:, 1, hs], op=ALU.mult)
                    V("pool", "tensor_tensor", [R_tm, R_hx], [R_hx], out=hx[:, sub, hs], in0=hx[:, sub, hs], in1=tm, op=ALU.add)
            if t + 1 < NP:
                load_at(t + 1)
            ffn_pre(1)
            norm_T(2)
            wdn_load(1)
            ffn(2, 1)
            V("dve", "memset", [], [R_ss], ap=ss[:, 0:4], constant=0.0)
            for sub in range(4):
                ACT(junk, hx[:, sub, :], AF.Square, [R_hx], [R_junk, R_ss], accum_out=ss[:, sub:sub + 1])
            rs = rstd_of(ss[:, 0:4], 4, D, R_ss)
            for sub in range(4):
                ob, R_ob = obs[sub % 2]
                ACT(ob, hx[:, sub, :], AF.Identity, [R_hx, R_small], [R_ob], scale=rs[:, sub:sub + 1])
                V("dve", "tensor_tensor", [R_ob, R_nf], [R_ob], out=ob, in0=ob, in1=nf_bc, op=ALU.mult)
                kb.dma("sp", out_d[tok0 + sub * 128:tok0 + (sub + 1) * 128, :], ob, reads=[R_ob])
        kb.barrier()
        kb.emit()
    return nc


def _consts(S, r):
    half = 32
    inv_freq = (np.float32(10000.0) ** (-np.arange(half, dtype=np.float32) / np.float32(half))).astype(np.float32)
    pos = np.arange(S, dtype=np.float32)
    ang = (pos[:, None] * inv_freq[None, :]).astype(np.float32)
    cosG = np.cos(ang).astype(np.float32)
    sinG = np.sin(ang).astype(np.float32)
    NT = S // T
    NP = NT // 2
    own = [2 * p + r for p in range(NP)]
    oth = [2 * p + 1 - r for p in range(NP)]
    gat = lambda tab, tiles: np.concatenate([tab[t * T:(t + 1) * T] for t in tiles], axis=0)
    cosT = np.ascontiguousarray(np.stack([gat(cosG, own), gat(cosG, oth)], axis=0))
    sinT = np.ascontiguousarray(np.stack([gat(sinG, own), gat(sinG, oth)], axis=0))
    NB = S // BLK
    kind = np.zeros((32, S), np.float32)
    for j in range(NB):
        kind[j, j * BLK:(j + 1) * BLK] = 1.0
    k = np.arange(128)[:, None]
    q = np.arange(256)[None, :]
    cmask = np.stack([(q >= k), (q >= k + 128)], axis=1).astype(np.float32)
    nsub = (S // 2) // 128
    patt = np.full((nsub, 32), -1e30, np.float32)
    for s_ in range(nsub):
        p, ib = s_ // 4, (s_ % 4) // 2
        L = 4 * p + ib
        G = 2 * (2 * p + r) + ib
        for j in range(NB):
            pj, which, ibj = j // 4, (j % 4) // 2, j % 2
            gt = 2 * pj + (r if which == 0 else 1 - r)
            gb = 2 * gt + ibj
            if j == L:
                patt[s_, j] = 1e30
            elif gb < G:
                patt[s_, j] = 0.0
    tt = np.arange(128)[:, None]
    s2 = np.arange(128)[None, :]
    tril = (s2 <= tt).astype(np.float32)
    return dict(ident=np.eye(128, dtype=np.float32), cosT=cosT, sinT=sinT, kind=kind, cmask=cmask,
                patt=np.ascontiguousarray(patt.reshape(1, -1)), tril=tril), own, oth


def make_in_maps(inputs, S, cores):
    f = lambda a: np.ascontiguousarray(np.asarray(a, dtype=np.float32))
    b_ada = f(inputs["b_ada"])[0]
    shared = dict(
        bgate=f(b_ada.reshape(9, D)[[2, 5, 8]]),
        nfin=f(inputs["norm_final"]).reshape(1, D),
        lng=f(inputs["gmlp_ln_g"])[0].reshape(1, 512),
        lnb=f(inputs["gmlp_ln_b"])[0].reshape(1, 512),
        w_s=f(inputs["gmlp_w_s"])[0],
        w_ada=f(inputs["w_ada"])[0],
        w_gu1=f(inputs["w_ffn1_gu"])[0], w_gu2=f(inputs["w_ffn2_gu"])[0],
        w_dn1=f(inputs["w_ffn1_down"])[0], w_dn2=f(inputs["w_ffn2_down"])[0],
        w_in=f(inputs["w_in"])[0], w_out=f(inputs["w_out"])[0],
    )
    x = f(inputs["x"])
    c = f(inputs["c"])
    cst = {r: _consts(S, r) for r in (0, 1)}
    maps = []
    for (b, r) in cores:
        vecs = np.concatenate([
            b_ada.reshape(72, 128),
            f(inputs["norm_ffn1"])[0].reshape(8, 128),
            f(inputs["norm_mix"])[0].reshape(8, 128),
            f(inputs["norm_ffn2"])[0].reshape(8, 128),
            c[b].reshape(8, 128),
            f(inputs["g_attn_out"])[0].reshape(4, 128),
            f(inputs["g_gmlp_out"])[0].reshape(4, 128),
            f(inputs["gmlp_b_s"])[0].reshape(8, 128),
        ], axis=0)
        cd, own, oth = cst[r]
        m = dict(shared)
        m.update(cd)
        xb = x[b, :S]
        gat = lambda tiles: np.concatenate([xb[t * T:(t + 1) * T] for t in tiles], axis=0)
        m["x"] = np.ascontiguousarray(np.stack([gat(own), gat(oth)], axis=0))
        m["vecs"] = np.ascontiguousarray(vecs)
        maps.append(m)
    return maps


def assemble(results, S, cores, nbatch):
    out = np.zeros((nbatch, S, D), np.float32)
    NP = (S // T) // 2
    for (b, r), res in zip(cores, results):
        o = np.asarray(res["out"], dtype=np.float32)
        for p in range(NP):
            t = 2 * p + r
            out[b, t * T:(t + 1) * T] = o[p * T:(p + 1) * T]
    return out


_NC_CACHE = {}


def kernel(**inputs):
    S = 8192
    if S not in _NC_CACHE:
        _NC_CACHE[S] = build(S)
    nc = _NC_CACHE[S]
    cores = [(c // 2, c % 2) for c in range(8)]
    maps = make_in_maps(inputs, S, cores)
    res = run_bass_kernel_spmd(nc, maps, core_ids=list(range(8)))
    return assemble(res.results, S, cores, 4)
```

```python
import contextlib
import os
import numpy as np
import concourse.bass as bass
import concourse.mybir as mybir
from concourse.bass_utils import run_bass_kernel_spmd

F32 = mybir.dt.float32
BF16 = mybir.dt.bfloat16
AF = mybir.ActivationFunctionType
ALU = mybir.AluOpType
AX = mybir.AxisListType

ENGS = ("pe", "act", "dve", "pool", "sp")
SELF_SYNC = {"pe": False, "act": True, "dve": True, "pool": True, "sp": False}


class Res:
    __slots__ = ("name", "w", "r", "dsem")

    def __init__(self, name):
        self.name = name
        self.w = {}
        self.r = {}
        self.dsem = None


class KB:
    def __init__(self, nc):
        self.nc = nc
        self.q = {e: [] for e in ENGS}
        self.cnt = {e: 0 for e in ENGS}
        self.seen = {e: {} for e in ENGS}
        self.pend = {e: ([], []) for e in ENGS}
        self.dcnt = {}
        self.sems = {}
        self.nd = 0

    def _need(self, eng, waits, sem, c):
        if sem == eng and not SELF_SYNC[eng]:
            return
        if self.seen[eng].get(sem, 0) >= c:
            return
        if waits.get(sem, 0) < c:
            waits[sem] = c

    def _deps(self, eng, reads, writes):
        waits = {}
        for r in reads:
            for s, c in r.w.items():
                self._need(eng, waits, s, c)
        for w in writes:
            for s, c in w.w.items():
                self._need(eng, waits, s, c)
            for s, c in w.r.items():
                self._need(eng, waits, s, c)
        for s, c in waits.items():
            self.q[eng].append(("wait", s, c))
            self.seen[eng][s] = c

    def _mark(self, ev, reads, writes):
        s, c = ev
        for r in reads:
            if r.r.get(s, 0) < c:
                r.r[s] = c
        for w in writes:
            w.w = {s: c}
            w.r = {}

    def op(self, eng, fn, reads=(), writes=(), inc=True):
        for r in list(reads) + list(writes):
            for e2 in ENGS:
                if e2 != eng and (r in self.pend[e2][1]):
                    raise RuntimeError("resource %s pending on %s" % (r.name, e2))
        for w in writes:
            for e2 in ENGS:
                if e2 != eng and (w in self.pend[e2][0]):
                    raise RuntimeError("resource %s pending-read on %s" % (w.name, e2))
        self._deps(eng, reads, writes)
        self.q[eng].append(("op", fn, inc))
        if inc:
            self.cnt[eng] += 1
            ev = (eng, self.cnt[eng])
            pr, pw = self.pend[eng]
            self._mark(ev, pr, pw)
            self.pend[eng] = ([], [])
            self._mark(ev, reads, writes)
        else:
            self.pend[eng][0].extend(reads)
            self.pend[eng][1].extend(writes)

    def dma(self, eng, out, in_, reads=(), writes=(), sem=None, **kw):
        if sem is None:
            sem = (list(writes) + list(reads))[0]
        if isinstance(sem, Res):
            if sem.dsem is None:
                sem.dsem = "d%d_%s" % (self.nd, sem.name)
                self.nd += 1
            sem = sem.dsem
        self._deps(eng, reads, writes)
        self.dcnt[sem] = self.dcnt.get(sem, 0) + 16
        self.q[eng].append(("dma", out, in_, sem, kw))
        self._mark((sem, self.dcnt[sem]), reads, writes)

    def barrier(self, skip_prefix=None):
        for e in ENGS:
            assert not self.pend[e][0] and not self.pend[e][1], e
        allev = [(e, self.cnt[e]) for e in ENGS if self.cnt[e] > 0]
        allev += [(k_, v_) for k_, v_ in self.dcnt.items() if not (skip_prefix and k_.startswith(skip_prefix))]
        for e in ENGS:
            for s, c in allev:
                if s == e or c == 0:
                    continue
                if self.seen[e].get(s, 0) < c:
                    self.q[e].append(("wait", s, c))
                    self.seen[e][s] = c

    def emit(self):
        nc = self.nc
        names = list(ENGS) + list(self.dcnt.keys())
        import contextlib
        with contextlib.ExitStack() as es:
            for n in names:
                self.sems[n] = es.enter_context(nc.semaphore("s_" + n))
            block = es.enter_context(nc.Block())

            def run(e, eng):
                for it in self.q[e]:
                    if it[0] == "wait":
                        eng.wait_ge(self.sems[it[1]], it[2])
                    elif it[0] == "op":
                        ins = it[1](eng)
                        if it[2]:
                            ins.then_inc(self.sems[e], 1)
                    else:
                        _, out, in_, sem, kw = it
                        eng.dma_start(out=out, in_=in_, **kw).then_inc(self.sems[sem], 16)

            @block.tensor
            def _(eng):
                run("pe", eng)

            @block.scalar
            def _(eng):
                run("act", eng)

            @block.vector
            def _(eng):
                run("dve", eng)

            @block.gpsimd
            def _(eng):
                run("pool", eng)

            @block.sync
            def _(eng):
                run("sp", eng)


D = 1024
DFF = 2816
NJ = DFF // 128
H = 8
DH = 64
T = 512
BLK = 256
EPS = 1e-6
BIG = 30000.0
CW = 53000


class Arena:
    def __init__(self, ap):
        self.ap = ap
        self.off = 0

    def alloc(self, shape, dt):
        n = 1
        for s in shape[1:]:
            n *= s
        nf = n if dt == F32 else (n + 1) // 2
        a = self.ap[:, self.off:self.off + nf]
        self.off += nf
        assert self.off <= CW, ("arena overflow", self.off)
        v = a if dt == F32 else a.bitcast(BF16)[:, 0:n]
        if len(shape) == 3:
            v = v.rearrange("p (a b) -> p a b", a=shape[1])
        elif len(shape) == 4:
            v = v.rearrange("p (a b c) -> p a b c", a=shape[1], b=shape[2])
        return v


def build(S, debug=False, stop_after=9):
    NT = S // T
    NP = NT // 2
    SO = S // 2
    NKT = S // 128
    NB = S // BLK
    nc = bass.Bass("TRN2", target_bir_lowering=False)

    def din(name, shape, dt=F32):
        return nc.dram_tensor(name, list(shape), dt, kind="ExternalInput").ap()

    def dscr(name, shape, dt, dbg=False):
        kind = "ExternalOutput" if (debug and dbg) else "Internal"
        return nc.dram_tensor(name, list(shape), dt, kind=kind).ap()

    x_d = din("x", [2, SO, D])
    vecs_d = din("vecs", [120, 128])
    bgate_d = din("bgate", [3, D])
    nfin_d = din("nfin", [1, D])
    lng_d = din("lng", [1, 512])
    lnb_d = din("lnb", [1, 512])
    ws_d = din("w_s", [8, 128, 128])
    wada_d = din("w_ada", [D, 9 * D])
    wgu_d = [din("w_gu1", [D, 2 * DFF]), din("w_gu2", [D, 2 * DFF])]
    wdn_d = [din("w_dn1", [DFF, D]), din("w_dn2", [DFF, D])]
    win_d = din("w_in", [D, 2560])
    wout_d = din("w_out", [D, D])
    ident_d = din("ident", [128, 128])
    cos_d = din("cosT", [2, SO, 32])
    sin_d = din("sinT", [2, SO, 32])
    kind_d = din("kind", [32, S])
    cm_d = din("cmask", [128, 2, 256])
    patt_d = din("patt", [1, (SO // 128) * 32])
    tril_d = din("tril", [128, 128])
    out_d = nc.dram_tensor("out", [SO, D], F32, kind="ExternalOutput").ap()

    wgu_b = [dscr("wgu1_b", [D, 2 * DFF], BF16), dscr("wgu2_b", [D, 2 * DFF], BF16)]
    wdn_b = [dscr("wdn1_b", [DFF, D], BF16), dscr("wdn2_b", [DFF, D], BF16)]
    win_b = dscr("win_b", [D, 2560], BF16)
    h1_scr = dscr("h1_scr", [SO, D], F32, True)
    q_scr = dscr("q_scr", [4, 128, SO], F32, True)
    k_scr = dscr("k_scr", [4, 128, S], BF16)
    v_scr = dscr("v_scr", [8, 128, NKT, 65], BF16)
    gmT_scr = dscr("gmT_scr", [4, 128, SO], BF16)
    km_scr = dscr("km_scr", [4, 128, NB], F32, True)
    attn_scr = dscr("attn_scr", [SO, 512], F32, True)

    es = contextlib.ExitStack()
    with es:
        arena_t = es.enter_context(nc.sbuf_tensor("arena", [128, CW], F32))
        AR = Arena(arena_t[:, :])
        PGU = [es.enter_context(nc.psum_tensor("pgu%d" % i, [128, 512], F32))[:, :] for i in range(4)]
        PO = [es.enter_context(nc.psum_tensor("po%d" % i, [128, 512], F32))[:, :] for i in range(2)]
        PT = [es.enter_context(nc.psum_tensor("pt%d" % i, [128, 1024], BF16))[:, :] for i in range(2)]
        R_PGU = [Res("pgu%d" % i) for i in range(4)]
        R_PO = [Res("po%d" % i) for i in range(2)]
        R_PT = []
        for i in range(2):
            r_ = Res("pt%d" % i)
            R_PT.append([r_, r_])
        kb = KB(nc)

        def MM(out, lhsT, rhs, st, sp, R=(), W=(), inc=True):
            kb.op("pe", lambda e: e.matmul(out, lhsT=lhsT, rhs=rhs, start=st, stop=sp), R, W, inc)

        def TR(out, in_, ident, R=(), W=(), inc=True):
            kb.op("pe", lambda e: e.transpose(out=out, in_=in_, identity=ident), R, W, inc)

        def ACT(out, in_, func, R=(), W=(), **kw):
            kb.op("act", lambda e: e.activation(out=out, in_=in_, func=func, **kw), R, W)

        def CPA(out, in_, R=(), W=()):
            kb.op("act", lambda e: e.copy(out=out, in_=in_), R, W)

        def V(eng, name, R=(), W=(), **kw):
            if eng == "pool":
                eng = "dve"
            kb.op(eng, lambda e: getattr(e, name)(**kw), R, W)

        po_i = [0]

        def next_po():
            i = po_i[0] % 2
            po_i[0] += 1
            return PO[i], R_PO[i]

        def A(shape, dt, name):
            return AR.alloc(shape, dt), Res(name)

        idf, R_idf = A([128, 128], F32, "idf")
        idb, R_idb = A([128, 128], BF16, "idb")
        vT, R_vT = A([128, 120], F32, "vT")
        modT, R_modT = A([128, 72], F32, "modT")
        Asc, R_Asc = A([128, 3, 8], F32, "Asc")
        gt_bc, R_gt = A([128, 3, 1024], F32, "gt_bc")
        lng_bc, R_lng = A([128, 512], F32, "lng")
        lnb_bc, R_lnb = A([128, 512], F32, "lnb")
        WsT, R_WsT = A([128, 8, 128], BF16, "WsT")
        patt, R_patt = A([128, SO // 128, 32], F32, "patt")
        cm, R_cm = A([128, 2, 256], BF16, "cm")
        onesc, R_onesc = A([128, 2], F32, "onesc")
        kms, R_kms = A([128, 4, NKT], F32, "kms")
        small, R_small = A([128, 64], F32, "small")
        MARK_P = AR.off
        hx, R_hx = A([128, 4, 1024], F32, "hx")
        tb, R_tb = A([128, 4, 1024], BF16, "tb")
        yT, R_yT = A([128, 8, 512], BF16, "yT")
        aT, R_aT = A([128, NJ, 512], BF16, "aT")
        NWG = 2
        wg = [A([128, 8, 2, 256], BF16, "wg%d" % i) for i in range(NWG)]
        wdn, R_wdn = A([128, NJ, 1024], BF16, "wdn")
        sg = [A([128, 512], F32, "sg%d" % i) for i in range(2)]
        tmpo = [A([128, 512], F32, "tmpo%d" % i) for i in range(2)]
        ss, R_ss = A([128, 16], F32, "ss")
        nsm, _ = A([128, 32], F32, "nsm")
        ssq, R_ssq = A([128, 8], F32, "ssq")
        R_nsq = [Res("nsq%d" % i) for i in range(4)]
        junk, R_junk = A([128, 1024], BF16, "junk")
        MARK0 = AR.off
        MARK1 = MARK0

        kb.dma("sp", idf, ident_d, writes=[R_idf])
        V("dve", "tensor_copy", [R_idf], [R_idb], out=idb, in_=idf)
        R_wgub = [Res("wgu1b"), Res("wgu2b")]
        R_wdnb = [Res("wdn1b"), Res("wdn2b")]
        R_winb = Res("winb")

        def cast_w(dst, src, rows, step, R, sem):
            for r0 in range(0, rows, step):
                kb.dma("pool", dst[r0:r0 + step, :], src[r0:r0 + step, :], writes=[R], sem=sem)

        cast_w(wgu_b[0], wgu_d[0], D, 128, R_wgub[0], "pre_gu1")
        kb.dma("pool", cm, cm_d, writes=[R_cm])

        vecs_sb, R_vecs = A([128, 128], F32, "vecs")
        kb.dma("sp", vecs_sb[0:120, :], vecs_d, writes=[R_vecs])
        kb.dma("sp", lng_bc, lng_d.partition_broadcast(128), writes=[R_lng])
        kb.dma("sp", lnb_bc, lnb_d.partition_broadcast(128), writes=[R_lnb])
        kb.dma("sp", patt.rearrange("p a b -> p (a b)"), patt_d.partition_broadcast(128), writes=[R_patt])
        bg_bc, R_bg = A([128, 3, 1024], F32, "bg_bc")
        for gi in range(3):
            kb.dma("sp", bg_bc[:, gi, :], bgate_d[gi:gi + 1, :].partition_broadcast(128), writes=[R_bg])
        V("dve", "memset", [], [R_onesc], ap=onesc, constant=1.0 / BLK)
        TR(PO[0][:, 0:120], vecs_sb[0:120, :], idf[0:120, 0:120], [R_vecs, R_idf], [R_PO[0]])
        CPA(vT, PO[0][:, 0:120], [R_PO[0]], [R_vT])
        cact, R_cact = A([128, 8], F32, "cact")
        cT, R_cT = A([128, 8], BF16, "cT")
        crep, R_crep = A([128, 8, 128], BF16, "crep")
        ACT(cact, vT[:, 96:104], AF.Silu, [R_vT], [R_cact])
        V("dve", "tensor_copy", [R_cact], [R_cT], out=cT, in_=cact)
        for kc in range(8):
            V("dve", "tensor_copy", [R_cT], [R_crep], out=crep[:, kc, :],
              in_=cT[:, kc:kc + 1].to_broadcast([128, 128]))
        for gi in range(3):
            V("dve", "tensor_scalar", [R_bg], [R_bg], out=bg_bc[:, gi, :], in0=bg_bc[:, gi, :],
              scalar1=(1.0 if gi == 1 else 0.5), scalar2=None, op0=ALU.mult)
        wa = [A([128, 8, 512], BF16, "wa%d" % i) for i in range(4)]
        wada_v = wada_d.rearrange("(kc p) n -> p kc n", p=128)
        PMOD, R_PMOD = PGU[0], R_PGU[0]
        for cg in range(18):
            wab, R_wab = wa[cg % 4]
            for kh in range(2):
                kb.dma("pool", wab[:, 4 * kh:4 * kh + 4, :], wada_v[:, 4 * kh:4 * kh + 4, cg * 512:(cg + 1) * 512],
                       writes=[R_wab])
            for cc in range(4):
                col = cg * 4 + cc
                for kc in range(8):
                    MM(PMOD[:, col:col + 1], wab[:, kc, cc * 128:(cc + 1) * 128], cT[:, kc:kc + 1],
                       kc == 0, kc == 7, [R_wab, R_cT], [R_PMOD], inc=(kc == 7))
            v = cg // 2
            if v in (2, 5, 8):
                gi = (v - 2) // 3
                half = cg % 2
                ps, R_ps = PGU[2 + half], R_PGU[2 + half]
                for kc in range(8):
                    MM(ps, crep[:, kc, :], wab[:, kc, :], kc == 0, kc == 7, [R_wab, R_crep], [R_ps], inc=(kc == 7))
                V("dve", "scalar_tensor_tensor", [R_ps, R_bg], [R_gt], out=gt_bc[:, gi, half * 512:(half + 1) * 512],
                  in0=ps, scalar=(1.0 if gi == 1 else 0.5), in1=bg_bc[:, gi, half * 512:(half + 1) * 512],
                  op0=ALU.mult, op1=ALU.add)
        cast_w(wdn_b[0], wdn_d[0], DFF, 256, R_wdnb[0], "pre_dn1")
        cast_w(win_b, win_d, D, 256, R_winb, "pre_in")
        cast_w(wgu_b[1], wgu_d[1], D, 128, R_wgub[1], "pre_gu2")
        cast_w(wdn_b[1], wdn_d[1], DFF, 256, R_wdnb[1], "pre_dn2")
        V("dve", "tensor_tensor", [R_PMOD, R_vT], [R_modT], out=modT, in0=PMOD[:, 0:72], in1=vT[:, 0:72], op=ALU.add)
        for k in range(3):
            V("dve", "scalar_tensor_tensor", [R_modT, R_vT], [R_Asc], out=Asc[:, k, :],
              in0=modT[:, (3 * k + 1) * 8:(3 * k + 1) * 8 + 8], scalar=1.0, in1=vT[:, 72 + 8 * k:80 + 8 * k],
              op0=ALU.add, op1=ALU.mult)
        ws32, R_ws32 = A([128, 8, 128], F32, "ws32")
        trilm, R_tril = A([128, 128], F32, "tril")
        wsm, R_wsm = A([128, 8, 128], BF16, "wsm")
        kb.dma("sp", ws32, ws_d.rearrange("h t s -> t h s"), writes=[R_ws32])
        kb.dma("sp", trilm, tril_d, writes=[R_tril])
        for h in range(8):
            V("dve", "tensor_tensor", [R_ws32, R_tril], [R_wsm], out=wsm[:, h, :], in0=ws32[:, h, :], in1=trilm, op=ALU.mult)
        for h in range(8):
            TR(PT[0][:, h * 128:(h + 1) * 128], wsm[:, h, :], idb, [R_wsm, R_idb], [R_PT[0][0], R_PT[0][1]], inc=(h == 7))
        V("dve", "tensor_copy", [R_PT[0][0], R_PT[0][1]], [R_WsT], out=WsT.rearrange("p h t -> p (h t)"), in_=PT[0][:, 0:1024])
        kb.barrier()
        AR.off = MARK0
        if stop_after == 0:
            kb.emit()
            return nc

        def rstd_of(ss_ap, n, nfeat, R_in):
            V("dve", "tensor_scalar", [R_in], [R_small], out=small[:, 16:16 + n], in0=ss_ap, scalar1=1.0 / nfeat,
              scalar2=EPS, op0=ALU.mult, op1=ALU.add)
            ACT(small[:, 32:32 + n], small[:, 16:16 + n], AF.Sqrt, [R_small], [R_small])
            V("dve", "reciprocal", [R_small], [R_small], out=small[:, 0:n], in_=small[:, 32:32 + n])
            return small[:, 0:n]

        def norm_T(k, pre=False):
            norm_stats(pre)
            norm_tr(k)

        def ssq_reset():
            V("dve", "memset", [], [R_ssq], ap=ssq, constant=0.0)

        def ssq_acc(sub, half):
            hs_ = slice(half * 512, (half + 1) * 512)
            ACT(junk[:, 0:512], hx[:, sub, hs_], AF.Square, [R_hx, R_ssq], [R_junk, R_ssq], accum_out=ssq[:, 2 * sub + half:2 * sub + half + 1])

        def ssq_sum():
            sq4 = ssq.rearrange("p (s two) -> p s two", two=2)
            V("dve", "tensor_tensor", [R_ssq], [R_ss], out=ss[:, 0:4].unsqueeze(2), in0=sq4[:, :, 0:1], in1=sq4[:, :, 1:2], op=ALU.add)

        def norm_stats(pre=False):
            KD = 9
            R_sq = [R_nsq[i] for i in range(4)]
            if pre:
                ssq_sum()
            else:
                V("dve", "memset", [], [R_ss], ap=ss[:, 0:4], constant=0.0)

            def sq(sub):
                if not pre:
                    ACT(junk, hx[:, sub, :], AF.Square, [R_hx, R_ss], [R_junk, R_sq[sub]], accum_out=ss[:, sub:sub + 1])

            sq(0)
            for sub in range(4):
                if sub + 1 < 4:
                    sq(sub + 1)
                c0 = 16 + sub
                V("dve", "tensor_scalar", [R_sq[sub], R_ss], [R_sq[sub]], out=nsm[:, c0:c0 + 1], in0=ss[:, sub:sub + 1], scalar1=1.0 / D,
                  scalar2=EPS, op0=ALU.mult, op1=ALU.add)
                ACT(nsm[:, 8 + sub:9 + sub], nsm[:, c0:c0 + 1], AF.Sqrt, [R_sq[sub]], [R_sq[sub]])
                V("dve", "reciprocal", [R_sq[sub]], [R_sq[sub]], out=nsm[:, sub:sub + 1], in_=nsm[:, 8 + sub:9 + sub])
                if sub % 2 == 0:
                    V("dve", "tensor_scalar", [R_hx, R_sq[sub]], [R_tb], out=tb[:, sub, :], in0=hx[:, sub, :],
                      scalar1=nsm[:, sub:sub + 1], scalar2=None, op0=ALU.mult)
                else:
                    ACT(tb[:, sub, :], hx[:, sub, :], AF.Identity, [R_hx, R_sq[sub]], [R_tb], scale=nsm[:, sub:sub + 1])

        def norm_tr(k):
            KD = 9
            for c in range(8):
                b, hf = c % 2, 0
                for sub in range(4):
                    TR(PT[b][:, hf * 512 + sub * 128: hf * 512 + (sub + 1) * 128], tb[:, sub, c * 128:(c + 1) * 128], idb,
                       [R_tb, R_idb], [R_PT[b][hf]], inc=(sub == 3))
                if KD == 3:
                    continue
                if c % 2 == 0:
                    ACT(yT[:, c, :], PT[b][:, hf * 512:(hf + 1) * 512], AF.Identity, [R_PT[b][hf], R_Asc, R_modT], [R_yT],
                        scale=Asc[:, k, c:c + 1], bias=modT[:, 3 * k * 8 + c:3 * k * 8 + c + 1])
                else:
                    V("dve", "tensor_scalar", [R_PT[b][hf], R_Asc, R_modT], [R_yT], out=yT[:, c, :], in0=PT[b][:, hf * 512:(hf + 1) * 512],
                      scalar1=Asc[:, k, c:c + 1], scalar2=modT[:, 3 * k * 8 + c:3 * k * 8 + c + 1], op0=ALU.mult, op1=ALU.add)

        def wg_load(f, jp):
            wgu_v = wgu_b[f].rearrange("(kc p) (two n) -> p kc two n", p=128, two=2)
            wgb, R_wgb = wg[jp % NWG]
            for two in range(2):
                kb.dma("sp", wgb[:, :, two, :], wgu_v[:, :, two, jp * 256:(jp + 1) * 256], reads=[R_wgub[f]], writes=[R_wgb])

        def ffn_pre(f):
            for jp in range(NWG):
                wg_load(f, jp)

        def wdn_load(f):
            wdn_v = wdn_b[f].rearrange("(j p) d -> p j d", p=128)
            ssq_reset()
            for jh in range(2):
                kb.dma("sp", wdn[:, 11 * jh:11 * jh + 11, :], wdn_v[:, 11 * jh:11 * jh + 11, :], reads=[R_wdnb[f]], writes=[R_wdn])

        def ffn(k, f):
            for jp in range(NJ // 2):
                wgb, R_wgb = wg[jp % NWG]
                if jp >= NWG:
                    wg_load(f, jp)
                for jl in range(2):
                    jj = 2 * jp + jl
                    s = jj % 2
                    pg, R_pg = PGU[2 * s], R_PGU[2 * s]
                    pu, R_pu = PGU[2 * s + 1], R_PGU[2 * s + 1]
                    for kc in range(8):
                        MM(pg, wgb[:, kc, 0, jl * 128:(jl + 1) * 128], yT[:, kc, :], kc == 0, kc == 7, [R_wgb, R_yT], [R_pg], inc=(kc == 7))
                    for kc in range(8):
                        MM(pu, wgb[:, kc, 1, jl * 128:(jl + 1) * 128], yT[:, kc, :], kc == 0, kc == 7, [R_wgb, R_yT], [R_pu], inc=(kc == 7))
                    sgb, R_sgb = sg[s]
                    ACT(sgb, pg, AF.Silu, [R_pg], [R_sgb])
                    V("dve", "tensor_tensor", [R_sgb, R_pu], [R_aT], out=aT[:, jj, :], in0=sgb, in1=pu, op=ALU.mult)
            for sub in range(4):
                for half in range(2):
                    ps, R_ps = next_po()
                    for jj in range(NJ):
                        MM(ps, aT[:, jj, sub * 128:(sub + 1) * 128], wdn[:, jj, half * 512:(half + 1) * 512], jj == 0, jj == NJ - 1,
                           [R_aT, R_wdn], [R_ps], inc=(jj == NJ - 1))
                    tm, R_tm = tmpo[(sub * 2 + half) % 2]
                    V("dve", "tensor_tensor", [R_ps, R_gt], [R_tm], out=tm, in0=ps, in1=gt_bc[:, k, half * 512:(half + 1) * 512], op=ALU.mult)
                    V("pool", "tensor_tensor", [R_tm, R_hx], [R_hx], out=hx[:, sub, half * 512:(half + 1) * 512],
                      in0=hx[:, sub, half * 512:(half + 1) * 512], in1=tm, op=ALU.add)
                    ssq_acc(sub, half)

        wi = [A([128, 8, 512], BF16, "wi%d" % i) for i in range(2)]
        win_v = win_b.rearrange("(kc p) n -> p kc n", p=128)
        cosb, R_cos = A([128, 4, 32], F32, "cosb")
        sinb, R_sin = A([128, 4, 32], F32, "sinb")
        qr, R_qr = A([128, 8, 2, 32], F32, "qr")
        rtmp, R_rtmp = A([128, 8, 32], F32, "rtmp")
        k16, R_k16 = A([128, 512], BF16, "k16")
        qT_st, R_qTst = A([128, 4, 512], F32, "qTst")
        kT_st, R_kTst = A([128, 4, 512], BF16, "kTst")
        v_st, R_vst = A([128, 4, 8, 65], BF16, "vst")
        V("dve", "memset", [], [R_vst], ap=v_st, constant=1.0)
        gmT_st, R_gmTst = A([128, 4, 512], BF16, "gmTst")
        gug, R_gug = A([128, 4, 512], F32, "gug")
        gvg, R_gvg = A([128, 512], F32, "gvg")
        vn, R_vn = A([128, 512], BF16, "vn")
        gm, R_gm = A([128, 512], F32, "gm")
        gmn, R_gmn = A([128, 512], BF16, "gmn")
        bsT = vT[:, 112:120]
        wi_i = [0]

        def rope(ps, dst, R_ps, R_dst, sub):
            p4 = ps.rearrange("p (h t d) -> p h t d", h=8, t=2)
            cb = cosb[:, sub:sub + 1, :].to_broadcast([128, 8, 32])
            sb_ = sinb[:, sub:sub + 1, :].to_broadcast([128, 8, 32])
            V("dve", "tensor_tensor", [R_ps, R_cos], [R_dst], out=dst[:, :, 0, :], in0=p4[:, :, 0, :], in1=cb, op=ALU.mult)
            V("dve", "tensor_tensor", [R_ps, R_sin], [R_rtmp], out=rtmp, in0=p4[:, :, 1, :], in1=sb_, op=ALU.mult)
            V("dve", "tensor_tensor", [R_dst, R_rtmp], [R_dst], out=dst[:, :, 0, :], in0=dst[:, :, 0, :], in1=rtmp, op=ALU.subtract)
            V("dve", "tensor_tensor", [R_ps, R_cos], [R_dst], out=dst[:, :, 1, :], in0=p4[:, :, 1, :], in1=cb, op=ALU.mult)
            V("dve", "tensor_tensor", [R_ps, R_sin], [R_rtmp], out=rtmp, in0=p4[:, :, 0, :], in1=sb_, op=ALU.mult)
            V("dve", "tensor_tensor", [R_dst, R_rtmp], [R_dst], out=dst[:, :, 1, :], in0=dst[:, :, 1, :], in1=rtmp, op=ALU.add)

        ALLPO = [(PO[0], R_PO[0]), (PO[1], R_PO[1])] + [(PGU[i], R_PGU[i]) for i in range(4)]
        po6 = [0]

        def next_po6():
            i = po6[0] % 6
            po6[0] += 1
            return ALLPO[i]

        def load_x(t):
            pr, which = t // 2, t % 2
            kb.dma("sp", hx, x_d[which, pr * T:(pr + 1) * T, :].rearrange("(s p) d -> p s d", p=128), writes=[R_hx])

        def load_cs(t):
            pr, which = t // 2, t % 2
            kb.dma("sp", cosb, cos_d[which, pr * T:(pr + 1) * T, :].rearrange("(s p) d -> p s d", p=128), writes=[R_cos])
            kb.dma("sp", sinb, sin_d[which, pr * T:(pr + 1) * T, :].rearrange("(s p) d -> p s d", p=128), writes=[R_sin])

        def stage_b(t, grp, sub, ps, R_ps):
            sl = slice(sub * 128, (sub + 1) * 128)
            st_i = t * 4 + sub
            if grp == 0:
                rope(ps, qr, R_ps, R_qr, sub)
                qf = qr.rearrange("p h t d -> p (h t d)")
                ps2, R_ps2 = next_po6()
                for hp in range(4):
                    TR(ps2[:, hp * 128:(hp + 1) * 128], qf[:, hp * 128:(hp + 1) * 128], idf, [R_qr, R_idf], [R_ps2], inc=(hp == 3))
                CPA(qT_st[:, :, sl], ps2.rearrange("p (h s) -> p h s", h=4), [R_ps2], [R_qTst])
            elif grp == 1:
                rope(ps, qr, R_ps, R_qr, sub)
                kf = qr.rearrange("p h t d -> p (h t d)")
                ps2, R_ps2 = next_po6()
                for hp in range(4):
                    MM(ps2[:, hp:hp + 1], kf[:, hp * 128:(hp + 1) * 128], onesc[:, 0:1], True, True, [R_qr, R_onesc], [R_ps2], inc=(hp == 3))
                V("dve", "tensor_copy", [R_ps2], [R_kms], out=kms[:, :, st_i], in_=ps2[:, 0:4])
                V("dve", "tensor_copy", [R_qr], [R_k16], out=k16, in_=kf)
                for hp in range(4):
                    TR(PT[0][:, hp * 128:(hp + 1) * 128], k16[:, hp * 128:(hp + 1) * 128], idb, [R_k16, R_idb], [R_PT[0][0]], inc=(hp == 3))
                CPA(kT_st[:, :, sl], PT[0][:, 0:512].rearrange("p (h s) -> p h s", h=4), [R_PT[0][0]], [R_kTst])
            elif grp == 2:
                CPA(v_st[:, sub, :, 0:64], ps.rearrange("p (h d) -> p h d", h=8), [R_ps], [R_vst])
            elif grp == 3:
                ACT(gug[:, sub, :], ps, AF.Gelu_apprx_tanh, [R_ps], [R_gug])
            else:
                V("dve", "memset", [], [R_ss], ap=ss[:, 4:8], constant=0.0)
                ACT(gvg, ps, AF.Gelu_apprx_tanh, [R_ps], [R_gvg, R_ss], accum_out=ss[:, 4:5])
                ACT(junk[:, 0:512], gvg, AF.Square, [R_gvg], [R_junk, R_ss], accum_out=ss[:, 5:6])
                V("dve", "tensor_scalar", [R_ss], [R_ss], out=ss[:, 8:9], in0=ss[:, 4:5], scalar1=1.0 / 512, scalar2=None, op0=ALU.mult)
                V("dve", "tensor_tensor", [R_ss], [R_ss], out=ss[:, 9:10], in0=ss[:, 8:9], in1=ss[:, 8:9], op=ALU.mult)
                V("dve", "scalar_tensor_tensor", [R_ss], [R_ss], out=ss[:, 10:11], in0=ss[:, 5:6], scalar=1.0 / 512, in1=ss[:, 9:10],
                  op0=ALU.mult, op1=ALU.subtract)
                V("dve", "tensor_scalar", [R_ss], [R_ss], out=ss[:, 11:12], in0=ss[:, 10:11], scalar1=EPS, scalar2=None, op0=ALU.add)
                ACT(ss[:, 12:13], ss[:, 11:12], AF.Sqrt, [R_ss], [R_ss])
                V("dve", "reciprocal", [R_ss], [R_ss], out=ss[:, 13:14], in_=ss[:, 12:13])
                V("dve", "scalar_tensor_tensor", [R_ss], [R_ss], out=ss[:, 14:15], in0=ss[:, 8:9], scalar=-1.0, in1=ss[:, 13:14],
                  op0=ALU.mult, op1=ALU.mult)
                ACT(gvg, gvg, AF.Identity, [R_gvg, R_ss], [R_gvg], scale=ss[:, 13:14], bias=ss[:, 14:15])
                V("dve", "tensor_tensor", [R_gvg, R_lng], [R_gvg], out=gvg, in0=gvg, in1=lng_bc, op=ALU.mult)
                V("dve", "tensor_tensor", [R_gvg, R_lnb], [R_vn], out=vn, in0=gvg, in1=lnb_bc, op=ALU.add)
                ps3, R_ps3 = next_po6()
                for hg in range(8):
                    MM(ps3[:, hg * 64:(hg + 1) * 64], WsT[:, hg, :], vn[:, hg * 64:(hg + 1) * 64], True, True, [R_WsT, R_vn], [R_ps3], inc=(hg == 7))
                V("dve", "tensor_tensor", [R_ps3, R_vT], [R_gm], out=gm.rearrange("p (h d) -> p h d", h=8),
                  in0=ps3.rearrange("p (h d) -> p h d", h=8), in1=bsT.unsqueeze(2).to_broadcast([128, 8, 64]), op=ALU.add)
                V("dve", "tensor_tensor", [R_gm, R_gug], [R_gm], out=gm, in0=gm, in1=gug[:, sub, :], op=ALU.mult)
                ACT(junk[:, 0:512], gm, AF.Square, [R_gm], [R_junk, R_ss], accum_out=ss[:, 6:7])
                rs = rstd_of(ss[:, 6:7], 1, 512, R_ss)
                V("dve", "tensor_scalar", [R_gm, R_small], [R_gmn], out=gmn, in0=gm, scalar1=rs[:, 0:1], scalar2=None, op0=ALU.mult)
                for fc in range(4):
                    TR(PT[1][:, fc * 128:(fc + 1) * 128], gmn[:, fc * 128:(fc + 1) * 128], idb, [R_gmn, R_idb], [R_PT[1][0]], inc=(fc == 3))
                CPA(gmT_st[:, :, sl], PT[1][:, 0:512].rearrange("p (h s) -> p h s", h=4), [R_PT[1][0]], [R_gmTst])

        load_x(0)
        load_cs(0)
        for t in range(NT):
            pr, which = t // 2, t % 2
            own_t = (which == 0)
            tok0 = t * T
            tsl = slice(tok0, tok0 + T)
            osl = slice(pr * T, (pr + 1) * T)
            ffn_pre(0)
            if t == 0:
                norm_stats()
            norm_tr(0)
            wdn_load(0)
            ffn(0, 0)
            if own_t:
                kb.dma("sp", h1_scr[osl, :].rearrange("(s p) d -> p s d", p=128), hx, reads=[R_hx])
            grps = list(range(5)) if own_t else [1, 2]

            def wi_load(grp_):
                wib_, R_wib_ = wi[wi_i[0] % 2]
                wi_i[0] += 1
                for kh in range(2):
                    kb.dma("sp", wib_[:, 4 * kh:4 * kh + 4, :], win_v[:, 4 * kh:4 * kh + 4, grp_ * 512:(grp_ + 1) * 512], reads=[R_winb], writes=[R_wib_])
                return wib_, R_wib_

            nxt_w = wi_load(grps[0])
            norm_T(1, pre=True)
            if t + 1 < NT:
                load_x(t + 1)
            for gi_, grp in enumerate(grps):
                wib, R_wib = nxt_w
                if gi_ + 1 < len(grps):
                    nxt_w = wi_load(grps[gi_ + 1])
                if gi_ == len(grps) - 1 and t + 1 < NT:
                    norm_stats()
                pend = None
                for sub in range(4):
                    sl = slice(sub * 128, (sub + 1) * 128)
                    ps, R_ps = next_po6()
                    for kc in range(8):
                        MM(ps, yT[:, kc, sl], wib[:, kc, :], kc == 0, kc == 7, [R_yT, R_wib], [R_ps], inc=(kc == 7))
                    if pend is not None:
                        stage_b(t, grp, *pend)
                    pend = (sub, ps, R_ps)
                stage_b(t, grp, *pend)
                if grp == 0:
                    kb.dma("sp", q_scr.rearrange("h p s -> p h s")[:, :, osl], qT_st, reads=[R_qTst])
                elif grp == 1:
                    kb.dma("sp", k_scr.rearrange("h p s -> p h s")[:, :, tsl], kT_st, reads=[R_kTst])
                    if t + 1 < NT:
                        load_cs(t + 1)
                elif grp == 2:
                    for sub_ in range(4):
                        kb.dma("act", v_scr.rearrange("h p k d -> p k h d")[:, 4 * t + sub_, :, :], v_st[:, sub_, :, :], reads=[R_vst])
                elif grp == 4:
                    kb.dma("sp", gmT_scr.rearrange("h p s -> p h s")[:, :, osl], gmT_st, reads=[R_gmTst])
        kmv = kms.rearrange("p h (b two) -> p h b two", two=2)
        kmb, R_kmb = A([128, 4, NB], F32, "kmb")
        V("dve", "tensor_tensor", [R_kms], [R_kmb], out=kmb, in0=kmv[:, :, :, 0], in1=kmv[:, :, :, 1], op=ALU.add)
        kb.dma("sp", km_scr.rearrange("h p b -> p h b"), kmb, reads=[R_kmb])
        kb.barrier()
        AR.off = MARK1
        if stop_after == 1:
            kb.emit()
            return nc

        NG = NP
        AR.off = MARK_P
        Kaug = [A([128, S], BF16, "kaug%d" % i) for i in range(2)]
        Vaug = [A([128, NKT, 65], BF16, "vaug%d" % i) for i in range(2)]
        kmh = [A([128, NB], F32, "kmh%d" % i) for i in range(2)]
        Q32 = [A([128, 512], F32, "q32_%d" % i) for i in range(3)]
        Qaug = [A([128, 512], BF16, "qaug%d" % i) for i in range(2)]
        NPB = 3
        pT = [A([128, 512], BF16, "pT%d" % i) for i in range(NPB)]
        osb, R_osb = A([128, 512], F32, "osb")
        stg, R_stg = A([128, 4, 96], BF16, "stg")
        gsb, R_gsb = A([128, 32], F32, "gsb")
        mx8, R_mx8 = A([128, 8], F32, "mx8")
        mtmp, R_mtmp = A([128, 32], F32, "mtmp")
        rec, R_rec = A([128, 4], F32, "rec")
        attn_st = [A([128, 4, 64], F32, "attnst%d" % i) for i in range(2)]
        V("dve", "memset", [], [R_stg], ap=stg, constant=0.0)
        V("dve", "memset", [], [R_gsb], ap=gsb, constant=-1e30)
        for i in range(2):
            kb.dma("pool", Kaug[i][0][64:96, :], kind_d, writes=[Kaug[i][1]], sem="kind%d" % i)
        SB = [(PGU[0], R_PGU[0]), (PGU[1], R_PGU[1]), (PGU[2], R_PGU[2])]
        PM, R_PM = PGU[3], R_PGU[3]
        attn_v = attn_scr.rearrange("(s p) f -> p s f", p=128)

        def load_head(h):
            hp, r0 = h // 2, (h % 2) * 64
            hb = h % 2
            kb.dma("act", Kaug[hb][0][0:64, :], k_scr[hp, r0:r0 + 64, :], writes=[Kaug[hb][1]])
            kb.dma("act", Vaug[hb][0], v_scr[h], writes=[Vaug[hb][1]])
            kb.dma("act", kmh[hb][0][0:64, :], km_scr[hp, r0:r0 + 64, :], writes=[kmh[hb][1]])

        items = [(h, g) for h in range(H) for g in range(NG)]

        def q_load(i):
            h, g = items[i]
            hp, r0 = h // 2, (h % 2) * 64
            Q3, R_Q3 = Q32[i % 3]
            kb.dma("sp", Q3[0:64, :], q_scr[hp, r0:r0 + 64, g * 512:(g + 1) * 512], writes=[R_Q3])

        def S1(i):
            h, g = items[i]
            km, R_km = kmh[h % 2]
            Q3, R_Q3 = Q32[i % 3]
            Qa, R_Qa = Qaug[i % 2]
            V("dve", "tensor_copy", [R_Q3], [R_Qa], out=Qa[0:64, :], in_=Q3[0:64, :])
            for sub in range(4):
                MM(PM[:, sub * 32:sub * 32 + NB], Q3[0:64, sub * 128:(sub + 1) * 128], km[0:64, 0:NB], True, True, [R_Q3, R_km], [R_PM], inc=(sub == 3))
            for sub in range(4):
                V("dve", "tensor_tensor", [R_PM, R_patt], [R_gsb], out=gsb[:, 0:NB], in0=PM[:, sub * 32:sub * 32 + NB], in1=patt[:, 4 * g + sub, 0:NB], op=ALU.add)
                V("dve", "max", [R_gsb], [R_mx8], out=mx8, in_=gsb)
                V("dve", "tensor_scalar", [R_mx8], [R_mx8], out=mx8[:, 4:5], in0=mx8[:, 3:4], scalar1=-1e29, scalar2=None, op0=ALU.max)
                V("dve", "tensor_scalar", [R_gsb, R_mx8], [R_mtmp], out=mtmp, in0=gsb, scalar1=mx8[:, 4:5], scalar2=BIG,
                  op0=ALU.is_ge, op1=ALU.mult)
                V("dve", "tensor_scalar", [R_mtmp], [R_stg], out=stg[:, sub, 64:96], in0=mtmp, scalar1=-BIG, scalar2=None, op0=ALU.add)

        def S2(i):
            Qa, R_Qa = Qaug[i % 2]
            for sub in range(4):
                TR(PT[0][0:96, sub * 128:(sub + 1) * 128], stg[:, sub, :], idb, [R_stg, R_idb], [R_PT[0][0]], inc=(sub == 3))
            V("dve", "tensor_copy", [R_PT[0][0]], [R_Qa], out=Qa[64:96, :], in_=PT[0][64:96, 0:512])

        sbi = 0
        pbi = 0
        load_head(0)
        q_load(0)
        if len(items) > 1:
            q_load(1)
        S1(0)
        S2(0)
        for i, (h, g) in enumerate(items):
            hb = h % 2
            Ka, R_Ka = Kaug[hb]
            Va, R_Va = Vaug[hb]
            Qa, R_Qa = Qaug[i % 2]
            if g == 0 and h + 1 < H:
                load_head(h + 1)
            if i + 2 < len(items):
                q_load(i + 2)
            if i + 1 < len(items):
                S1(i + 1)
            nkt = min(NKT, 8 * (g + 1))
            oacc, R_oacc = PO[i % 2], R_PO[i % 2]
            LA = 2
            slots = {}
            for it_ in range(nkt + LA):
                if it_ < nkt:
                    kt = it_
                    sps, R_sps = SB[sbi % 3]
                    sbi += 1
                    slots[kt] = (sps, R_sps)
                    MM(sps, Ka[0:96, kt * 128:(kt + 1) * 128], Qa[0:96, :], True, True, [R_Ka, R_Qa], [R_sps])
                kt = it_ - LA
                if kt >= 0:
                    sps, R_sps = slots.pop(kt)
                    pb, R_pb = pT[pbi % NPB]
                    pbi += 1
                    ACT(pb, sps, AF.Exp, [R_sps], [R_pb], scale=DH ** -0.5)
                    blk = kt // 2
                    if blk in (4 * g, 4 * g + 1):
                        c0 = (blk - 4 * g) * 256
                        V("dve", "tensor_tensor", [R_pb, R_cm], [R_pb], out=pb[:, c0:c0 + 256], in0=pb[:, c0:c0 + 256], in1=cm[:, kt % 2, :], op=ALU.mult)
                    MM(oacc[0:65, :], Va[:, kt, 0:65], pb, kt == 0, kt == nkt - 1, [R_Va, R_pb], [R_oacc], inc=True)
                if it_ == nkt // 2 and i + 1 < len(items):
                    S2(i + 1)
            V("dve", "tensor_copy", [R_oacc], [R_osb], out=osb[0:65, :], in_=oacc[0:65, :])
            for sub in range(4):
                TR(PM[:, sub * 128:sub * 128 + 65], osb[0:65, sub * 128:(sub + 1) * 128], idf[0:65, 0:65], [R_osb, R_idf], [R_PM], inc=(sub == 3))
            pm3 = PM.rearrange("p (s c) -> p s c", c=128)
            V("dve", "reciprocal", [R_PM], [R_rec], out=rec.unsqueeze(2), in_=pm3[:, :, 64:65])
            ast, R_ast = attn_st[i % 2]
            V("dve", "tensor_tensor", [R_PM, R_rec], [R_ast], out=ast, in0=pm3[:, :, 0:64], in1=rec.unsqueeze(2).to_broadcast([128, 4, 64]), op=ALU.mult)
            kb.dma("sp", attn_v[:, 4 * g:4 * g + 4, h * 64:(h + 1) * 64], ast, reads=[R_ast])
        kb.barrier()
        AR.off = MARK1
        if stop_after == 2:
            kb.emit()
            return nc

        wo_sb, R_wo = A([128, 8, 1024], BF16, "wo")
        wo32 = [A([128, 1024], F32, "wo32_%d" % i) for i in range(1)]
        at, R_at = A([128, 4, 512], F32, "at")
        an, R_an = A([128, 4, 512], BF16, "an")
        anT, R_anT = A([128, 4, 512], BF16, "anT")
        gmT, R_gmT = A([128, 4, 512], BF16, "gmT")
        obs = [A([128, 1024], F32, "ob%d" % i) for i in range(2)]
        nf_bc, R_nf = A([128, 1024], F32, "nf_bc")
        kb.dma("sp", nf_bc, nfin_d.partition_broadcast(128), writes=[R_nf])
        wout_v = wout_d.rearrange("(kc p) n -> p kc n", p=128)
        for kc in range(8):
            w32, R_w32 = wo32[0]
            kb.dma("sp", w32, wout_v[:, kc, :], writes=[R_w32])
            V("dve", "tensor_scalar", [R_w32, R_vT], [R_wo], out=wo_sb[:, kc, :], in0=w32, scalar1=vT[:, 104 + kc:105 + kc], scalar2=None, op0=ALU.mult)
        def load_at(t):
            tsl_ = slice(t * T, (t + 1) * T)
            kb.dma("sp", at, attn_scr[tsl_, :].rearrange("(s p) f -> p s f", p=128), writes=[R_at])
            kb.dma("sp", gmT, gmT_scr.rearrange("h p s -> p h s")[:, :, tsl_], writes=[R_gmT])

        load_at(0)
        for t in range(NP):
            tok0 = t * T
            tsl = slice(tok0, tok0 + T)
            kb.dma("sp", hx, h1_scr[tsl, :].rearrange("(s p) d -> p s d", p=128), writes=[R_hx])
            V("dve", "memset", [], [R_ss], ap=ss[:, 4:8], constant=0.0)
            for sub in range(4):
                ACT(junk[:, 0:512], at[:, sub, :], AF.Square, [R_at], [R_junk, R_ss], accum_out=ss[:, 4 + sub:5 + sub])
            rs = rstd_of(ss[:, 4:8], 4, 512, R_ss)
            for sub in range(4):
                V("pool", "tensor_scalar", [R_at, R_small], [R_an], out=an[:, sub, :], in0=at[:, sub, :], scalar1=rs[:, sub:sub + 1], scalar2=None, op0=ALU.mult)
            for fc in range(4):
                b, hf = fc % 2, 0
                for sub in range(4):
                    TR(PT[b][:, hf * 512 + sub * 128:hf * 512 + (sub + 1) * 128], an[:, sub, fc * 128:(fc + 1) * 128], idb, [R_an, R_idb], [R_PT[b][hf]], inc=(sub == 3))
                CPA(anT[:, fc, :], PT[b][:, hf * 512:(hf + 1) * 512], [R_PT[b][hf]], [R_anT])
            ssq_reset()
            for sub in range(4):
                sl = slice(sub * 128, (sub + 1) * 128)
                for half in range(2):
                    hs = slice(half * 512, (half + 1) * 512)
                    ps, R_ps = next_po()
                    for fc in range(4):
                        MM(ps, anT[:, fc, sl], wo_sb[:, fc, hs], fc == 0, False, [R_anT, R_wo], [R_ps], inc=False)
                    for fc in range(4):
                        MM(ps, gmT[:, fc, sl], wo_sb[:, 4 + fc, hs], False, fc == 3, [R_gmT, R_wo], [R_ps], inc=(fc == 3))
                    tm, R_tm = tmpo[(sub * 2 + half) % 2]
                    V("dve", "tensor_tensor", [R_ps, R_gt], [R_tm], out=tm, in0=ps, in1=gt_bc[:, 1, hs], op=ALU.mult)
                    V("pool", "tensor_tensor", [R_tm, R_hx], [R_hx], out=hx[:, sub, hs], in0=hx[:, sub, hs], in1=tm, op=ALU.add)
                    ssq_acc(sub, half)
            if t + 1 < NP:
                load_at(t + 1)
            ffn_pre(1)
            norm_T(2, pre=True)
            wdn_load(1)
            ffn(2, 1)
            ssq_sum()
            rs = rstd_of(ss[:, 0:4], 4, D, R_ss)
            for sub in range(4):
                ob, R_ob = obs[sub % 2]
                ACT(ob, hx[:, sub, :], AF.Identity, [R_hx, R_small], [R_ob], scale=rs[:, sub:sub + 1])
                V("dve", "tensor_tensor", [R_ob, R_nf], [R_ob], out=ob, in0=ob, in1=nf_bc, op=ALU.mult)
                kb.dma("sp", out_d[tok0 + sub * 128:tok0 + (sub + 1) * 128, :], ob, reads=[R_ob])
        kb.barrier()
        kb.emit()
    return nc


def _consts(S, r):
    half = 32
    inv_freq = (np.float32(10000.0) ** (-np.arange(half, dtype=np.float32) / np.float32(half))).astype(np.float32)
    pos = np.arange(S, dtype=np.float32)
    ang = (pos[:, None] * inv_freq[None, :]).astype(np.float32)
    cosG = np.cos(ang).astype(np.float32)
    sinG = np.sin(ang).astype(np.float32)
    NT = S // T
    NP = NT // 2
    own = [2 * p + r for p in range(NP)]
    oth = [2 * p + 1 - r for p in range(NP)]
    gat = lambda tab, tiles: np.concatenate([tab[t * T:(t + 1) * T] for t in tiles], axis=0)
    cosT = np.ascontiguousarray(np.stack([gat(cosG, own), gat(cosG, oth)], axis=0))
    sinT = np.ascontiguousarray(np.stack([gat(sinG, own), gat(sinG, oth)], axis=0))
    NB = S // BLK
    kind = np.zeros((32, S), np.float32)
    for j in range(NB):
        kind[j, j * BLK:(j + 1) * BLK] = 1.0
    k = np.arange(128)[:, None]
    q = np.arange(256)[None, :]
    cmask = np.stack([(q >= k), (q >= k + 128)], axis=1).astype(np.float32)
    nsub = (S // 2) // 128
    patt = np.full((nsub, 32), -1e30, np.float32)
    for s_ in range(nsub):
        p, ib = s_ // 4, (s_ % 4) // 2
        L = 4 * p + ib
        G = 2 * (2 * p + r) + ib
        for j in range(NB):
            pj, which, ibj = j // 4, (j % 4) // 2, j % 2
            gt = 2 * pj + (r if which == 0 else 1 - r)
            gb = 2 * gt + ibj
            if j == L:
                patt[s_, j] = 1e30
            elif gb < G:
                patt[s_, j] = 0.0
    tt = np.arange(128)[:, None]
    s2 = np.arange(128)[None, :]
    tril = (s2 <= tt).astype(np.float32)
    return dict(ident=np.eye(128, dtype=np.float32), cosT=cosT, sinT=sinT, kind=kind, cmask=cmask,
                patt=np.ascontiguousarray(patt.reshape(1, -1)), tril=tril), own, oth


def make_in_maps(inputs, S, cores):
    f = lambda a: np.ascontiguousarray(np.asarray(a, dtype=np.float32))
    b_ada = f(inputs["b_ada"])[0]
    shared = dict(
        bgate=f(b_ada.reshape(9, D)[[2, 5, 8]]),
        nfin=f(inputs["norm_final"]).reshape(1, D),
        lng=f(inputs["gmlp_ln_g"])[0].reshape(1, 512),
        lnb=f(inputs["gmlp_ln_b"])[0].reshape(1, 512),
        w_s=f(inputs["gmlp_w_s"])[0],
        w_ada=f(inputs["w_ada"])[0],
        w_gu1=f(inputs["w_ffn1_gu"])[0], w_gu2=f(inputs["w_ffn2_gu"])[0],
        w_dn1=f(inputs["w_ffn1_down"])[0], w_dn2=f(inputs["w_ffn2_down"])[0],
        w_in=f(inputs["w_in"])[0], w_out=f(inputs["w_out"])[0],
    )
    x = f(inputs["x"])
    c = f(inputs["c"])
    cst = {r: _consts(S, r) for r in (0, 1)}
    maps = []
    for (b, r) in cores:
        vecs = np.concatenate([
            b_ada.reshape(72, 128),
            f(inputs["norm_ffn1"])[0].reshape(8, 128),
            f(inputs["norm_mix"])[0].reshape(8, 128),
            f(inputs["norm_ffn2"])[0].reshape(8, 128),
            c[b].reshape(8, 128),
            f(inputs["g_attn_out"])[0].reshape(4, 128),
            f(inputs["g_gmlp_out"])[0].reshape(4, 128),
            f(inputs["gmlp_b_s"])[0].reshape(8, 128),
        ], axis=0)
        cd, own, oth = cst[r]
        m = dict(shared)
        m.update(cd)
        xb = x[b, :S]
        gat = lambda tiles: np.concatenate([xb[t * T:(t + 1) * T] for t in tiles], axis=0)
        m["x"] = np.ascontiguousarray(np.stack([gat(own), gat(oth)], axis=0))
        m["vecs"] = np.ascontiguousarray(vecs)
        maps.append(m)
    return maps


def assemble(results, S, cores, nbatch):
    out = np.zeros((nbatch, S, D), np.float32)
    NP = (S // T) // 2
    for (b, r), res in zip(cores, results):
        o = np.asarray(res["out"], dtype=np.float32)
        for p in range(NP):
            t = 2 * p + r
            out[b, t * T:(t + 1) * T] = o[p * T:(p + 1) * T]
    return out


_NC_CACHE = {}


def kernel(**inputs):
    S = 8192
    if S not in _NC_CACHE:
        _NC_CACHE[S] = build(S)
    nc = _NC_CACHE[S]
    cores = [(c // 2, c % 2) for c in range(8)]
    maps = make_in_maps(inputs, S, cores)
    res = run_bass_kernel_spmd(nc, maps, core_ids=list(range(8)))
    return assemble(res.results, S, cores, 4)
```

```python
import contextlib
import os
import numpy as np
import concourse.bass as bass
import concourse.mybir as mybir
from concourse.bass_utils import run_bass_kernel_spmd

F32 = mybir.dt.float32
BF16 = mybir.dt.bfloat16
AF = mybir.ActivationFunctionType
ALU = mybir.AluOpType
AX = mybir.AxisListType

ENGS = ("pe", "act", "dve", "pool", "sp")
SELF_SYNC = {"pe": False, "act": True, "dve": True, "pool": True, "sp": False}


class Res:
    __slots__ = ("name", "w", "r", "dsem")

    def __init__(self, name):
        self.name = name
        self.w = {}
        self.r = {}
        self.dsem = None


class KB:
    def __init__(self, nc):
        self.nc = nc
        self.q = {e: [] for e in ENGS}
        self.cnt = {e: 0 for e in ENGS}
        self.seen = {e: {} for e in ENGS}
        self.pend = {e: ([], []) for e in ENGS}
        self.dcnt = {}
        self.sems = {}
        self.nd = 0

    def _need(self, eng, waits, sem, c):
        if sem == eng and not SELF_SYNC[eng]:
            return
        if self.seen[eng].get(sem, 0) >= c:
            return
        if waits.get(sem, 0) < c:
            waits[sem] = c

    def _deps(self, eng, reads, writes):
        waits = {}
        for r in reads:
            for s, c in r.w.items():
                self._need(eng, waits, s, c)
        for w in writes:
            for s, c in w.w.items():
                self._need(eng, waits, s, c)
            for s, c in w.r.items():
                self._need(eng, waits, s, c)
        for s, c in waits.items():
            self.q[eng].append(("wait", s, c))
            self.seen[eng][s] = c

    def _mark(self, ev, reads, writes):
        s, c = ev
        for r in reads:
            if r.r.get(s, 0) < c:
                r.r[s] = c
        for w in writes:
            w.w = {s: c}
            w.r = {}

    def op(self, eng, fn, reads=(), writes=(), inc=True):
        for r in list(reads) + list(writes):
            for e2 in ENGS:
                if e2 != eng and (r in self.pend[e2][1]):
                    raise RuntimeError("resource %s pending on %s" % (r.name, e2))
        for w in writes:
            for e2 in ENGS:
                if e2 != eng and (w in self.pend[e2][0]):
                    raise RuntimeError("resource %s pending-read on %s" % (w.name, e2))
        self._deps(eng, reads, writes)
        self.q[eng].append(("op", fn, inc))
        if inc:
            self.cnt[eng] += 1
            ev = (eng, self.cnt[eng])
            pr, pw = self.pend[eng]
            self._mark(ev, pr, pw)
            self.pend[eng] = ([], [])
            self._mark(ev, reads, writes)
        else:
            self.pend[eng][0].extend(reads)
            self.pend[eng][1].extend(writes)

    def dma(self, eng, out, in_, reads=(), writes=(), sem=None, **kw):
        if sem is None:
            sem = (list(writes) + list(reads))[0]
        if isinstance(sem, Res):
            if sem.dsem is None:
                sem.dsem = "d%d_%s" % (self.nd, sem.name)
                self.nd += 1
            sem = sem.dsem
        self._deps(eng, reads, writes)
        self.dcnt[sem] = self.dcnt.get(sem, 0) + 16
        self.q[eng].append(("dma", out, in_, sem, kw))
        self._mark((sem, self.dcnt[sem]), reads, writes)

    def barrier(self, skip_prefix=None):
        for e in ENGS:
            assert not self.pend[e][0] and not self.pend[e][1], e
        allev = [(e, self.cnt[e]) for e in ENGS if self.cnt[e] > 0]
        allev += [(k_, v_) for k_, v_ in self.dcnt.items() if not (skip_prefix and k_.startswith(skip_prefix))]
        for e in ENGS:
            for s, c in allev:
                if s == e or c == 0:
                    continue
                if self.seen[e].get(s, 0) < c:
                    self.q[e].append(("wait", s, c))
                    self.seen[e][s] = c

    def emit(self):
        nc = self.nc
        names = list(ENGS) + list(self.dcnt.keys())
        import contextlib
        with contextlib.ExitStack() as es:
            for n in names:
                self.sems[n] = es.enter_context(nc.semaphore("s_" + n))
            block = es.enter_context(nc.Block())

            def run(e, eng):
                for it in self.q[e]:
                    if it[0] == "wait":
                        eng.wait_ge(self.sems[it[1]], it[2])
                    elif it[0] == "op":
                        ins = it[1](eng)
                        if it[2]:
                            ins.then_inc(self.sems[e], 1)
                    else:
                        _, out, in_, sem, kw = it
                        eng.dma_start(out=out, in_=in_, **kw).then_inc(self.sems[sem], 16)

            @block.tensor
            def _(eng):
                run("pe", eng)

            @block.scalar
            def _(eng):
                run("act", eng)

            @block.vector
            def _(eng):
                run("dve", eng)

            @block.gpsimd
            def _(eng):
                run("pool", eng)

            @block.sync
            def _(eng):
                run("sp", eng)


D = 1024
DFF = 2816
NJ = DFF // 128
H = 8
DH = 64
T = 512
BLK = 256
EPS = 1e-6
BIG = 30000.0
CW = 53000


class Arena:
    def __init__(self, ap):
        self.ap = ap
        self.off = 0

    def alloc(self, shape, dt):
        n = 1
        for s in shape[1:]:
            n *= s
        nf = n if dt == F32 else (n + 1) // 2
        a = self.ap[:, self.off:self.off + nf]
        self.off += nf
        assert self.off <= CW, ("arena overflow", self.off)
        v = a if dt == F32 else a.bitcast(BF16)[:, 0:n]
        if len(shape) == 3:
            v = v.rearrange("p (a b) -> p a b", a=shape[1])
        elif len(shape) == 4:
            v = v.rearrange("p (a b c) -> p a b c", a=shape[1], b=shape[2])
        return v


def build(S, debug=False, stop_after=9):
    NT = S // T
    NP = NT // 2
    SO = S // 2
    NKT = S // 128
    NB = S // BLK
    nc = bass.Bass("TRN2", target_bir_lowering=False)

    def din(name, shape, dt=F32):
        return nc.dram_tensor(name, list(shape), dt, kind="ExternalInput").ap()

    def dscr(name, shape, dt, dbg=False):
        kind = "ExternalOutput" if (debug and dbg) else "Internal"
        return nc.dram_tensor(name, list(shape), dt, kind=kind).ap()

    x_d = din("x", [2, SO, D])
    vecs_d = din("vecs", [120, 128])
    bgate_d = din("bgate", [3, D])
    nfin_d = din("nfin", [1, D])
    lng_d = din("lng", [1, 512])
    lnb_d = din("lnb", [1, 512])
    ws_d = din("w_s", [8, 128, 128])
    wada_d = din("w_ada", [D, 9 * D])
    wgu_d = [din("w_gu1", [D, 2 * DFF]), din("w_gu2", [D, 2 * DFF])]
    wdn_d = [din("w_dn1", [DFF, D]), din("w_dn2", [DFF, D])]
    win_d = din("w_in", [D, 2560])
    wout_d = din("w_out", [D, D])
    ident_d = din("ident", [128, 128])
    cos_d = din("cosT", [2, SO, 32])
    sin_d = din("sinT", [2, SO, 32])
    kind_d = din("kind", [32, S])
    cm_d = din("cmask", [128, 2, 256])
    patt_d = din("patt", [1, (SO // 128) * 32])
    tril_d = din("tril", [128, 128])
    out_d = nc.dram_tensor("out", [SO, D], F32, kind="ExternalOutput").ap()

    wgu_b = [dscr("wgu1_b", [D, 2 * DFF], BF16), dscr("wgu2_b", [D, 2 * DFF], BF16)]
    wdn_b = [dscr("wdn1_b", [DFF, D], BF16), dscr("wdn2_b", [DFF, D], BF16)]
    win_b = dscr("win_b", [D, 2560], BF16)
    h1_scr = dscr("h1_scr", [SO, D], F32, True)
    q_scr = dscr("q_scr", [4, 128, SO], F32, True)
    k_scr = dscr("k_scr", [4, 128, S], BF16)
    v_scr = dscr("v_scr", [8, 128, NKT, 65], BF16)
    gmT_scr = dscr("gmT_scr", [4, 128, SO], BF16)
    km_scr = dscr("km_scr", [4, 128, NB], F32, True)
    attn_scr = dscr("attn_scr", [SO, 512], F32, True)

    es = contextlib.ExitStack()
    with es:
        arena_t = es.enter_context(nc.sbuf_tensor("arena", [128, CW], F32))
        AR = Arena(arena_t[:, :])
        PGU = [es.enter_context(nc.psum_tensor("pgu%d" % i, [128, 512], F32))[:, :] for i in range(4)]
        PO = [es.enter_context(nc.psum_tensor("po%d" % i, [128, 512], F32))[:, :] for i in range(2)]
        PT = [es.enter_context(nc.psum_tensor("pt%d" % i, [128, 1024], BF16))[:, :] for i in range(2)]
        R_PGU = [Res("pgu%d" % i) for i in range(4)]
        R_PO = [Res("po%d" % i) for i in range(2)]
        R_PT = []
        for i in range(2):
            r_ = Res("pt%d" % i)
            R_PT.append([r_, r_])
        kb = KB(nc)

        def MM(out, lhsT, rhs, st, sp, R=(), W=(), inc=True):
            kb.op("pe", lambda e: e.matmul(out, lhsT=lhsT, rhs=rhs, start=st, stop=sp), R, W, inc)

        def TR(out, in_, ident, R=(), W=(), inc=True):
            kb.op("pe", lambda e: e.transpose(out=out, in_=in_, identity=ident), R, W, inc)

        def ACT(out, in_, func, R=(), W=(), **kw):
            kb.op("act", lambda e: e.activation(out=out, in_=in_, func=func, **kw), R, W)

        def CPA(out, in_, R=(), W=()):
            kb.op("act", lambda e: e.copy(out=out, in_=in_), R, W)

        def V(eng, name, R=(), W=(), **kw):
            if eng == "pool":
                eng = "dve"
            kb.op(eng, lambda e: getattr(e, name)(**kw), R, W)

        po_i = [0]

        def next_po():
            i = po_i[0] % 2
            po_i[0] += 1
            return PO[i], R_PO[i]

        def A(shape, dt, name):
            return AR.alloc(shape, dt), Res(name)

        idf, R_idf = A([128, 128], F32, "idf")
        idb, R_idb = A([128, 128], BF16, "idb")
        vT, R_vT = A([128, 120], F32, "vT")
        modT, R_modT = A([128, 72], F32, "modT")
        Asc, R_Asc = A([128, 3, 8], F32, "Asc")
        gt_bc, R_gt = A([128, 3, 1024], F32, "gt_bc")
        lng_bc, R_lng = A([128, 512], F32, "lng")
        lnb_bc, R_lnb = A([128, 512], F32, "lnb")
        WsT, R_WsT = A([128, 8, 128], BF16, "WsT")
        patt, R_patt = A([128, SO // 128, 32], F32, "patt")
        cm, R_cm = A([128, 2, 256], BF16, "cm")
        onesc, R_onesc = A([128, 2], F32, "onesc")
        kms, R_kms = A([128, 4, NKT], F32, "kms")
        small, R_small = A([128, 64], F32, "small")
        MARK_P = AR.off
        hx, R_hx = A([128, 4, 1024], F32, "hx")
        tb, R_tb = A([128, 4, 1024], BF16, "tb")
        yT, R_yT = A([128, 8, 512], BF16, "yT")
        aT, R_aT = A([128, NJ, 512], BF16, "aT")
        NWG = 2
        wg = [A([128, 8, 2, 256], BF16, "wg%d" % i) for i in range(NWG)]
        wdn, R_wdn = A([128, NJ, 1024], BF16, "wdn")
        sg = [A([128, 512], F32, "sg%d" % i) for i in range(2)]
        tmpo = [A([128, 512], F32, "tmpo%d" % i) for i in range(2)]
        ss, R_ss = A([128, 16], F32, "ss")
        nsm, _ = A([128, 32], F32, "nsm")
        ssq, R_ssq = A([128, 8], F32, "ssq")
        R_nsq = [Res("nsq%d" % i) for i in range(4)]
        junk, R_junk = A([128, 1024], BF16, "junk")
        MARK0 = AR.off
        MARK1 = MARK0

        kb.dma("sp", idf, ident_d, writes=[R_idf])
        V("dve", "tensor_copy", [R_idf], [R_idb], out=idb, in_=idf)
        R_wgub = [Res("wgu1b"), Res("wgu2b")]
        R_wdnb = [Res("wdn1b"), Res("wdn2b")]
        R_winb = Res("winb")

        def cast_w(dst, src, rows, step, R, sem):
            for r0 in range(0, rows, step):
                kb.dma("pool", dst[r0:r0 + step, :], src[r0:r0 + step, :], writes=[R], sem=sem)

        cast_w(wgu_b[0], wgu_d[0], D, 128, R_wgub[0], "pre_gu1")
        kb.dma("pool", cm, cm_d, writes=[R_cm])

        vecs_sb, R_vecs = A([128, 128], F32, "vecs")
        kb.dma("sp", vecs_sb[0:120, :], vecs_d, writes=[R_vecs])
        kb.dma("sp", lng_bc, lng_d.partition_broadcast(128), writes=[R_lng])
        kb.dma("sp", lnb_bc, lnb_d.partition_broadcast(128), writes=[R_lnb])
        kb.dma("sp", patt.rearrange("p a b -> p (a b)"), patt_d.partition_broadcast(128), writes=[R_patt])
        bg_bc, R_bg = A([128, 3, 1024], F32, "bg_bc")
        for gi in range(3):
            kb.dma("sp", bg_bc[:, gi, :], bgate_d[gi:gi + 1, :].partition_broadcast(128), writes=[R_bg])
        V("dve", "memset", [], [R_onesc], ap=onesc, constant=1.0 / BLK)
        TR(PO[0][:, 0:120], vecs_sb[0:120, :], idf[0:120, 0:120], [R_vecs, R_idf], [R_PO[0]])
        CPA(vT, PO[0][:, 0:120], [R_PO[0]], [R_vT])
        cact, R_cact = A([128, 8], F32, "cact")
        cT, R_cT = A([128, 8], BF16, "cT")
        crep, R_crep = A([128, 8, 128], BF16, "crep")
        ACT(cact, vT[:, 96:104], AF.Silu, [R_vT], [R_cact])
        V("dve", "tensor_copy", [R_cact], [R_cT], out=cT, in_=cact)
        for kc in range(8):
            V("dve", "tensor_copy", [R_cT], [R_crep], out=crep[:, kc, :],
              in_=cT[:, kc:kc + 1].to_broadcast([128, 128]))
        for gi in range(3):
            V("dve", "tensor_scalar", [R_bg], [R_bg], out=bg_bc[:, gi, :], in0=bg_bc[:, gi, :],
              scalar1=(1.0 if gi == 1 else 0.5), scalar2=None, op0=ALU.mult)
        wa = [A([128, 8, 512], BF16, "wa%d" % i) for i in range(4)]
        wada_v = wada_d.rearrange("(kc p) n -> p kc n", p=128)
        PMOD, R_PMOD = PGU[0], R_PGU[0]
        for cg in range(18):
            wab, R_wab = wa[cg % 4]
            for kh in range(2):
                kb.dma("pool", wab[:, 4 * kh:4 * kh + 4, :], wada_v[:, 4 * kh:4 * kh + 4, cg * 512:(cg + 1) * 512],
                       writes=[R_wab])
            for cc in range(4):
                col = cg * 4 + cc
                for kc in range(8):
                    MM(PMOD[:, col:col + 1], wab[:, kc, cc * 128:(cc + 1) * 128], cT[:, kc:kc + 1],
                       kc == 0, kc == 7, [R_wab, R_cT], [R_PMOD], inc=(kc == 7))
            v = cg // 2
            if v in (2, 5, 8):
                gi = (v - 2) // 3
                half = cg % 2
                ps, R_ps = PGU[2 + half], R_PGU[2 + half]
                for kc in range(8):
                    MM(ps, crep[:, kc, :], wab[:, kc, :], kc == 0, kc == 7, [R_wab, R_crep], [R_ps], inc=(kc == 7))
                V("dve", "scalar_tensor_tensor", [R_ps, R_bg], [R_gt], out=gt_bc[:, gi, half * 512:(half + 1) * 512],
                  in0=ps, scalar=(1.0 if gi == 1 else 0.5), in1=bg_bc[:, gi, half * 512:(half + 1) * 512],
                  op0=ALU.mult, op1=ALU.add)
        cast_w(wdn_b[0], wdn_d[0], DFF, 256, R_wdnb[0], "pre_dn1")
        cast_w(win_b, win_d, D, 256, R_winb, "pre_in")
        cast_w(wgu_b[1], wgu_d[1], D, 128, R_wgub[1], "pre_gu2")
        cast_w(wdn_b[1], wdn_d[1], DFF, 256, R_wdnb[1], "pre_dn2")
        V("dve", "tensor_tensor", [R_PMOD, R_vT], [R_modT], out=modT, in0=PMOD[:, 0:72], in1=vT[:, 0:72], op=ALU.add)
        for k in range(3):
            V("dve", "scalar_tensor_tensor", [R_modT, R_vT], [R_Asc], out=Asc[:, k, :],
              in0=modT[:, (3 * k + 1) * 8:(3 * k + 1) * 8 + 8], scalar=1.0, in1=vT[:, 72 + 8 * k:80 + 8 * k],
              op0=ALU.add, op1=ALU.mult)
        ws32, R_ws32 = A([128, 8, 128], F32, "ws32")
        trilm, R_tril = A([128, 128], F32, "tril")
        wsm, R_wsm = A([128, 8, 128], BF16, "wsm")
        kb.dma("sp", ws32, ws_d.rearrange("h t s -> t h s"), writes=[R_ws32])
        kb.dma("sp", trilm, tril_d, writes=[R_tril])
        for h in range(8):
            V("dve", "tensor_tensor", [R_ws32, R_tril], [R_wsm], out=wsm[:, h, :], in0=ws32[:, h, :], in1=trilm, op=ALU.mult)
        for h in range(8):
            TR(PT[0][:, h * 128:(h + 1) * 128], wsm[:, h, :], idb, [R_wsm, R_idb], [R_PT[0][0], R_PT[0][1]], inc=(h == 7))
        V("dve", "tensor_copy", [R_PT[0][0], R_PT[0][1]], [R_WsT], out=WsT.rearrange("p h t -> p (h t)"), in_=PT[0][:, 0:1024])
        kb.barrier()
        AR.off = MARK0
        if stop_after == 0:
            kb.emit()
            return nc

        def rstd_of(ss_ap, n, nfeat, R_in):
            V("dve", "tensor_scalar", [R_in], [R_small], out=small[:, 16:16 + n], in0=ss_ap, scalar1=1.0 / nfeat,
              scalar2=EPS, op0=ALU.mult, op1=ALU.add)
            ACT(small[:, 32:32 + n], small[:, 16:16 + n], AF.Sqrt, [R_small], [R_small])
            V("dve", "reciprocal", [R_small], [R_small], out=small[:, 0:n], in_=small[:, 32:32 + n])
            return small[:, 0:n]

        def norm_T(k, pre=False):
            norm_stats(pre)
            norm_tr(k)

        def ssq_reset():
            V("dve", "memset", [], [R_ssq], ap=ssq, constant=0.0)

        def ssq_acc(sub, half):
            hs_ = slice(half * 512, (half + 1) * 512)
            ACT(junk[:, 0:512], hx[:, sub, hs_], AF.Square, [R_hx, R_ssq], [R_junk, R_ssq], accum_out=ssq[:, 2 * sub + half:2 * sub + half + 1])

        def ssq_sum():
            sq4 = ssq.rearrange("p (s two) -> p s two", two=2)
            V("dve", "tensor_tensor", [R_ssq], [R_ss], out=ss[:, 0:4].unsqueeze(2), in0=sq4[:, :, 0:1], in1=sq4[:, :, 1:2], op=ALU.add)

        def norm_stats(pre=False):
            KD = 9
            R_sq = [R_nsq[i] for i in range(4)]
            if pre:
                ssq_sum()
            else:
                V("dve", "memset", [], [R_ss], ap=ss[:, 0:4], constant=0.0)

            def sq(sub):
                if not pre:
                    ACT(junk, hx[:, sub, :], AF.Square, [R_hx, R_ss], [R_junk, R_sq[sub]], accum_out=ss[:, sub:sub + 1])

            sq(0)
            for sub in range(4):
                if sub + 1 < 4:
                    sq(sub + 1)
                c0 = 16 + sub
                V("dve", "tensor_scalar", [R_sq[sub], R_ss], [R_sq[sub]], out=nsm[:, c0:c0 + 1], in0=ss[:, sub:sub + 1], scalar1=1.0 / D,
                  scalar2=EPS, op0=ALU.mult, op1=ALU.add)
                ACT(nsm[:, 8 + sub:9 + sub], nsm[:, c0:c0 + 1], AF.Sqrt, [R_sq[sub]], [R_sq[sub]])
                V("dve", "reciprocal", [R_sq[sub]], [R_sq[sub]], out=nsm[:, sub:sub + 1], in_=nsm[:, 8 + sub:9 + sub])
                if sub % 2 == 0:
                    V("dve", "tensor_scalar", [R_hx, R_sq[sub]], [R_tb], out=tb[:, sub, :], in0=hx[:, sub, :],
                      scalar1=nsm[:, sub:sub + 1], scalar2=None, op0=ALU.mult)
                else:
                    ACT(tb[:, sub, :], hx[:, sub, :], AF.Identity, [R_hx, R_sq[sub]], [R_tb], scale=nsm[:, sub:sub + 1])

        def norm_tr(k):
            KD = 9
            for c in range(8):
                b, hf = c % 2, 0
                for sub in range(4):
                    TR(PT[b][:, hf * 512 + sub * 128: hf * 512 + (sub + 1) * 128], tb[:, sub, c * 128:(c + 1) * 128], idb,
                       [R_tb, R_idb], [R_PT[b][hf]], inc=(sub == 3))
                if KD == 3:
                    continue
                if c % 2 == 0:
                    ACT(yT[:, c, :], PT[b][:, hf * 512:(hf + 1) * 512], AF.Identity, [R_PT[b][hf], R_Asc, R_modT], [R_yT],
                        scale=Asc[:, k, c:c + 1], bias=modT[:, 3 * k * 8 + c:3 * k * 8 + c + 1])
                else:
                    V("dve", "tensor_scalar", [R_PT[b][hf], R_Asc, R_modT], [R_yT], out=yT[:, c, :], in0=PT[b][:, hf * 512:(hf + 1) * 512],
                      scalar1=Asc[:, k, c:c + 1], scalar2=modT[:, 3 * k * 8 + c:3 * k * 8 + c + 1], op0=ALU.mult, op1=ALU.add)

        def wg_load(f, jp):
            wgu_v = wgu_b[f].rearrange("(kc p) (two n) -> p kc two n", p=128, two=2)
            wgb, R_wgb = wg[jp % NWG]
            for two in range(2):
                kb.dma("sp", wgb[:, :, two, :], wgu_v[:, :, two, jp * 256:(jp + 1) * 256], reads=[R_wgub[f]], writes=[R_wgb])

        def ffn_pre(f):
            for jp in range(NWG):
                wg_load(f, jp)

        def wdn_load(f):
            wdn_v = wdn_b[f].rearrange("(j p) d -> p j d", p=128)
            ssq_reset()
            for jh in range(2):
                kb.dma("sp", wdn[:, 11 * jh:11 * jh + 11, :], wdn_v[:, 11 * jh:11 * jh + 11, :], reads=[R_wdnb[f]], writes=[R_wdn])

        def ffn(k, f):
            for jp in range(NJ // 2):
                wgb, R_wgb = wg[jp % NWG]
                if jp >= NWG:
                    wg_load(f, jp)
                for jl in range(2):
                    jj = 2 * jp + jl
                    s = jj % 2
                    pg, R_pg = PGU[2 * s], R_PGU[2 * s]
                    pu, R_pu = PGU[2 * s + 1], R_PGU[2 * s + 1]
                    for kc in range(8):
                        MM(pg, wgb[:, kc, 0, jl * 128:(jl + 1) * 128], yT[:, kc, :], kc == 0, kc == 7, [R_wgb, R_yT], [R_pg], inc=(kc == 7))
                    for kc in range(8):
                        MM(pu, wgb[:, kc, 1, jl * 128:(jl + 1) * 128], yT[:, kc, :], kc == 0, kc == 7, [R_wgb, R_yT], [R_pu], inc=(kc == 7))
                    sgb, R_sgb = sg[s]
                    ACT(sgb, pg, AF.Silu, [R_pg], [R_sgb])
                    V("dve", "tensor_tensor", [R_sgb, R_pu], [R_aT], out=aT[:, jj, :], in0=sgb, in1=pu, op=ALU.mult)
            for sub in range(4):
                for half in range(2):
                    ps, R_ps = next_po()
                    for jj in range(NJ):
                        MM(ps, aT[:, jj, sub * 128:(sub + 1) * 128], wdn[:, jj, half * 512:(half + 1) * 512], jj == 0, jj == NJ - 1,
                           [R_aT, R_wdn], [R_ps], inc=(jj == NJ - 1))
                    tm, R_tm = tmpo[(sub * 2 + half) % 2]
                    V("dve", "tensor_tensor", [R_ps, R_gt], [R_tm], out=tm, in0=ps, in1=gt_bc[:, k, half * 512:(half + 1) * 512], op=ALU.mult)
                    V("pool", "tensor_tensor", [R_tm, R_hx], [R_hx], out=hx[:, sub, half * 512:(half + 1) * 512],
                      in0=hx[:, sub, half * 512:(half + 1) * 512], in1=tm, op=ALU.add)
                    ssq_acc(sub, half)

        wi = [A([128, 8, 512], BF16, "wi%d" % i) for i in range(2)]
        win_v = win_b.rearrange("(kc p) n -> p kc n", p=128)
        cosb, R_cos = A([128, 4, 32], F32, "cosb")
        sinb, R_sin = A([128, 4, 32], F32, "sinb")
        qr, R_qr = A([128, 8, 2, 32], F32, "qr")
        rtmp, R_rtmp = A([128, 8, 32], F32, "rtmp")
        k16, R_k16 = A([128, 512], BF16, "k16")
        qT_st, R_qTst = A([128, 4, 512], F32, "qTst")
        kT_st, R_kTst = A([128, 4, 512], BF16, "kTst")
        v_st, R_vst = A([128, 4, 8, 65], BF16, "vst")
        V("dve", "memset", [], [R_vst], ap=v_st, constant=1.0)
        gmT_st, R_gmTst = A([128, 4, 512], BF16, "gmTst")
        gug, R_gug = A([128, 4, 512], F32, "gug")
        gvg, R_gvg = A([128, 512], F32, "gvg")
        vn, R_vn = A([128, 512], BF16, "vn")
        gm, R_gm = A([128, 512], F32, "gm")
        gmn, R_gmn = A([128, 512], BF16, "gmn")
        bsT = vT[:, 112:120]
        wi_i = [0]

        def rope(ps, dst, R_ps, R_dst, sub):
            p4 = ps.rearrange("p (h t d) -> p h t d", h=8, t=2)
            cb = cosb[:, sub:sub + 1, :].to_broadcast([128, 8, 32])
            sb_ = sinb[:, sub:sub + 1, :].to_broadcast([128, 8, 32])
            V("dve", "tensor_tensor", [R_ps, R_cos], [R_dst], out=dst[:, :, 0, :], in0=p4[:, :, 0, :], in1=cb, op=ALU.mult)
            V("dve", "tensor_tensor", [R_ps, R_sin], [R_rtmp], out=rtmp, in0=p4[:, :, 1, :], in1=sb_, op=ALU.mult)
            V("dve", "tensor_tensor", [R_dst, R_rtmp], [R_dst], out=dst[:, :, 0, :], in0=dst[:, :, 0, :], in1=rtmp, op=ALU.subtract)
            V("dve", "tensor_tensor", [R_ps, R_cos], [R_dst], out=dst[:, :, 1, :], in0=p4[:, :, 1, :], in1=cb, op=ALU.mult)
            V("dve", "tensor_tensor", [R_ps, R_sin], [R_rtmp], out=rtmp, in0=p4[:, :, 0, :], in1=sb_, op=ALU.mult)
            V("dve", "tensor_tensor", [R_dst, R_rtmp], [R_dst], out=dst[:, :, 1, :], in0=dst[:, :, 1, :], in1=rtmp, op=ALU.add)

        ALLPO = [(PO[0], R_PO[0]), (PO[1], R_PO[1])] + [(PGU[i], R_PGU[i]) for i in range(4)]
        po6 = [0]

        def next_po6():
            i = po6[0] % 6
            po6[0] += 1
            return ALLPO[i]

        def load_x(t):
            pr, which = t // 2, t % 2
            kb.dma("sp", hx, x_d[which, pr * T:(pr + 1) * T, :].rearrange("(s p) d -> p s d", p=128), writes=[R_hx])

        def load_cs(t):
            pr, which = t // 2, t % 2
            kb.dma("sp", cosb, cos_d[which, pr * T:(pr + 1) * T, :].rearrange("(s p) d -> p s d", p=128), writes=[R_cos])
            kb.dma("sp", sinb, sin_d[which, pr * T:(pr + 1) * T, :].rearrange("(s p) d -> p s d", p=128), writes=[R_sin])

        def stage_b(t, grp, sub, ps, R_ps):
            sl = slice(sub * 128, (sub + 1) * 128)
            st_i = t * 4 + sub
            if grp == 0:
                rope(ps, qr, R_ps, R_qr, sub)
                qf = qr.rearrange("p h t d -> p (h t d)")
                ps2, R_ps2 = next_po6()
                for hp in range(4):
                    TR(ps2[:, hp * 128:(hp + 1) * 128], qf[:, hp * 128:(hp + 1) * 128], idf, [R_qr, R_idf], [R_ps2], inc=(hp == 3))
                CPA(qT_st[:, :, sl], ps2.rearrange("p (h s) -> p h s", h=4), [R_ps2], [R_qTst])
            elif grp == 1:
                rope(ps, qr, R_ps, R_qr, sub)
                kf = qr.rearrange("p h t d -> p (h t d)")
                ps2, R_ps2 = next_po6()
                for hp in range(4):
                    MM(ps2[:, hp:hp + 1], kf[:, hp * 128:(hp + 1) * 128], onesc[:, 0:1], True, True, [R_qr, R_onesc], [R_ps2], inc=(hp == 3))
                V("dve", "tensor_copy", [R_ps2], [R_kms], out=kms[:, :, st_i], in_=ps2[:, 0:4])
                V("dve", "tensor_copy", [R_qr], [R_k16], out=k16, in_=kf)
                for hp in range(4):
                    TR(PT[0][:, hp * 128:(hp + 1) * 128], k16[:, hp * 128:(hp + 1) * 128], idb, [R_k16, R_idb], [R_PT[0][0]], inc=(hp == 3))
                CPA(kT_st[:, :, sl], PT[0][:, 0:512].rearrange("p (h s) -> p h s", h=4), [R_PT[0][0]], [R_kTst])
            elif grp == 2:
                CPA(v_st[:, sub, :, 0:64], ps.rearrange("p (h d) -> p h d", h=8), [R_ps], [R_vst])
            elif grp == 3:
                ACT(gug[:, sub, :], ps, AF.Gelu_apprx_tanh, [R_ps], [R_gug])
            else:
                V("dve", "memset", [], [R_ss], ap=ss[:, 4:8], constant=0.0)
                ACT(gvg, ps, AF.Gelu_apprx_tanh, [R_ps], [R_gvg, R_ss], accum_out=ss[:, 4:5])
                ACT(junk[:, 0:512], gvg, AF.Square, [R_gvg], [R_junk, R_ss], accum_out=ss[:, 5:6])
                V("dve", "tensor_scalar", [R_ss], [R_ss], out=ss[:, 8:9], in0=ss[:, 4:5], scalar1=1.0 / 512, scalar2=None, op0=ALU.mult)
                V("dve", "tensor_tensor", [R_ss], [R_ss], out=ss[:, 9:10], in0=ss[:, 8:9], in1=ss[:, 8:9], op=ALU.mult)
                V("dve", "scalar_tensor_tensor", [R_ss], [R_ss], out=ss[:, 10:11], in0=ss[:, 5:6], scalar=1.0 / 512, in1=ss[:, 9:10],
                  op0=ALU.mult, op1=ALU.subtract)
                V("dve", "tensor_scalar", [R_ss], [R_ss], out=ss[:, 11:12], in0=ss[:, 10:11], scalar1=EPS, scalar2=None, op0=ALU.add)
                ACT(ss[:, 12:13], ss[:, 11:12], AF.Sqrt, [R_ss], [R_ss])
                V("dve", "reciprocal", [R_ss], [R_ss], out=ss[:, 13:14], in_=ss[:, 12:13])
                V("dve", "scalar_tensor_tensor", [R_ss], [R_ss], out=ss[:, 14:15], in0=ss[:, 8:9], scalar=-1.0, in1=ss[:, 13:14],
                  op0=ALU.mult, op1=ALU.mult)
                ACT(gvg, gvg, AF.Identity, [R_gvg, R_ss], [R_gvg], scale=ss[:, 13:14], bias=ss[:, 14:15])
                V("dve", "tensor_tensor", [R_gvg, R_lng], [R_gvg], out=gvg, in0=gvg, in1=lng_bc, op=ALU.mult)
                V("dve", "tensor_tensor", [R_gvg, R_lnb], [R_vn], out=vn, in0=gvg, in1=lnb_bc, op=ALU.add)
                ps3, R_ps3 = next_po6()
                for hg in range(8):
                    MM(ps3[:, hg * 64:(hg + 1) * 64], WsT[:, hg, :], vn[:, hg * 64:(hg + 1) * 64], True, True, [R_WsT, R_vn], [R_ps3], inc=(hg == 7))
                V("dve", "tensor_tensor", [R_ps3, R_vT], [R_gm], out=gm.rearrange("p (h d) -> p h d", h=8),
                  in0=ps3.rearrange("p (h d) -> p h d", h=8), in1=bsT.unsqueeze(2).to_broadcast([128, 8, 64]), op=ALU.add)
                V("dve", "tensor_tensor", [R_gm, R_gug], [R_gm], out=gm, in0=gm, in1=gug[:, sub, :], op=ALU.mult)
                ACT(junk[:, 0:512], gm, AF.Square, [R_gm], [R_junk, R_ss], accum_out=ss[:, 6:7])
                rs = rstd_of(ss[:, 6:7], 1, 512, R_ss)
                V("dve", "tensor_scalar", [R_gm, R_small], [R_gmn], out=gmn, in0=gm, scalar1=rs[:, 0:1], scalar2=None, op0=ALU.mult)
                for fc in range(4):
                    TR(PT[1][:, fc * 128:(fc + 1) * 128], gmn[:, fc * 128:(fc + 1) * 128], idb, [R_gmn, R_idb], [R_PT[1][0]], inc=(fc == 3))
                CPA(gmT_st[:, :, sl], PT[1][:, 0:512].rearrange("p (h s) -> p h s", h=4), [R_PT[1][0]], [R_gmTst])

        load_x(0)
        load_cs(0)
        for t in range(NT):
            pr, which = t // 2, t % 2
            own_t = (which == 0)
            tok0 = t * T
            tsl = slice(tok0, tok0 + T)
            osl = slice(pr * T, (pr + 1) * T)
            ffn_pre(0)
            if t == 0:
                norm_stats()
            norm_tr(0)
            wdn_load(0)
            ffn(0, 0)
            if own_t:
                kb.dma("sp", h1_scr[osl, :].rearrange("(s p) d -> p s d", p=128), hx, reads=[R_hx])
            grps = list(range(5)) if own_t else [1, 2]

            def wi_load(grp_):
                wib_, R_wib_ = wi[wi_i[0] % 2]
                wi_i[0] += 1
                for kh in range(2):
                    kb.dma("sp", wib_[:, 4 * kh:4 * kh + 4, :], win_v[:, 4 * kh:4 * kh + 4, grp_ * 512:(grp_ + 1) * 512], reads=[R_winb], writes=[R_wib_])
                return wib_, R_wib_

            nxt_w = wi_load(grps[0])
            norm_T(1, pre=True)
            if t + 1 < NT:
                load_x(t + 1)
            for gi_, grp in enumerate(grps):
                wib, R_wib = nxt_w
                if gi_ + 1 < len(grps):
                    nxt_w = wi_load(grps[gi_ + 1])
                if gi_ == len(grps) - 1 and t + 1 < NT:
                    norm_stats()
                pend = None
                for sub in range(4):
                    sl = slice(sub * 128, (sub + 1) * 128)
                    ps, R_ps = next_po6()
                    for kc in range(8):
                        MM(ps, yT[:, kc, sl], wib[:, kc, :], kc == 0, kc == 7, [R_yT, R_wib], [R_ps], inc=(kc == 7))
                    if pend is not None:
                        stage_b(t, grp, *pend)
                    pend = (sub, ps, R_ps)
                stage_b(t, grp, *pend)
                if grp == 0:
                    kb.dma("sp", q_scr.rearrange("h p s -> p h s")[:, :, osl], qT_st, reads=[R_qTst])
                elif grp == 1:
                    kb.dma("sp", k_scr.rearrange("h p s -> p h s")[:, :, tsl], kT_st, reads=[R_kTst])
                    if t + 1 < NT:
                        load_cs(t + 1)
                elif grp == 2:
                    for sub_ in range(4):
                        kb.dma("act", v_scr.rearrange("h p k d -> p k h d")[:, 4 * t + sub_, :, :], v_st[:, sub_, :, :], reads=[R_vst])
                elif grp == 4:
                    kb.dma("sp", gmT_scr.rearrange("h p s -> p h s")[:, :, osl], gmT_st, reads=[R_gmTst])
        kmv = kms.rearrange("p h (b two) -> p h b two", two=2)
        kmb, R_kmb = A([128, 4, NB], F32, "kmb")
        V("dve", "tensor_tensor", [R_kms], [R_kmb], out=kmb, in0=kmv[:, :, :, 0], in1=kmv[:, :, :, 1], op=ALU.add)
        kb.dma("sp", km_scr.rearrange("h p b -> p h b"), kmb, reads=[R_kmb])
        kb.barrier()
        AR.off = MARK1
        if stop_after == 1:
            kb.emit()
            return nc

        NG = NP
        AR.off = MARK_P
        Kaug = [A([128, S], BF16, "kaug%d" % i) for i in range(2)]
        Vaug = [A([128, NKT, 65], BF16, "vaug%d" % i) for i in range(2)]
        kmh = [A([128, NB], F32, "kmh%d" % i) for i in range(2)]
        Q32 = [A([128, 512], F32, "q32_%d" % i) for i in range(3)]
        Qaug = [A([128, 512], BF16, "qaug%d" % i) for i in range(2)]
        NPB = 3
        pT = [A([128, 512], BF16, "pT%d" % i) for i in range(NPB)]
        osb, R_osb = A([128, 512], F32, "osb")
        stg, R_stg = A([128, 4, 96], BF16, "stg")
        gsb, R_gsb = A([128, 32], F32, "gsb")
        mx8, R_mx8 = A([128, 8], F32, "mx8")
        mtmp, R_mtmp = A([128, 32], F32, "mtmp")
        rec, R_rec = A([128, 4], F32, "rec")
        attn_st = [A([128, 4, 64], F32, "attnst%d" % i) for i in range(2)]
        V("dve", "memset", [], [R_stg], ap=stg, constant=0.0)
        V("dve", "memset", [], [R_gsb], ap=gsb, constant=-1e30)
        for i in range(2):
            kb.dma("pool", Kaug[i][0][64:96, :], kind_d, writes=[Kaug[i][1]], sem="kind%d" % i)
        SB = [(PGU[0], R_PGU[0]), (PGU[1], R_PGU[1]), (PGU[2], R_PGU[2])]
        PM, R_PM = PGU[3], R_PGU[3]
        attn_v = attn_scr.rearrange("(s p) f -> p s f", p=128)

        def load_head(h):
            hp, r0 = h // 2, (h % 2) * 64
            hb = h % 2
            kb.dma("act", Kaug[hb][0][0:64, :], k_scr[hp, r0:r0 + 64, :], writes=[Kaug[hb][1]])
            kb.dma("act", Vaug[hb][0], v_scr[h], writes=[Vaug[hb][1]])
            kb.dma("act", kmh[hb][0][0:64, :], km_scr[hp, r0:r0 + 64, :], writes=[kmh[hb][1]])

        items = [(h, g) for h in range(H) for g in range(NG)]

        def q_load(i):
            h, g = items[i]
            hp, r0 = h // 2, (h % 2) * 64
            Q3, R_Q3 = Q32[i % 3]
            kb.dma("sp", Q3[0:64, :], q_scr[hp, r0:r0 + 64, g * 512:(g + 1) * 512], writes=[R_Q3])

        def S1(i):
            h, g = items[i]
            km, R_km = kmh[h % 2]
            Q3, R_Q3 = Q32[i % 3]
            Qa, R_Qa = Qaug[i % 2]
            V("dve", "tensor_copy", [R_Q3], [R_Qa], out=Qa[0:64, :], in_=Q3[0:64, :])
            for sub in range(4):
                MM(PM[:, sub * 32:sub * 32 + NB], Q3[0:64, sub * 128:(sub + 1) * 128], km[0:64, 0:NB], True, True, [R_Q3, R_km], [R_PM], inc=(sub == 3))
            for sub in range(4):
                V("dve", "tensor_tensor", [R_PM, R_patt], [R_gsb], out=gsb[:, 0:NB], in0=PM[:, sub * 32:sub * 32 + NB], in1=patt[:, 4 * g + sub, 0:NB], op=ALU.add)
                V("dve", "max", [R_gsb], [R_mx8], out=mx8, in_=gsb)
                V("dve", "tensor_scalar", [R_mx8], [R_mx8], out=mx8[:, 4:5], in0=mx8[:, 3:4], scalar1=-1e29, scalar2=None, op0=ALU.max)
                V("dve", "tensor_scalar", [R_gsb, R_mx8], [R_mtmp], out=mtmp, in0=gsb, scalar1=mx8[:, 4:5], scalar2=BIG,
                  op0=ALU.is_ge, op1=ALU.mult)
                V("dve", "tensor_scalar", [R_mtmp], [R_stg], out=stg[:, sub, 64:96], in0=mtmp, scalar1=-BIG, scalar2=None, op0=ALU.add)

        def S2(i):
            Qa, R_Qa = Qaug[i % 2]
            for sub in range(4):
                TR(PT[0][0:96, sub * 128:(sub + 1) * 128], stg[:, sub, :], idb, [R_stg, R_idb], [R_PT[0][0]], inc=(sub == 3))
            V("dve", "tensor_copy", [R_PT[0][0]], [R_Qa], out=Qa[64:96, :], in_=PT[0][64:96, 0:512])

        sbi = 0
        pbi = 0
        load_head(0)
        q_load(0)
        if len(items) > 1:
            q_load(1)
        S1(0)
        S2(0)
        for i, (h, g) in enumerate(items):
            hb = h % 2
            Ka, R_Ka = Kaug[hb]
            Va, R_Va = Vaug[hb]
            Qa, R_Qa = Qaug[i % 2]
            if g == 0 and h + 1 < H:
                load_head(h + 1)
            if i + 2 < len(items):
                q_load(i + 2)
            if i + 1 < len(items):
                S1(i + 1)
            nkt = min(NKT, 8 * (g + 1))
            oacc, R_oacc = PO[i % 2], R_PO[i % 2]
            LA = 2
            slots = {}
            for it_ in range(nkt + LA):
                if it_ < nkt:
                    kt = it_
                    sps, R_sps = SB[sbi % 3]
                    sbi += 1
                    slots[kt] = (sps, R_sps)
                    MM(sps, Ka[0:96, kt * 128:(kt + 1) * 128], Qa[0:96, :], True, True, [R_Ka, R_Qa], [R_sps])
                kt = it_ - LA
                if kt >= 0:
                    sps, R_sps = slots.pop(kt)
                    pb, R_pb = pT[pbi % NPB]
                    pbi += 1
                    ACT(pb, sps, AF.Exp, [R_sps], [R_pb], scale=DH ** -0.5)
                    blk = kt // 2
                    if blk in (4 * g, 4 * g + 1):
                        c0 = (blk - 4 * g) * 256
                        V("dve", "tensor_tensor", [R_pb, R_cm], [R_pb], out=pb[:, c0:c0 + 256], in0=pb[:, c0:c0 + 256], in1=cm[:, kt % 2, :], op=ALU.mult)
                    MM(oacc[0:65, :], Va[:, kt, 0:65], pb, kt == 0, kt == nkt - 1, [R_Va, R_pb], [R_oacc], inc=True)
                if it_ == nkt // 2 and i + 1 < len(items):
                    S2(i + 1)
            V("dve", "tensor_copy", [R_oacc], [R_osb], out=osb[0:65, :], in_=oacc[0:65, :])
            for sub in range(4):
                TR(PM[:, sub * 128:sub * 128 + 65], osb[0:65, sub * 128:(sub + 1) * 128], idf[0:65, 0:65], [R_osb, R_idf], [R_PM], inc=(sub == 3))
            pm3 = PM.rearrange("p (s c) -> p s c", c=128)
            V("dve", "reciprocal", [R_PM], [R_rec], out=rec.unsqueeze(2), in_=pm3[:, :, 64:65])
            ast, R_ast = attn_st[i % 2]
            V("dve", "tensor_tensor", [R_PM, R_rec], [R_ast], out=ast, in0=pm3[:, :, 0:64], in1=rec.unsqueeze(2).to_broadcast([128, 4, 64]), op=ALU.mult)
            kb.dma("sp", attn_v[:, 4 * g:4 * g + 4, h * 64:(h + 1) * 64], ast, reads=[R_ast])
        kb.barrier()
        AR.off = MARK1
        if stop_after == 2:
            kb.emit()
            return nc

        wo_sb, R_wo = A([128, 8, 1024], BF16, "wo")
        wo32 = [A([128, 1024], F32, "wo32_%d" % i) for i in range(1)]
        at, R_at = A([128, 4, 512], F32, "at")
        an, R_an = A([128, 4, 512], BF16, "an")
        anT, R_anT = A([128, 4, 512], BF16, "anT")
        gmT, R_gmT = A([128, 4, 512], BF16, "gmT")
        obs = [A([128, 1024], F32, "ob%d" % i) for i in range(2)]
        nf_bc, R_nf = A([128, 1024], F32, "nf_bc")
        kb.dma("sp", nf_bc, nfin_d.partition_broadcast(128), writes=[R_nf])
        wout_v = wout_d.rearrange("(kc p) n -> p kc n", p=128)
        for kc in range(8):
            w32, R_w32 = wo32[0]
            kb.dma("sp", w32, wout_v[:, kc, :], writes=[R_w32])
            V("dve", "tensor_scalar", [R_w32, R_vT], [R_wo], out=wo_sb[:, kc, :], in0=w32, scalar1=vT[:, 104 + kc:105 + kc], scalar2=None, op0=ALU.mult)
        def load_at(t):
            tsl_ = slice(t * T, (t + 1) * T)
            kb.dma("sp", at, attn_scr[tsl_, :].rearrange("(s p) f -> p s f", p=128), writes=[R_at])
            kb.dma("sp", gmT, gmT_scr.rearrange("h p s -> p h s")[:, :, tsl_], writes=[R_gmT])

        def attn_norm():
            V("dve", "memset", [], [R_ss], ap=ss[:, 4:8], constant=0.0)
            for sub in range(4):
                ACT(junk[:, 0:512], at[:, sub, :], AF.Square, [R_at], [R_junk, R_ss], accum_out=ss[:, 4 + sub:5 + sub])
            rs = rstd_of(ss[:, 4:8], 4, 512, R_ss)
            for sub in range(4):
                V("pool", "tensor_scalar", [R_at, R_small], [R_an], out=an[:, sub, :], in0=at[:, sub, :], scalar1=rs[:, sub:sub + 1], scalar2=None, op0=ALU.mult)

        load_at(0)
        attn_norm()
        for t in range(NP):
            tok0 = t * T
            tsl = slice(tok0, tok0 + T)
            kb.dma("sp", hx, h1_scr[tsl, :].rearrange("(s p) d -> p s d", p=128), writes=[R_hx])
            for fc in range(4):
                b, hf = fc % 2, 0
                for sub in range(4):
                    TR(PT[b][:, hf * 512 + sub * 128:hf * 512 + (sub + 1) * 128], an[:, sub, fc * 128:(fc + 1) * 128], idb, [R_an, R_idb], [R_PT[b][hf]], inc=(sub == 3))
                CPA(anT[:, fc, :], PT[b][:, hf * 512:(hf + 1) * 512], [R_PT[b][hf]], [R_anT])
            ssq_reset()
            for sub in range(4):
                sl = slice(sub * 128, (sub + 1) * 128)
                for half in range(2):
                    hs = slice(half * 512, (half + 1) * 512)
                    ps, R_ps = next_po()
                    for fc in range(4):
                        MM(ps, anT[:, fc, sl], wo_sb[:, fc, hs], fc == 0, False, [R_anT, R_wo], [R_ps], inc=False)
                    for fc in range(4):
                        MM(ps, gmT[:, fc, sl], wo_sb[:, 4 + fc, hs], False, fc == 3, [R_gmT, R_wo], [R_ps], inc=(fc == 3))
                    tm, R_tm = tmpo[(sub * 2 + half) % 2]
                    V("dve", "tensor_tensor", [R_ps, R_gt], [R_tm], out=tm, in0=ps, in1=gt_bc[:, 1, hs], op=ALU.mult)
                    V("pool", "tensor_tensor", [R_tm, R_hx], [R_hx], out=hx[:, sub, hs], in0=hx[:, sub, hs], in1=tm, op=ALU.add)
                    ssq_acc(sub, half)
            if t + 1 < NP:
                load_at(t + 1)
            ffn_pre(1)
            norm_T(2, pre=True)
            if t + 1 < NP:
                attn_norm()
            wdn_load(1)
            ffn(2, 1)
            ssq_sum()
            rs = rstd_of(ss[:, 0:4], 4, D, R_ss)
            for sub in range(4):
                ob, R_ob = obs[sub % 2]
                ACT(ob, hx[:, sub, :], AF.Identity, [R_hx, R_small], [R_ob], scale=rs[:, sub:sub + 1])
                V("dve", "tensor_tensor", [R_ob, R_nf], [R_ob], out=ob, in0=ob, in1=nf_bc, op=ALU.mult)
                kb.dma("sp", out_d[tok0 + sub * 128:tok0 + (sub + 1) * 128, :], ob, reads=[R_ob])
        kb.barrier()
        kb.emit()
    return nc


def _consts(S, r):
    half = 32
    inv_freq = (np.float32(10000.0) ** (-np.arange(half, dtype=np.float32) / np.float32(half))).astype(np.float32)
    pos = np.arange(S, dtype=np.float32)
    ang = (pos[:, None] * inv_freq[None, :]).astype(np.float32)
    cosG = np.cos(ang).astype(np.float32)
    sinG = np.sin(ang).astype(np.float32)
    NT = S // T
    NP = NT // 2
    own = [2 * p + r for p in range(NP)]
    oth = [2 * p + 1 - r for p in range(NP)]
    gat = lambda tab, tiles: np.concatenate([tab[t * T:(t + 1) * T] for t in tiles], axis=0)
    cosT = np.ascontiguousarray(np.stack([gat(cosG, own), gat(cosG, oth)], axis=0))
    sinT = np.ascontiguousarray(np.stack([gat(sinG, own), gat(sinG, oth)], axis=0))
    NB = S // BLK
    kind = np.zeros((32, S), np.float32)
    for j in range(NB):
        kind[j, j * BLK:(j + 1) * BLK] = 1.0
    k = np.arange(128)[:, None]
    q = np.arange(256)[None, :]
    cmask = np.stack([(q >= k), (q >= k + 128)], axis=1).astype(np.float32)
    nsub = (S // 2) // 128
    patt = np.full((nsub, 32), -1e30, np.float32)
    for s_ in range(nsub):
        p, ib = s_ // 4, (s_ % 4) // 2
        L = 4 * p + ib
        G = 2 * (2 * p + r) + ib
        for j in range(NB):
            pj, which, ibj = j // 4, (j % 4) // 2, j % 2
            gt = 2 * pj + (r if which == 0 else 1 - r)
            gb = 2 * gt + ibj
            if j == L:
                patt[s_, j] = 1e30
            elif gb < G:
                patt[s_, j] = 0.0
    tt = np.arange(128)[:, None]
    s2 = np.arange(128)[None, :]
    tril = (s2 <= tt).astype(np.float32)
    return dict(ident=np.eye(128, dtype=np.float32), cosT=cosT, sinT=sinT, kind=kind, cmask=cmask,
                patt=np.ascontiguousarray(patt.reshape(1, -1)), tril=tril), own, oth


def make_in_maps(inputs, S, cores):
    f = lambda a: np.ascontiguousarray(np.asarray(a, dtype=np.float32))
    b_ada = f(inputs["b_ada"])[0]
    shared = dict(
        bgate=f(b_ada.reshape(9, D)[[2, 5, 8]]),
        nfin=f(inputs["norm_final"]).reshape(1, D),
        lng=f(inputs["gmlp_ln_g"])[0].reshape(1, 512),
        lnb=f(inputs["gmlp_ln_b"])[0].reshape(1, 512),
        w_s=f(inputs["gmlp_w_s"])[0],
        w_ada=f(inputs["w_ada"])[0],
        w_gu1=f(inputs["w_ffn1_gu"])[0], w_gu2=f(inputs["w_ffn2_gu"])[0],
        w_dn1=f(inputs["w_ffn1_down"])[0], w_dn2=f(inputs["w_ffn2_down"])[0],
        w_in=f(inputs["w_in"])[0], w_out=f(inputs["w_out"])[0],
    )
    x = f(inputs["x"])
    c = f(inputs["c"])
    cst = {r: _consts(S, r) for r in (0, 1)}
    maps = []
    for (b, r) in cores:
        vecs = np.concatenate([
            b_ada.reshape(72, 128),
            f(inputs["norm_ffn1"])[0].reshape(8, 128),
            f(inputs["norm_mix"])[0].reshape(8, 128),
            f(inputs["norm_ffn2"])[0].reshape(8, 128),
            c[b].reshape(8, 128),
            f(inputs["g_attn_out"])[0].reshape(4, 128),
            f(inputs["g_gmlp_out"])[0].reshape(4, 128),
            f(inputs["gmlp_b_s"])[0].reshape(8, 128),
        ], axis=0)
        cd, own, oth = cst[r]
        m = dict(shared)
        m.update(cd)
        xb = x[b, :S]
        gat = lambda tiles: np.concatenate([xb[t * T:(t + 1) * T] for t in tiles], axis=0)
        m["x"] = np.ascontiguousarray(np.stack([gat(own), gat(oth)], axis=0))
        m["vecs"] = np.ascontiguousarray(vecs)
        maps.append(m)
    return maps


def assemble(results, S, cores, nbatch):
    out = np.zeros((nbatch, S, D), np.float32)
    NP = (S // T) // 2
    for (b, r), res in zip(cores, results):
        o = np.asarray(res["out"], dtype=np.float32)
        for p in range(NP):
            t = 2 * p + r
            out[b, t * T:(t + 1) * T] = o[p * T:(p + 1) * T]
    return out


_NC_CACHE = {}


def kernel(**inputs):
    S = 8192
    if S not in _NC_CACHE:
        _NC_CACHE[S] = build(S)
    nc = _NC_CACHE[S]
    cores = [(c // 2, c % 2) for c in range(8)]
    maps = make_in_maps(inputs, S, cores)
    res = run_bass_kernel_spmd(nc, maps, core_ids=list(range(8)))
    return assemble(res.results, S, cores, 4)
```
